# Optimizing a Trainium2 kernel written in Bass

```python
import math
import jax, jax.numpy as jnp
from jax import lax
import numpy as np

D_MODEL = 1024
BATCH = 8
SEQ = 4096
DEPTH = 4

CHUNK = 64

MIX_WIDTH = D_MODEL
SSM_WIDTH = MIX_WIDTH // 2
SSM_GROUP_CH = 16
SSM_GROUPS = SSM_WIDTH // SSM_GROUP_CH
SSM_STATE = 64
DT_MIN = 1e-3
DT_MAX = 1e-1
ATTN_WIDTH = MIX_WIDTH - SSM_WIDTH
HEAD_DIM = 64
N_HEADS = ATTN_WIDTH // HEAD_DIM
Q_BLOCK = 128
IN_WIDTH = SSM_WIDTH + 3 * ATTN_WIDTH + N_HEADS

PEER_HEADS = 8
PEER_N_KEYS = 128
PEER_EXPERTS = PEER_N_KEYS * PEER_N_KEYS
PEER_TOPK = 16
PEER_QUERY_DIM = 128
PEER_HALF = PEER_QUERY_DIM // 2
PEER_BLOCK = 128

RMS_EPS = 1e-6

kernel_name = "hybrid_s5_fox_peer_trunk"


def rms_norm(x, g):
    xf = x.astype(jnp.float32)
    y = xf * lax.rsqrt(jnp.mean(xf * xf, axis=-1, keepdims=True) + RMS_EPS)
    return (y * g.astype(jnp.float32)).astype(x.dtype)


def _ssm_combine(e1, e2):
    a1r, a1i, b1r, b1i = e1
    a2r, a2i, b2r, b2i = e2
    return (a2r * a1r - a2i * a1i,
            a2r * a1i + a2i * a1r,
            a2r * b1r - a2i * b1i + b2r,
            a2r * b1i + a2i * b1r + b2i)


def s5_mixer(u, lam_re, lam_im, log_dt, b_re, b_im, c_re, c_im, d_skip, w_glu, b_glu):
    bsz, seq, _ = u.shape
    f32 = jnp.float32
    uf = u.astype(f32).reshape(bsz, seq, SSM_GROUPS, SSM_GROUP_CH)
    dt = jnp.exp(log_dt.astype(f32))[:, None]
    lr = lam_re.astype(f32)
    li = lam_im.astype(f32)
    mag = jnp.exp(lr * dt)
    a_re = mag * jnp.cos(li * dt)
    a_im = mag * jnp.sin(li * dt)
    den = lr * lr + li * li
    nr = a_re - 1.0
    z_re = (nr * lr + a_im * li) / den
    z_im = (a_im * lr - nr * li) / den
    br = b_re.astype(f32)
    bi = b_im.astype(f32)
    bb_re = z_re[..., None] * br - z_im[..., None] * bi
    bb_im = z_re[..., None] * bi + z_im[..., None] * br
    x_re = jnp.einsum('bsgh,gph->bsgp', uf, bb_re)
    x_im = jnp.einsum('bsgh,gph->bsgp', uf, bb_im)
    ar = jnp.broadcast_to(a_re[None, None], (1, seq, SSM_GROUPS, SSM_STATE))
    ai = jnp.broadcast_to(a_im[None, None], (1, seq, SSM_GROUPS, SSM_STATE))
    _, _, s_re, s_im = lax.associative_scan(_ssm_combine, (ar, ai, x_re, x_im), axis=1)
    y = (jnp.einsum('bsgp,ghp->bsgh', s_re, c_re.astype(f32))
         - jnp.einsum('bsgp,ghp->bsgh', s_im, c_im.astype(f32))
         + d_skip.astype(f32) * uf)
    g = jax.nn.gelu(y.reshape(bsz, seq, SSM_WIDTH))
    out = g * jax.nn.sigmoid(g @ w_glu.astype(f32) + b_glu.astype(f32))
    return out.astype(u.dtype)


def fox_attention(q, k, v, f_logit):
    bsz, seq, _ = q.shape
    qh = q.reshape(bsz, seq, N_HEADS, HEAD_DIM).transpose(0, 2, 1, 3)
    kh = k.reshape(bsz, seq, N_HEADS, HEAD_DIM).transpose(0, 2, 1, 3)
    vh = v.reshape(bsz, seq, N_HEADS, HEAD_DIM).transpose(0, 2, 1, 3)
    log_f = jax.nn.log_sigmoid(f_logit.astype(jnp.float32))
    cum = jnp.cumsum(log_f, axis=1).transpose(0, 2, 1)
    scale = HEAD_DIM ** -0.5
    outs = []
    for blk in range(seq // Q_BLOCK):
        q0 = blk * Q_BLOCK
        q1 = q0 + Q_BLOCK
        logits = jnp.einsum('bhqd,bhkd->bhqk', qh[:, :, q0:q1], kh[:, :, :q1]).astype(jnp.float32) * scale
        logits = logits + cum[:, :, q0:q1, None] - cum[:, :, None, :q1]
        causal = jnp.arange(q0, q1)[:, None] >= jnp.arange(q1)[None, :]
        logits = jnp.where(causal, logits, -jnp.inf)
        probs = jax.nn.softmax(logits, axis=-1).astype(vh.dtype)
        outs.append(jnp.einsum('bhqk,bhkd->bhqd', probs, vh[:, :, :q1]))
    out = jnp.concatenate(outs, axis=2)
    return out.transpose(0, 2, 1, 3).reshape(bsz, seq, ATTN_WIDTH)


def peer_ffn(h, w_q, sub_keys, u_tab, v_tab):
    bsz, seq, d = h.shape
    q = (h @ w_q).reshape(bsz, seq, PEER_HEADS, 2, PEER_HALF)
    s1 = jnp.einsum('bshd,hnd->bshn', q[..., 0, :], sub_keys[:, 0]).astype(jnp.float32)
    s2 = jnp.einsum('bshd,hnd->bshn', q[..., 1, :], sub_keys[:, 1]).astype(jnp.float32)
    t1, i1 = lax.top_k(s1, PEER_TOPK)
    t2, i2 = lax.top_k(s2, PEER_TOPK)
    cand = (t1[..., :, None] + t2[..., None, :]).reshape(bsz, seq, PEER_HEADS, PEER_TOPK * PEER_TOPK)
    cand_idx = (i1[..., :, None] * PEER_N_KEYS + i2[..., None, :]).reshape(bsz, seq, PEER_HEADS, PEER_TOPK * PEER_TOPK)
    best, pos = lax.top_k(cand, PEER_TOPK)
    idx = jnp.take_along_axis(cand_idx, pos, axis=-1)
    gate = jax.nn.softmax(best, axis=-1)
    n_tok = bsz * seq
    n_blk = n_tok // PEER_BLOCK
    hf = h.reshape(n_blk, PEER_BLOCK, d)
    idxf = idx.reshape(n_blk, PEER_BLOCK, PEER_HEADS * PEER_TOPK)
    gf = gate.reshape(n_blk, PEER_BLOCK, PEER_HEADS * PEER_TOPK)

    def block(args):
        hb, ib, gb = args
        ub = jnp.take(u_tab, ib, axis=0)
        vb = jnp.take(v_tab, ib, axis=0)
        act = jax.nn.gelu(jnp.einsum('tkd,td->tk', ub, hb).astype(jnp.float32))
        coef = (act * gb).astype(hb.dtype)
        return jnp.einsum('tk,tkd->td', coef, vb)

    out = lax.map(block, (hf, idxf, gf))
    return out.reshape(bsz, seq, d)


def setup_inputs(seed: int = 0) -> dict:
    key = jax.random.key(seed)
    ks = jax.random.split(key, 24)
    f32 = jnp.float32
    L, D, G, P, Hc = DEPTH, D_MODEL, SSM_GROUPS, SSM_STATE, SSM_GROUP_CH
    nrm = lambda k, shape, s: jax.random.normal(k, shape, f32) * s
    x = jax.random.normal(ks[0], (BATCH, SEQ, D), f32)
    norm1_g = 1.0 + nrm(ks[1], (L, D), 0.01)
    w_in = nrm(ks[2], (L, D, IN_WIDTH), D ** -0.5)
    n_idx = jnp.arange(P, dtype=f32)
    ssm_lambda_re = -0.5 + nrm(ks[3], (L, G, P), 0.01)
    ssm_lambda_im = math.pi * n_idx + nrm(ks[4], (L, G, P), 0.01)
    ssm_log_dt = jax.random.uniform(ks[5], (L, G), f32, math.log(DT_MIN), math.log(DT_MAX))
    b_scale = (2.0 * Hc) ** -0.5
    ssm_b_re = nrm(ks[6], (L, G, P, Hc), b_scale)
    ssm_b_im = nrm(ks[7], (L, G, P, Hc), b_scale)
    c_scale = (2.0 * P) ** -0.5
    ssm_c_re = nrm(ks[8], (L, G, Hc, P), c_scale)
    ssm_c_im = nrm(ks[9], (L, G, Hc, P), c_scale)
    ssm_d = nrm(ks[10], (L, G, Hc), 1.0)
    ssm_w_glu = nrm(ks[11], (L, SSM_WIDTH, SSM_WIDTH), SSM_WIDTH ** -0.5)
    ssm_b_glu = nrm(ks[12], (L, SSM_WIDTH), 0.01)
    fox_b_f = jnp.linspace(1.0, 5.0, N_HEADS, dtype=f32)[None, :] + nrm(ks[13], (L, N_HEADS), 0.1)
    g_ssm_out = 1.0 + nrm(ks[14], (L, SSM_WIDTH), 0.01)
    g_attn_out = 1.0 + nrm(ks[15], (L, ATTN_WIDTH), 0.01)
    w_o = nrm(ks[16], (L, MIX_WIDTH, D), MIX_WIDTH ** -0.5)
    norm2_g = 1.0 + nrm(ks[17], (L, D), 0.01)
    peer_w_q = nrm(ks[18], (L, D, PEER_HEADS * PEER_QUERY_DIM), D ** -0.5)
    peer_keys = nrm(ks[19], (L, PEER_HEADS, 2, PEER_N_KEYS, PEER_HALF), PEER_HALF ** -0.5)
    peer_u = nrm(ks[20], (L, PEER_EXPERTS, D), D ** -0.5)
    peer_v = nrm(ks[21], (L, PEER_EXPERTS, D), 0.25)
    norm_f = 1.0 + nrm(ks[22], (D,), 0.01)
    return {"x": x, "norm1_g": norm1_g, "w_in": w_in,
            "ssm_lambda_re": ssm_lambda_re, "ssm_lambda_im": ssm_lambda_im, "ssm_log_dt": ssm_log_dt,
            "ssm_b_re": ssm_b_re, "ssm_b_im": ssm_b_im, "ssm_c_re": ssm_c_re, "ssm_c_im": ssm_c_im,
            "ssm_d": ssm_d, "ssm_w_glu": ssm_w_glu, "ssm_b_glu": ssm_b_glu,
            "fox_b_f": fox_b_f, "g_ssm_out": g_ssm_out, "g_attn_out": g_attn_out, "w_o": w_o,
            "norm2_g": norm2_g, "peer_w_q": peer_w_q, "peer_keys": peer_keys,
            "peer_u": peer_u, "peer_v": peer_v, "norm_f": norm_f}


def reference(x, norm1_g, w_in, ssm_lambda_re, ssm_lambda_im, ssm_log_dt, ssm_b_re, ssm_b_im,
              ssm_c_re, ssm_c_im, ssm_d, ssm_w_glu, ssm_b_glu, fox_b_f, g_ssm_out, g_attn_out, w_o,
              norm2_g, peer_w_q, peer_keys, peer_u, peer_v, norm_f):
    h = x
    o_q = SSM_WIDTH
    o_k = o_q + ATTN_WIDTH
    o_v = o_k + ATTN_WIDTH
    o_f = o_v + ATTN_WIDTH
    for l in range(DEPTH):
        xn = rms_norm(h, norm1_g[l])
        proj = xn @ w_in[l]
        u_ssm = proj[..., :o_q]
        q = proj[..., o_q:o_k]
        k = proj[..., o_k:o_v]
        v = proj[..., o_v:o_f]
        f_logit = proj[..., o_f:] + fox_b_f[l]
        y_ssm = s5_mixer(u_ssm, ssm_lambda_re[l], ssm_lambda_im[l], ssm_log_dt[l], ssm_b_re[l], ssm_b_im[l],
                         ssm_c_re[l], ssm_c_im[l], ssm_d[l], ssm_w_glu[l], ssm_b_glu[l])
        y_att = fox_attention(q, k, v, f_logit)
        mixed = jnp.concatenate([rms_norm(y_ssm, g_ssm_out[l]), rms_norm(y_att, g_attn_out[l])], axis=-1)
        h = h + mixed @ w_o[l]
        h = h + peer_ffn(rms_norm(h, norm2_g[l]), peer_w_q[l], peer_keys[l], peer_u[l], peer_v[l])
    return rms_norm(h, norm_f)
```

```python
import math
from contextlib import ExitStack
import numpy as np
import concourse.bass as bass
import concourse.mybir as mybir
from concourse.bass_utils import run_bass_kernel_spmd

F32 = mybir.dt.float32
BF16 = mybir.dt.bfloat16
I32 = mybir.dt.int32
U32 = mybir.dt.uint32
ALU = mybir.AluOpType
AF = mybir.ActivationFunctionType
AX = mybir.AxisListType

D = 1024
NH = 8
EPS = 1e-6
NEXP = 16384


class Prog:
    NDMA = 24

    def __init__(self, nc):
        self.nc = nc
        self.eng = {"pe": nc.tensor, "act": nc.scalar, "dve": nc.vector,
                    "pool": nc.gpsimd, "sp": nc.sync}
        self.sem = {k: nc.alloc_semaphore("sem_" + k) for k in self.eng}
        self.cnt = {k: 0 for k in self.eng}
        self.dsem = [nc.alloc_semaphore("dsem%d" % i) for i in range(2 * self.NDMA)]
        self.dval = [0] * (2 * self.NDMA)
        self.dnext = {"sp": 0, "pool": 0}
        self.seen = {k: {} for k in self.eng}
        self.lastw = {}
        self.readers = {}
        self.ninst = 0

    def _wait(self, e, tok):
        sem, val, uid = tok
        if e == "pe" and uid == "epe":
            return
        if self.seen[e].get(uid, 0) >= val:
            return
        self.seen[e][uid] = val
        self.eng[e].wait_ge(sem, val)
        self.ninst += 1

    def _deps(self, e, reads, writes):
        for k in reads:
            t = self.lastw.get(k)
            if t is not None:
                self._wait(e, t)
        for k in writes:
            t = self.lastw.get(k)
            if t is not None:
                self._wait(e, t)
            for t in self.readers.get(k, ()):
                self._wait(e, t)

    def _commit(self, tok, reads, writes):
        for k in reads:
            self.readers.setdefault(k, []).append(tok)
        for k in writes:
            self.lastw[k] = tok
            self.readers[k] = []
        self.ninst += 1

    def op(self, e, fn, reads=(), writes=()):
        self._deps(e, reads, writes)
        inst = fn(self.eng[e])
        self.cnt[e] += 1
        inst.then_inc(self.sem[e], 1)
        tok = (self.sem[e], self.cnt[e], "e" + e)
        self._commit(tok, reads, writes)
        return tok

    def dma(self, e, fn, reads=(), writes=()):
        k = self.dnext[e] + (self.NDMA if e == "pool" else 0)
        self.dnext[e] = (self.dnext[e] + 1) % self.NDMA
        if self.dval[k] > 0:
            self._wait(e, (self.dsem[k], self.dval[k], "d%d" % k))
        self._deps(e, reads, writes)
        inst = fn(self.eng[e])
        self.dval[k] += 16
        inst.then_inc(self.dsem[k], 16)
        tok = (self.dsem[k], self.dval[k], "d%d" % k)
        self._commit(tok, reads, writes)
        return tok

    def barrier(self):
        for e in self.eng:
            for o in self.eng:
                if self.cnt[o] > 0:
                    self._wait(e, (self.sem[o], self.cnt[o], "e" + o))
            for k in range(2 * self.NDMA):
                if self.dval[k] > 0:
                    self._wait(e, (self.dsem[k], self.dval[k], "d%d" % k))
        self.lastw = {}
        self.readers = {}


def build(L, S, dbg=False, phases="12345", last_final=True, stop=99):
    nc = bass.Bass("TRN2", target_bir_lowering=False)
    P = Prog(nc)
    NT, NG = S // 128, S // 512
    LOGS = int(math.log2(S))
    assert 1 << LOGS == S and NG >= 1

    def din(name, shape, dt=F32):
        return nc.dram_tensor(name, list(shape), dt, kind="ExternalInput").ap()

    def dscr(name, shape, dt):
        return nc.dram_tensor(name, list(shape), dt, kind=("ExternalOutput" if dbg else "Internal")).ap()

    uniq = [0]

    def sb(st, name, shape, dt):
        uniq[0] += 1
        return st.enter_context(nc.sbuf_tensor("s%d_%s" % (uniq[0], name), list(shape), dt))

    x_d = din("x", [S, D])
    g1T_d = din("g1T", [L, 128, 8])
    w_in_d = din("w_in", [L, D, 2056])
    bf_d = din("b_f", [L, 8, 1])
    lamB_re_d = din("lamB_re", [L, 4, 128, 64])
    lamB_im_d = din("lamB_im", [L, 4, 128, 64])
    dtB_d = din("dtB", [L, 4, 128, 1])
    bT_re_d = din("bT_re", [L, 4, 128, 64])
    bT_im_d = din("bT_im", [L, 4, 128, 64])
    lamT_re_d = din("lamT_re", [L, 128, 16])
    lamT_im_d = din("lamT_im", [L, 128, 16])
    dtT_d = din("dtT", [L, 128, 16])
    cT_re_d = din("cT_re", [L, 128, 16, 32])
    cT_im_d = din("cT_im", [L, 128, 16, 32])
    dmat_d = din("dmat", [L, 128, 16, 32])
    wglu_d = din("w_glu", [L, 512, 512])
    bgluT_d = din("bgluT", [L, 128, 4])
    gsaT_d = din("gsaT", [L, 128, 8])
    w_o_d = din("w_o", [L, D, D])
    g2_d = din("g2", [L, 1, D])
    w_q_d = din("w_q", [L, D, D])
    keysT_d = din("keysT", [L, 128, 16, 128])
    pu_d = din("peer_u", [L * NEXP, D])
    pv_d = din("peer_v", [L * NEXP, D])
    nf_d = din("norm_f", [1, D])
    ident_d = din("ident", [128, 128])
    negmask_d = din("negmask", [128, 128])
    gmask_d = din("gmask", [128, 8])
    iota16_d = din("iota16", [128, 16])
    out_d = nc.dram_tensor("out", [S, D], F32, kind="ExternalOutput").ap()

    h_d = dscr("h_scr", [S, D], F32)
    uT_d = dscr("uT_scr", [512, S], BF16)
    qT_d = dscr("qT_scr", [8, 70, S], BF16)
    kT_d = dscr("kT_scr", [8, 70, S], BF16)
    ysT_d = dscr("ysT_scr", [512, S], F32)
    ya_d = dscr("ya_scr", [S, 512], BF16) if dbg else None
    uv_d = nc.dram_tensor("uv_scr", [L * NEXP, 2 * D], BF16, kind="Internal").ap()
    idx_dbg = dscr("idx_dbg", [S, 128], I32) if dbg else None
    gate_dbg = dscr("gate_dbg", [S, 128], F32) if dbg else None
    sc_dbg = dscr("sc_dbg", [S, 2048], F32) if dbg else None

    top = ExitStack()
    ident_f = sb(top, "ident_f", [128, 128], F32)
    ident = sb(top, "ident", [128, 128], BF16)
    negm_f = sb(top, "negm_f", [128, 128], F32)
    negm = sb(top, "negm", [128, 128], BF16)
    gmask = sb(top, "gmask", [128, 8], F32)
    iota16 = sb(top, "iota16", [128, 16], F32)
    eps_t = sb(top, "eps_t", [128, 1], F32)
    one_t = sb(top, "one_t", [128, 1], F32)
    ones_bf = sb(top, "ones_bf", [128, 1], BF16)
    nfb = sb(top, "nfb", [128, D], F32)

    P.dma("sp", lambda e: e.dma_start(out=ident_f[:], in_=ident_d), writes=["ident_f"])
    P.dma("sp", lambda e: e.dma_start(out=negm_f[:], in_=negmask_d), writes=["negm_f"])
    P.dma("sp", lambda e: e.dma_start(out=gmask[:], in_=gmask_d), writes=["gmask"])
    P.dma("sp", lambda e: e.dma_start(out=iota16[:], in_=iota16_d), writes=["iota16"])
    P.dma("sp", lambda e: e.dma_start(out=nfb[:], in_=nf_d.to_broadcast([128, D])), writes=["nfb"])
    P.op("act", lambda e: e.activation(out=ident[:], in_=ident_f[:], func=AF.Copy), reads=["ident_f"], writes=["ident"])
    P.op("act", lambda e: e.activation(out=negm[:], in_=negm_f[:], func=AF.Copy), reads=["negm_f"], writes=["negm"])
    P.op("dve", lambda e: e.memset(eps_t[:], EPS), writes=["eps_t"])
    P.op("dve", lambda e: e.memset(one_t[:], 1.0), writes=["one_t"])
    P.op("dve", lambda e: e.memset(ones_bf[:], 1.0), writes=["ones_bf"])

    pb = [nc.alloc_psum_tensor("pb%d" % i, [128, 512], F32) for i in range(7)]
    pT = nc.alloc_psum_tensor("pT", [128, 1024], BF16)

    evac_rr = [0]

    def evac(out_ap, in_ap, reads, writes, scale=None):
        evac_rr[0] ^= 1
        if evac_rr[0]:
            P.op("act", lambda e: e.activation(out=out_ap, in_=in_ap, func=AF.Copy), reads=reads, writes=writes)
        else:
            P.op("dve", lambda e: e.tensor_copy(out=out_ap, in_=in_ap), reads=reads, writes=writes)

    def rstd_from_ss(ss, rs, n, key_ss, key_rs, src_reads=()):
        P.op("act", lambda e: e.activation(out=rs, in_=ss, func=AF.Sqrt, bias=eps_t[:, 0:1], scale=1.0 / n),
             reads=[key_ss, "eps_t"] + list(src_reads), writes=[key_rs])
        P.op("dve", lambda e: e.reciprocal(out=rs, in_=rs), reads=[key_rs], writes=[key_rs])

    def load_w_bf(st, name, dst, src_ap, nchunk, ncols, gainT=None, gkey=None, wst=None):
        for c0 in range(0, ncols, 512):
            c1 = min(ncols, c0 + 512)
            n = c1 - c0
            P.dma("sp", lambda e: e.dma_start(out=wst[:, 0:nchunk, 0:n],
                                              in_=src_ap.rearrange("(c p) n -> p c n", p=128)[:, :, c0:c1]),
                  writes=["wst"])
            for c in range(nchunk):
                eng = "act" if c % 2 == 0 else "pool"
                if gainT is not None:
                    if eng == "act":
                        P.op("act", lambda e: e.activation(out=dst[:, c, c0:c1], in_=wst[:, c, 0:n], func=AF.Copy,
                                                           scale=gainT[:, c:c + 1]),
                             reads=["wst", gkey], writes=[(name, c, c0)])
                    else:
                        P.op("pool", lambda e: e.tensor_scalar(out=dst[:, c, c0:c1], in0=wst[:, c, 0:n],
                                                               scalar1=gainT[:, c:c + 1], scalar2=None, op0=ALU.mult),
                             reads=["wst", gkey], writes=[(name, c, c0)])
                else:
                    if eng == "act":
                        P.op("act", lambda e: e.activation(out=dst[:, c, c0:c1], in_=wst[:, c, 0:n], func=AF.Copy),
                             reads=["wst"], writes=[(name, c, c0)])
                    else:
                        P.op("pool", lambda e: e.tensor_copy(out=dst[:, c, c0:c1], in_=wst[:, c, 0:n]),
                             reads=["wst"], writes=[(name, c, c0)])

    def wkeys(name, nchunk, ncols):
        return [(name, c, c0) for c in range(nchunk) for c0 in range(0, ncols, 512)]

    def p0_gen(l0, cin, cout):
        NCB = len(cin)
        ci = 0
        for tb, src_d in ((0, pu_d), (1, pv_d)):
            for s_ in range(NEXP // 512):
                r0 = l0 * NEXP + s_ * 512
                bi = ci % NCB
                ci += 1
                P.dma("sp", lambda e: e.dma_start(out=cin[bi][:], in_=src_d[r0:r0 + 512, :].rearrange("(p i) d -> p i d", i=4)),
                      writes=["cin%d" % bi])
                if ci % 3 == 0:
                    P.op("pool", lambda e: e.tensor_copy(out=cout[bi][:], in_=cin[bi][:]),
                         reads=["cin%d" % bi], writes=["cout%d" % bi])
                else:
                    P.op("dve", lambda e: e.tensor_copy(out=cout[bi][:], in_=cin[bi][:]),
                         reads=["cin%d" % bi], writes=["cout%d" % bi])
                P.dma("sp", lambda e: e.dma_start(out=uv_d[r0:r0 + 512, tb * D:(tb + 1) * D].rearrange("(p i) d -> p i d", i=4),
                                                  in_=cout[bi][:]), reads=["cout%d" % bi], writes=[("uv_d", l0, tb, s_)])
                yield

    for l in range(L):
        hsrc = x_d if l == 0 else h_d
        va_st = ExitStack()
        Vaug = sb(va_st, "Vaug", [128, NT, NH, 65], BF16)
        P.op("pool", lambda e: e.memset(Vaug[:], 1.0), writes=["Vaug"])
        if "1" in phases:
          with ExitStack() as phA:
            fT = sb(phA, "fT", [8, S], F32)
            with ExitStack() as ph:
                Wbf = sb(ph, "Wbf", [128, 8, 2056], BF16)
                wst = sb(ph, "wst", [128, 8, 512], F32)
                g1T = sb(ph, "g1T", [128, 8], F32)
                hbuf = [sb(ph, "hbuf%d" % i, [128, D], F32) for i in range(2)]
                junk = sb(ph, "junk", [128, D], F32)
                xs = [sb(ph, "xs%d" % i, [128, D], BF16) for i in range(2)]
                xnT = [sb(ph, "xnT%d" % i, [128, 8, 512], BF16) for i in range(2)]
                ust = [sb(ph, "ust%d" % i, [128, 512], BF16) for i in range(3)]
                ss1 = sb(ph, "ss1", [128, 4], F32)
                bfb = sb(ph, "bfb", [8, 1], F32)

                P.dma("sp", lambda e: e.dma_start(out=g1T[:], in_=g1T_d[l]), writes=["g1T"])
                P.dma("sp", lambda e: e.dma_start(out=bfb[:], in_=bf_d[l]), writes=["bfb"])
                load_w_bf(ph, "Wbf", Wbf, w_in_d[l], 8, 2056, gainT=g1T, gkey="g1T", wst=wst)
                WK = wkeys("Wbf", 8, 2056)
                ust_i = [0]
                pbi = [0]

                def nextpb():
                    pbi[0] = (pbi[0] + 1) % 6
                    return pbi[0]

                def pre(tg):
                    xg = xnT[tg % 2]
                    xk = "xnT%d" % (tg % 2)
                    for ti in range(4):
                        t = tg * 4 + ti
                        ht = hbuf[t % 2]
                        hk = "hbuf%d" % (t % 2)
                        xsb = xs[t % 2]
                        xsk = "xs%d" % (t % 2)
                        P.dma("sp", lambda e: e.dma_start(out=ht[:], in_=hsrc[t * 128:(t + 1) * 128, :]), writes=[hk])
                        P.op("act", lambda e: e.activation(out=junk[:], in_=ht[:], func=AF.Square,
                                                           accum_out=ss1[:, 0:1]), reads=[hk], writes=["junk", "ss1a"])
                        rstd_from_ss(ss1[:, 0:1], ss1[:, 1:2], D, "ss1a", "ss1b")
                        P.op("act", lambda e: e.activation(out=xsb[:], in_=ht[:], func=AF.Copy, scale=ss1[:, 1:2]),
                             reads=[hk, "ss1b"], writes=[xsk])
                        for c in range(8):
                            P.op("pe", lambda e: e.transpose(out=pT[:, c * 128:(c + 1) * 128],
                                                             in_=xsb[:, c * 128:(c + 1) * 128], identity=ident[:]),
                                 reads=[xsk, "ident"], writes=[("pT", c)])
                        P.op("dve", lambda e: e.tensor_copy(out=xg[:, :, ti * 128:(ti + 1) * 128],
                                                            in_=pT[:].rearrange("p (c t) -> p c t", c=8)),
                             reads=[("pT", c) for c in range(8)], writes=[(xk, ti)])
                def mm(tg):
                    xg = xnT[tg % 2]
                    xk = "xnT%d" % (tg % 2)
                    XK = [(xk, ti) for ti in range(4)]
                    tsl = slice(tg * 512, (tg + 1) * 512)
                    for m in range(4):
                        b = nextpb()
                        for c in range(8):
                            P.op("pe", lambda e: e.matmul(pb[b][:, :], lhsT=Wbf[:, c, m * 128:(m + 1) * 128],
                                                          rhs=xg[:, c, :], start=(c == 0), stop=(c == 7)),
                                 reads=XK + [("Wbf", c, 0)], writes=[("pb", b)])
                        u = ust_i[0] = (ust_i[0] + 1) % 3
                        evac(ust[u][:, :], pb[b][:, :], [("pb", b)], ["ust%d" % u])
                        P.dma("sp", lambda e: e.dma_start(out=uT_d[m * 128:(m + 1) * 128, tsl], in_=ust[u][:, :]),
                              reads=["ust%d" % u], writes=[("uT_d", m)])
                    for hd in range(NH):
                        for nm, off, dst in (("q", 512, qT_d), ("k", 1024, kT_d)):
                            b = nextpb()
                            c0 = off + 64 * hd
                            for c in range(8):
                                P.op("pe", lambda e: e.matmul(pb[b][0:64, :], lhsT=Wbf[:, c, c0:c0 + 64],
                                                              rhs=xg[:, c, :], start=(c == 0), stop=(c == 7)),
                                     reads=XK + [("Wbf", c, (c0 // 512) * 512)], writes=[("pb", b)])
                            u = ust_i[0] = (ust_i[0] + 1) % 3
                            evac(ust[u][0:64, :], pb[b][0:64, :], [("pb", b)], ["ust%d" % u])
                            P.dma("sp", lambda e: e.dma_start(out=dst[hd, 0:64, tsl], in_=ust[u][0:64, :]),
                                  reads=["ust%d" % u], writes=[(nm + "T_d", hd)])
                    b = nextpb()
                    for c in range(8):
                        P.op("pe", lambda e: e.matmul(pb[b][0:8, :], lhsT=Wbf[:, c, 2048:2056], rhs=xg[:, c, :],
                                                      start=(c == 0), stop=(c == 7)),
                             reads=XK + [("Wbf", c, 2048)], writes=[("pb", b)])
                    P.op("act", lambda e: e.activation(out=fT[:, tsl], in_=pb[b][0:8, :], func=AF.Identity,
                                                       bias=bfb[:, 0:1]), reads=[("pb", b), "bfb"], writes=["fT"])
                    for ti in range(4):
                        t = tg * 4 + ti
                        b = nextpb()
                        for c in range(8):
                            P.op("pe", lambda e: e.matmul(pb[b][:, :], lhsT=xg[:, c, ti * 128:(ti + 1) * 128],
                                                          rhs=Wbf[:, c, 1536:2048], start=(c == 0), stop=(c == 7)),
                                 reads=XK + [("Wbf", c, 1536)], writes=[("pb", b)])
                        evac(Vaug[:, t, :, 0:64], pb[b][:, :].rearrange("p (h d) -> p h d", h=NH),
                             [("pb", b)], [("Vaug", t)])
                pre(0)
                for tg in range(NG):
                    if tg + 1 < NG:
                        pre(tg + 1)
                    mm(tg)
                P.barrier()
            with ExitStack() as ph:
                ft2 = sb(ph, "ft2", [8, S], F32)
                cpb = sb(ph, "cpb", [8, 3, S], BF16)
                cnb = sb(ph, "cnb", [8, 3, S], BF16)
                oneb = sb(ph, "oneb", [8, S], BF16)
                P.op("act", lambda e: e.activation(out=ft2[:], in_=fT[:], func=AF.Exp, scale=-1.0),
                     reads=["fT"], writes=["ft2"])
                P.op("act", lambda e: e.activation(out=ft2[:], in_=ft2[:], func=AF.Ln, bias=one_t[0:8, 0:1]),
                     reads=["ft2", "one_t"], writes=["ft2"])
                P.op("dve", lambda e: e.tensor_scalar(out=ft2[:], in0=ft2[:], scalar1=-8.0, scalar2=None, op0=ALU.mult),
                     reads=["ft2"], writes=["ft2"])
                P.op("dve", lambda e: e.memset(fT[:], 1.0), reads=[], writes=["fT"])
                P.op("dve", lambda e: e.tensor_tensor_scan(ft2[:], fT[:], ft2[:], 0.0, ALU.mult, ALU.add),
                     reads=["fT", "ft2"], writes=["ft2"])
                for i in range(3):
                    P.op("dve", lambda e: e.tensor_copy(out=cpb[:, i, :], in_=ft2[:]), reads=["ft2"], writes=[("cpb", i)])
                    if i < 2:
                        P.op("dve", lambda e: e.tensor_tensor(out=ft2[:], in0=ft2[:], in1=cpb[:, i, :], op=ALU.subtract),
                             reads=["ft2", ("cpb", i)], writes=["ft2"])
                    P.op("dve", lambda e: e.tensor_scalar(out=cnb[:, i, :], in0=cpb[:, i, :], scalar1=-1.0, scalar2=None,
                                                          op0=ALU.mult), reads=[("cpb", i)], writes=[("cnb", i)])
                P.op("dve", lambda e: e.memset(oneb[:], 1.0), writes=["oneb"])
                for i in range(3):
                    P.dma("sp", lambda e: e.dma_start(out=qT_d[:, 64 + i, :], in_=cpb[:, i, :]),
                          reads=[("cpb", i)], writes=[("qT_dc", i)])
                    P.dma("sp", lambda e: e.dma_start(out=qT_d[:, 67 + i, :], in_=oneb[:]),
                          reads=["oneb"], writes=[("qT_dc", 3 + i)])
                    P.dma("sp", lambda e: e.dma_start(out=kT_d[:, 64 + i, :], in_=oneb[:]),
                          reads=["oneb"], writes=[("kT_dc", i)])
                    P.dma("sp", lambda e: e.dma_start(out=kT_d[:, 67 + i, :], in_=cnb[:, i, :]),
                          reads=[("cnb", i)], writes=[("kT_dc", 3 + i)])
                P.barrier()

        if "2" in phases:
            with ExitStack() as ph:
                uTs = sb(ph, "uTs", [128, 4, S], BF16)
                XA = sb(ph, "XA", [128, 2, S], F32)
                XB = sb(ph, "XB", [128, 2, S], F32)
                Sb = sb(ph, "Sb", [128, 2, S], BF16)
                yst = sb(ph, "yst", [32, S], F32)
                LBr = sb(ph, "LBr", [128, 16, 128], BF16)
                LBi = sb(ph, "LBi", [128, 16, 128], BF16)
                Cre = sb(ph, "Cre", [128, 16, 32], BF16)
                Cim = sb(ph, "Cim", [128, 16, 32], BF16)
                Dm = sb(ph, "Dm", [128, 16, 32], BF16)
                cst = sb(ph, "cst", [128, 16, 32], F32)
                Apr = sb(ph, "Apr", [128, LOGS, 16], F32)
                Api = sb(ph, "Api", [128, LOGS, 16], F32)
                Apn = sb(ph, "Apn", [128, LOGS, 16], F32)
                pr = {nm: sb(ph, "pr_" + nm, [128, 64], F32) for nm in
                      ("lr", "li", "br", "bi", "t0", "t1", "t2", "t3", "ar", "ai", "zr", "zi", "bbr", "bbi")}
                dtb = sb(ph, "dtb", [128, 1], F32)
                BL, LBL = 16, 4
                NBK = S // BL
                EA = sb(ph, "EA", [128, 2, NBK], F32)
                EB = sb(ph, "EB", [128, 2, NBK], F32)
                PWr = sb(ph, "PWr", [128, 16, BL], F32)
                PWi = sb(ph, "PWi", [128, 16, BL], F32)
                pwt0 = sb(ph, "pwt0", [128, 16, BL // 2], F32)
                pwt1 = sb(ph, "pwt1", [128, 16, BL // 2], F32)
                pri = sb(ph, "pri", [128, 64], I32)

                P.dma("sp", lambda e: e.dma_start(out=uTs[:], in_=uT_d.rearrange("(c p) t -> p c t", p=128)),
                      writes=["uTs"])
                for nm, src, dst, sc in (("Cre", cT_re_d, Cre, 1.0), ("Cim", cT_im_d, Cim, -1.0), ("Dm", dmat_d, Dm, 1.0)):
                    P.dma("sp", lambda e: e.dma_start(out=cst[:], in_=src[l]), writes=["cst"])
                    P.op("act", lambda e: e.activation(out=dst[:], in_=cst[:], func=AF.Copy, scale=sc),
                         reads=["cst"], writes=[nm])

                def dv(fn, reads, writes):
                    P.op("dve", fn, reads=reads, writes=writes)

                def abar(lr, li, dt_ap, ar, ai, y, fr, tf, ti, sn, k):
                    if dt_ap.shape[-1] == 1:
                        dv(lambda e: e.tensor_scalar(out=y, in0=li, scalar1=dt_ap, scalar2=1.0 / (2 * math.pi),
                                                     op0=ALU.mult, op1=ALU.mult), [k + "li", k + "dt"], [k + "t0"])
                        dv(lambda e: e.tensor_scalar(out=ar, in0=lr, scalar1=dt_ap, scalar2=None, op0=ALU.mult),
                           [k + "lr", k + "dt"], [k + "ar"])
                    else:
                        dv(lambda e: e.tensor_tensor(out=y, in0=li, in1=dt_ap, op=ALU.mult), [k + "li", k + "dt"], [k + "t0"])
                        dv(lambda e: e.tensor_scalar(out=y, in0=y, scalar1=1.0 / (2 * math.pi), scalar2=None,
                                                     op0=ALU.mult), [k + "t0"], [k + "t0"])
                        dv(lambda e: e.tensor_tensor(out=ar, in0=lr, in1=dt_ap, op=ALU.mult), [k + "lr", k + "dt"], [k + "ar"])
                    P.op("act", lambda e: e.activation(out=ar, in_=ar, func=AF.Exp), reads=[k + "ar"], writes=[k + "ar"])
                    for shift, dst, dk in ((0.0, sn, "sn"), (0.25, ai, "ai")):
                        dv(lambda e: e.tensor_scalar(out=fr, in0=y, scalar1=shift, scalar2=None, op0=ALU.add),
                           [k + "t0"], [k + "t1"])
                        dv(lambda e: e.tensor_copy(out=ti, in_=fr), [k + "t1"], [k + "ti"])
                        dv(lambda e: e.tensor_copy(out=tf, in_=ti), [k + "ti"], [k + "t2"])
                        dv(lambda e: e.tensor_tensor(out=fr, in0=fr, in1=tf, op=ALU.subtract), [k + "t1", k + "t2"], [k + "t1"])
                        dv(lambda e: e.tensor_scalar(out=tf, in0=fr, scalar1=0.5, scalar2=None, op0=ALU.is_gt),
                           [k + "t1"], [k + "t2"])
                        dv(lambda e: e.tensor_tensor(out=fr, in0=fr, in1=tf, op=ALU.subtract), [k + "t1", k + "t2"], [k + "t1"])
                        dv(lambda e: e.tensor_scalar(out=tf, in0=fr, scalar1=-0.5, scalar2=None, op0=ALU.is_lt),
                           [k + "t1"], [k + "t2"])
                        dv(lambda e: e.tensor_tensor(out=fr, in0=fr, in1=tf, op=ALU.add), [k + "t1", k + "t2"], [k + "t1"])
                        P.op("act", lambda e: e.activation(out=dst, in_=fr, func=AF.Sin, scale=2 * math.pi),
                             reads=[k + "t1"], writes=[k + dk])
                    dv(lambda e: e.tensor_tensor(out=fr, in0=ai, in1=ar, op=ALU.mult), [k + "ai", k + "ar", k + "t1"], [k + "t1"])
                    dv(lambda e: e.tensor_tensor(out=ai, in0=sn, in1=ar, op=ALU.mult), [k + "t3", k + "ar", k + "ai"], [k + "ai"])
                    dv(lambda e: e.tensor_copy(out=ar, in_=fr), [k + "t1", k + "ar"], [k + "ar"])

                P.op("dve", lambda e: e.memset(LBr[:], 0.0), writes=["LBr"])
                P.op("dve", lambda e: e.memset(LBi[:], 0.0), writes=["LBi"])
                for q in range(4):
                    kq = "B_"
                    P.dma("sp", lambda e: e.dma_start(out=pr["lr"][:], in_=lamB_re_d[l, q]), writes=[kq + "lr"])
                    P.dma("sp", lambda e: e.dma_start(out=pr["li"][:], in_=lamB_im_d[l, q]), writes=[kq + "li"])
                    P.dma("sp", lambda e: e.dma_start(out=pr["br"][:], in_=bT_re_d[l, q]), writes=[kq + "br"])
                    P.dma("sp", lambda e: e.dma_start(out=pr["bi"][:], in_=bT_im_d[l, q]), writes=[kq + "bi"])
                    P.dma("sp", lambda e: e.dma_start(out=dtb[:], in_=dtB_d[l, q]), writes=[kq + "dtraw"])
                    P.op("act", lambda e: e.activation(out=dtb[:], in_=dtb[:], func=AF.Exp), reads=[kq + "dtraw"],
                         writes=[kq + "dt"])
                    abar(pr["lr"][:], pr["li"][:], dtb[:, 0:1], pr["ar"][:], pr["ai"][:], pr["t0"][:], pr["t1"][:],
                         pr["t2"][:], pri[:], pr["t3"][:], kq)
                    lr, li, ar, ai = pr["lr"][:], pr["li"][:], pr["ar"][:], pr["ai"][:]
                    t0, t1, t2, t3 = pr["t0"][:], pr["t1"][:], pr["t2"][:], pr["t3"][:]
                    zr, zi = pr["zr"][:], pr["zi"][:]
                    K = lambda s: kq + s
                    dv(lambda e: e.tensor_tensor(out=t0, in0=lr, in1=lr, op=ALU.mult), [K("lr"), K("t0")], [K("t0")])
                    dv(lambda e: e.tensor_tensor(out=t1, in0=li, in1=li, op=ALU.mult), [K("li"), K("t1")], [K("t1")])
                    dv(lambda e: e.tensor_tensor(out=t0, in0=t0, in1=t1, op=ALU.add), [K("t0"), K("t1")], [K("t0")])
                    dv(lambda e: e.reciprocal(out=t0, in_=t0), [K("t0")], [K("t0")])
                    dv(lambda e: e.tensor_scalar(out=t1, in0=ar, scalar1=-1.0, scalar2=None, op0=ALU.add), [K("ar"), K("t1")], [K("t1")])
                    dv(lambda e: e.tensor_tensor(out=zr, in0=t1, in1=lr, op=ALU.mult), [K("t1"), K("lr")], [K("zr")])
                    dv(lambda e: e.tensor_tensor(out=t2, in0=ai, in1=li, op=ALU.mult), [K("ai"), K("li")], [K("t2")])
                    dv(lambda e: e.tensor_tensor(out=zr, in0=zr, in1=t2, op=ALU.add), [K("zr"), K("t2")], [K("zr")])
                    dv(lambda e: e.tensor_tensor(out=zr, in0=zr, in1=t0, op=ALU.mult), [K("zr"), K("t0")], [K("zr")])
                    dv(lambda e: e.tensor_tensor(out=zi, in0=ai, in1=lr, op=ALU.mult), [K("ai"), K("lr")], [K("zi")])
                    dv(lambda e: e.tensor_tensor(out=t2, in0=t1, in1=li, op=ALU.mult), [K("t1"), K("li")], [K("t2")])
                    dv(lambda e: e.tensor_tensor(out=zi, in0=zi, in1=t2, op=ALU.subtract), [K("zi"), K("t2")], [K("zi")])
                    dv(lambda e: e.tensor_tensor(out=zi, in0=zi, in1=t0, op=ALU.mult), [K("zi"), K("t0")], [K("zi")])
                    br, bi, bbr, bbi = pr["br"][:], pr["bi"][:], pr["bbr"][:], pr["bbi"][:]
                    dv(lambda e: e.tensor_tensor(out=bbr, in0=zr, in1=br, op=ALU.mult), [K("zr"), K("br")], [K("bbr")])
                    dv(lambda e: e.tensor_tensor(out=t2, in0=zi, in1=bi, op=ALU.mult), [K("zi"), K("bi")], [K("t2")])
                    dv(lambda e: e.tensor_tensor(out=bbr, in0=bbr, in1=t2, op=ALU.subtract), [K("bbr"), K("t2")], [K("bbr")])
                    dv(lambda e: e.tensor_tensor(out=bbi, in0=zr, in1=bi, op=ALU.mult), [K("zr"), K("bi")], [K("bbi")])
                    dv(lambda e: e.tensor_tensor(out=t2, in0=zi, in1=br, op=ALU.mult), [K("zi"), K("br")], [K("t2")])
                    dv(lambda e: e.tensor_tensor(out=bbi, in0=bbi, in1=t2, op=ALU.add), [K("bbi"), K("t2")], [K("bbi")])
                    for i in range(4):
                        j = 4 * q + i
                        for g2 in range(2):
                            gi = 2 * i + g2
                            dv(lambda e: e.tensor_scalar(out=LBr[:, j, 64 * g2:64 * g2 + 64], in0=bbr,
                                                         scalar1=gmask[:, gi:gi + 1], scalar2=None, op0=ALU.mult),
                               [K("bbr"), "gmask", "LBr"], [("LBr", j, g2)])
                            dv(lambda e: e.tensor_scalar(out=LBi[:, j, 64 * g2:64 * g2 + 64], in0=bbi,
                                                         scalar1=gmask[:, gi:gi + 1], scalar2=None, op0=ALU.mult),
                               [K("bbi"), "gmask", "LBi"], [("LBi", j, g2)])
                aT = {nm: sb(ph, "aT_" + nm, [128, 16], F32) for nm in ("lr", "li", "dt", "ar", "ai", "t0", "t1", "t2", "t3")}
                aTi = sb(ph, "aTi", [128, 16], I32)
                P.dma("sp", lambda e: e.dma_start(out=aT["lr"][:], in_=lamT_re_d[l]), writes=["T_lr"])
                P.dma("sp", lambda e: e.dma_start(out=aT["li"][:], in_=lamT_im_d[l]), writes=["T_li"])
                P.dma("sp", lambda e: e.dma_start(out=aT["dt"][:], in_=dtT_d[l]), writes=["T_dtraw"])
                P.op("act", lambda e: e.activation(out=aT["dt"][:], in_=aT["dt"][:], func=AF.Exp), reads=["T_dtraw"],
                     writes=["T_dt"])
                abar(aT["lr"][:], aT["li"][:], aT["dt"][:], aT["ar"][:], aT["ai"][:], aT["t0"][:], aT["t1"][:],
                     aT["t2"][:], aTi[:], aT["t3"][:], "T_")
                dv(lambda e: e.tensor_copy(out=Apr[:, 0, :], in_=aT["ar"][:]), ["T_ar"], [("Apr", 0)])
                dv(lambda e: e.tensor_copy(out=Api[:, 0, :], in_=aT["ai"][:]), ["T_ai"], [("Api", 0)])
                for k in range(1, LOGS):
                    a_r, a_i = Apr[:, k - 1, :], Api[:, k - 1, :]
                    t0, t1 = aT["t0"][:], aT["t1"][:]
                    dv(lambda e: e.tensor_tensor(out=t0, in0=a_r, in1=a_r, op=ALU.mult), [("Apr", k - 1), "T_t0"], ["T_t0"])
                    dv(lambda e: e.tensor_tensor(out=t1, in0=a_i, in1=a_i, op=ALU.mult), [("Api", k - 1), "T_t1"], ["T_t1"])
                    dv(lambda e: e.tensor_tensor(out=Apr[:, k, :], in0=t0, in1=t1, op=ALU.subtract), ["T_t0", "T_t1"], [("Apr", k)])
                    dv(lambda e: e.tensor_tensor(out=t0, in0=a_r, in1=a_i, op=ALU.mult), [("Apr", k - 1), ("Api", k - 1), "T_t0"], ["T_t0"])
                    dv(lambda e: e.tensor_scalar(out=Api[:, k, :], in0=t0, scalar1=2.0, scalar2=None, op0=ALU.mult), ["T_t0"], [("Api", k)])
                dv(lambda e: e.tensor_scalar(out=Apn[:], in0=Api[:], scalar1=-1.0, scalar2=None, op0=ALU.mult),
                   [("Api", k) for k in range(LOGS)], ["Apn"])
                APK = [("Apr", k) for k in range(LOGS)] + [("Api", k) for k in range(LOGS)] + ["Apn"]
                dv(lambda e: e.tensor_copy(out=PWr[:, :, 0:1], in_=Apr[:, 0, :].unsqueeze(2)), APK, ["PW"])
                dv(lambda e: e.tensor_copy(out=PWi[:, :, 0:1], in_=Api[:, 0, :].unsqueeze(2)), APK + ["PW"], ["PW"])
                for k in range(LBL):
                    d = 1 << k
                    arb = Apr[:, k, :].unsqueeze(2).to_broadcast([128, 16, d])
                    aib = Api[:, k, :].unsqueeze(2).to_broadcast([128, 16, d])
                    t0, t1 = pwt0[:, :, 0:d], pwt1[:, :, 0:d]
                    dv(lambda e: e.tensor_tensor(out=t0, in0=PWr[:, :, 0:d], in1=arb, op=ALU.mult), APK + ["PW", "pwt0"], ["pwt0"])
                    dv(lambda e: e.tensor_tensor(out=t1, in0=PWi[:, :, 0:d], in1=aib, op=ALU.mult), APK + ["PW", "pwt1"], ["pwt1"])
                    dv(lambda e: e.tensor_tensor(out=PWr[:, :, d:2 * d], in0=t0, in1=t1, op=ALU.subtract), ["pwt0", "pwt1", "PW"], ["PW"])
                    dv(lambda e: e.tensor_tensor(out=t0, in0=PWr[:, :, 0:d], in1=aib, op=ALU.mult), APK + ["PW", "pwt0"], ["pwt0"])
                    dv(lambda e: e.tensor_tensor(out=t1, in0=PWi[:, :, 0:d], in1=arb, op=ALU.mult), APK + ["PW", "pwt1"], ["pwt1"])
                    dv(lambda e: e.tensor_tensor(out=PWi[:, :, d:2 * d], in0=t0, in1=t1, op=ALU.add), ["pwt0", "pwt1", "PW"], ["PW"])

                for j in range(16):
                    q, i = j // 4, j % 4
                    for ri, LB, lk in ((0, LBr, "LBr"), (1, LBi, "LBi")):
                        for n in range(NG):
                            b = 1 + (n + ri) % 4
                            P.op("pe", lambda e: e.matmul(pb[b][:, :], lhsT=LB[:, j, :], rhs=uTs[:, q, n * 512:(n + 1) * 512],
                                                          start=True, stop=True),
                                 reads=["uTs", (lk, j, 0), (lk, j, 1), lk], writes=[("pb", b)])
                            P.op("act", lambda e: e.activation(out=XA[:, ri, n * 512:(n + 1) * 512], in_=pb[b][:, :], func=AF.Copy),
                                 reads=[("pb", b)], writes=[("XA", ri)])
                    cur, curk, oth, othk = XA, "XA", XB, "XB"
                    v4 = lambda T, ri: T[:, ri, :].rearrange("p (b j) -> p b j", j=BL)
                    for k in range(LBL):
                        d = 1 << k
                        new, newk = oth, othk
                        ar_k, ai_k, an_k = Apr[:, k, j:j + 1], Api[:, k, j:j + 1], Apn[:, k, j:j + 1]
                        P.op("dve", lambda e: e.tensor_copy(out=new[:].rearrange("p r (b j) -> p r b j", j=BL)[:, :, :, 0:d],
                                                            in_=cur[:].rearrange("p r (b j) -> p r b j", j=BL)[:, :, :, 0:d]),
                             reads=[(curk, 0), (curk, 1)], writes=[(newk, 0), (newk, 1)])
                        P.op("dve", lambda e: e.scalar_tensor_tensor(out=v4(new, 0)[:, :, d:BL], in0=v4(cur, 0)[:, :, 0:BL - d], scalar=ar_k,
                                                                     in1=v4(cur, 0)[:, :, d:BL], op0=ALU.mult, op1=ALU.add),
                             reads=[(curk, 0)] + APK, writes=[(newk, 0)])
                        P.op("dve", lambda e: e.scalar_tensor_tensor(out=v4(new, 1)[:, :, d:BL], in0=v4(cur, 1)[:, :, 0:BL - d], scalar=ar_k,
                                                                     in1=v4(cur, 1)[:, :, d:BL], op0=ALU.mult, op1=ALU.add),
                             reads=[(curk, 1)] + APK, writes=[(newk, 1)])
                        P.op("dve", lambda e: e.scalar_tensor_tensor(out=v4(new, 0)[:, :, d:BL], in0=v4(cur, 1)[:, :, 0:BL - d], scalar=an_k,
                                                                     in1=v4(new, 0)[:, :, d:BL], op0=ALU.mult, op1=ALU.add),
                             reads=[(curk, 1), (newk, 0)] + APK, writes=[(newk, 0)])
                        P.op("dve", lambda e: e.scalar_tensor_tensor(out=v4(new, 1)[:, :, d:BL], in0=v4(cur, 0)[:, :, 0:BL - d], scalar=ai_k,
                                                                     in1=v4(new, 1)[:, :, d:BL], op0=ALU.mult, op1=ALU.add),
                             reads=[(curk, 0), (newk, 1)] + APK, writes=[(newk, 1)])
                        cur, curk, oth, othk = new, newk, cur, curk
                    ecur, ecurk, eoth, eothk = EA, "EA", EB, "EB"
                    P.op("dve", lambda e: e.tensor_copy(out=ecur[:], in_=cur[:].rearrange("p r (b j) -> p r b j", j=BL)[:, :, :, BL - 1]),
                         reads=[(curk, 0), (curk, 1)], writes=[(ecurk, 0), (ecurk, 1)])
                    for k in range(LOGS - LBL):
                        d = 1 << k
                        kk = LBL + k
                        ar_k, ai_k, an_k = Apr[:, kk, j:j + 1], Api[:, kk, j:j + 1], Apn[:, kk, j:j + 1]
                        P.op("dve", lambda e: e.tensor_copy(out=eoth[:, :, 0:d], in_=ecur[:, :, 0:d]),
                             reads=[(ecurk, 0), (ecurk, 1)], writes=[(eothk, 0), (eothk, 1)])
                        P.op("dve", lambda e: e.scalar_tensor_tensor(out=eoth[:, 0, d:NBK], in0=ecur[:, 0, 0:NBK - d], scalar=ar_k,
                                                                     in1=ecur[:, 0, d:NBK], op0=ALU.mult, op1=ALU.add),
                             reads=[(ecurk, 0)] + APK, writes=[(eothk, 0)])
                        P.op("dve", lambda e: e.scalar_tensor_tensor(out=eoth[:, 1, d:NBK], in0=ecur[:, 1, 0:NBK - d], scalar=ar_k,
                                                                     in1=ecur[:, 1, d:NBK], op0=ALU.mult, op1=ALU.add),
                             reads=[(ecurk, 1)] + APK, writes=[(eothk, 1)])
                        P.op("dve", lambda e: e.scalar_tensor_tensor(out=eoth[:, 0, d:NBK], in0=ecur[:, 1, 0:NBK - d], scalar=an_k,
                                                                     in1=eoth[:, 0, d:NBK], op0=ALU.mult, op1=ALU.add),
                             reads=[(ecurk, 1), (eothk, 0)] + APK, writes=[(eothk, 0)])
                        P.op("dve", lambda e: e.scalar_tensor_tensor(out=eoth[:, 1, d:NBK], in0=ecur[:, 0, 0:NBK - d], scalar=ai_k,
                                                                     in1=eoth[:, 1, d:NBK], op0=ALU.mult, op1=ALU.add),
                             reads=[(ecurk, 0), (eothk, 1)] + APK, writes=[(eothk, 1)])
                        ecur, ecurk, eoth, eothk = eoth, eothk, ecur, ecurk
                    NB1 = NBK - 1
                    bc_pw = lambda T: T[:, j, :].unsqueeze(1).to_broadcast([128, NB1, BL])
                    bc_c = lambda ri: ecur[:, ri, 0:NB1].unsqueeze(2).to_broadcast([128, NB1, BL])
                    blk1 = lambda T, ri: T[:, ri, BL:S].rearrange("p (b j) -> p b j", j=BL)
                    ECK = [(ecurk, 0), (ecurk, 1)]
                    P.op("act", lambda e: e.activation(out=Sb[:, :, 0:BL], in_=cur[:, :, 0:BL], func=AF.Copy),
                         reads=[(curk, 0), (curk, 1)], writes=[("Sb", 0, "h"), ("Sb", 1, "h")])
                    for eng, ri, c1, c2, lastop in (("dve", 0, 0, 1, ALU.subtract), ("pool", 1, 1, 0, ALU.add)):
                        tmp = blk1(oth, ri)
                        P.op(eng, lambda e: e.tensor_tensor(out=tmp, in0=bc_pw(PWr), in1=bc_c(c1), op=ALU.mult),
                             reads=ECK + ["PW"], writes=[(othk, ri)])
                        P.op(eng, lambda e: e.tensor_tensor(out=blk1(cur, ri), in0=blk1(cur, ri), in1=tmp, op=ALU.add),
                             reads=[(othk, ri), (curk, ri)], writes=[(curk, ri)])
                        P.op(eng, lambda e: e.tensor_tensor(out=tmp, in0=bc_pw(PWi), in1=bc_c(c2), op=ALU.mult),
                             reads=ECK + ["PW", (othk, ri)], writes=[(othk, ri)])
                        P.op(eng, lambda e: e.tensor_tensor(out=blk1(Sb, ri), in0=blk1(cur, ri), in1=tmp, op=lastop),
                             reads=[(othk, ri), (curk, ri)], writes=[("Sb", ri)])
                    SK = [("Sb", 0), ("Sb", 1), ("Sb", 0, "h"), ("Sb", 1, "h")]
                    for n in range(NG):
                        b = 5 + n % 2
                        sl = slice(n * 512, (n + 1) * 512)
                        P.op("pe", lambda e: e.matmul(pb[b][0:32, :], lhsT=Cre[:, j, :], rhs=Sb[:, 0, sl], start=True, stop=False),
                             reads=SK + ["Cre"], writes=[("pb", b)])
                        P.op("pe", lambda e: e.matmul(pb[b][0:32, :], lhsT=Cim[:, j, :], rhs=Sb[:, 1, sl], start=False, stop=False),
                             reads=SK + ["Cim"], writes=[("pb", b)])
                        P.op("pe", lambda e: e.matmul(pb[b][0:32, :], lhsT=Dm[:, j, :], rhs=uTs[:, q, sl], start=False, stop=True),
                             reads=["uTs", "Dm"], writes=[("pb", b)])
                        P.op("act", lambda e: e.activation(out=yst[:, sl], in_=pb[b][0:32, :], func=AF.Copy),
                             reads=[("pb", b)], writes=["yst"])
                    P.dma("sp", lambda e: e.dma_start(out=ysT_d[32 * j:32 * j + 32, :], in_=yst[:, :]),
                          reads=["yst"], writes=[("ysT_d", j)])
                P.barrier()

        ya_st = ExitStack()
        yatt = sb(ya_st, "yatt", [128, NT, 512], BF16)
        if "3" in phases:
            with ExitStack() as ph:
                qa = [sb(ph, "qa%d" % i, [70, S], BF16) for i in range(2)]
                ka = [sb(ph, "ka%d" % i, [70, S], BF16) for i in range(2)]
                PTb = [sb(ph, "PT%d" % i, [128, 512], BF16) for i in range(3)]
                rl = sb(ph, "rl", [128, 4], F32)
                oT = sb(ph, "oT", [65, 512], F32)
                cin = [sb(ph, "cin%d" % i, [128, 4, D], F32) for i in range(2)]
                cout = [sb(ph, "cout%d" % i, [128, 4, D], BF16) for i in range(2)]
                p0g = p0_gen(l, cin, cout) if "5" in phases else iter(())
                p0_every = max(1, (NH * NG * (2 * NG + 2)) // 70)
                items = [(hd, G, j) for hd in range(NH) for G in range(NG) for j in range(4 * G + 4)]
                SB_ = [0, 1, 6]

                def st1(n):
                    hd, G, j = items[n]
                    qh, kh = qa[hd % 2], ka[hd % 2]
                    qk_, kk_ = "qa%d" % (hd % 2), "ka%d" % (hd % 2)
                    if G == 0 and j == 0:
                        P.dma("sp", lambda e: e.dma_start(out=qh[:], in_=qT_d[hd]), writes=[qk_])
                        P.dma("sp", lambda e: e.dma_start(out=kh[:], in_=kT_d[hd]), writes=[kk_])
                    i0 = max(0, j - 4 * G)
                    diag = j >= 4 * G
                    c0 = i0 * 128
                    sp_b = SB_[n % 3]
                    PT, ptk = PTb[n % 3], "PT%d" % (n % 3)
                    P.op("pe", lambda e: e.matmul(pb[sp_b][:, c0:512], lhsT=kh[:, j * 128:(j + 1) * 128],
                                                  rhs=qh[:, G * 512 + c0:(G + 1) * 512], start=True, stop=not diag),
                         reads=[qk_, kk_], writes=[("pb", sp_b)])
                    if diag:
                        P.op("pe", lambda e: e.matmul(pb[sp_b][:, c0:c0 + 128], lhsT=ident[:], rhs=negm[:],
                                                      start=False, stop=True),
                             reads=["ident", "negm"], writes=[("pb", sp_b)])
                    P.op("act", lambda e: e.activation(out=PT[:, c0:512], in_=pb[sp_b][:, c0:512], func=AF.Exp,
                                                       scale=0.125), reads=[("pb", sp_b)], writes=[ptk])

                def st2(n):
                    hd, G, j = items[n]
                    i0 = max(0, j - 4 * G)
                    c0 = i0 * 128
                    PT, ptk = PTb[n % 3], "PT%d" % (n % 3)
                    P.op("pe", lambda e: e.matmul(pb[2][0:65, c0:512], lhsT=Vaug[:, j, hd, :], rhs=PT[:, c0:512],
                                                  start=(j == 0), stop=(j == 4 * G + 3)),
                         reads=[ptk, ("Vaug", j), "Vaug"], writes=[("pb", 2)])
                    if j == 4 * G + 3:
                        P.op("act", lambda e: e.activation(out=oT[:, :], in_=pb[2][0:65, :], func=AF.Copy),
                             reads=[("pb", 2)], writes=["oT"])
                        for i in range(4):
                            P.op("pe", lambda e: e.transpose(out=pb[3][:, i * 65:(i + 1) * 65], in_=oT[:, i * 128:(i + 1) * 128],
                                                             identity=ident_f[0:65, 0:65]),
                                 reads=["oT", "ident_f"], writes=[("pb", 3)])
                        o4 = pb[3][:, 0:260].rearrange("p (i d) -> p i d", i=4)
                        P.op("dve", lambda e: e.reciprocal(out=rl[:, :], in_=o4[:, :, 64]),
                             reads=[("pb", 3)], writes=["rl"])
                        for i in range(4):
                            t = 4 * G + i
                            P.op("dve", lambda e: e.tensor_scalar(out=yatt[:, t, hd * 64:(hd + 1) * 64], in0=o4[:, i, 0:64],
                                                                  scalar1=rl[:, i:i + 1], scalar2=None, op0=ALU.mult),
                                 reads=[("pb", 3), "rl"], writes=[("yatt", t, hd)])

                for n in range(len(items)):
                    st1(n)
                    if n >= 1:
                        st2(n - 1)
                    if (n + 1) % p0_every == 0:
                        next(p0g, None)
                st2(len(items) - 1)
                for _ in p0g:
                    pass
                if dbg:
                    for t in range(NT):
                        P.dma("sp", lambda e: e.dma_start(out=ya_d[t * 128:(t + 1) * 128, :], in_=yatt[:, t, :]),
                              reads=[("yatt", t, hd) for hd in range(NH)], writes=[("ya_d", t)])
                P.barrier()

        if "4" in phases:
            with ExitStack() as ph:
                wst = sb(ph, "wst", [128, 8, 512], F32)
                Wg = sb(ph, "Wg", [128, 4, 512], BF16)
                Wo = sb(ph, "Wo", [128, 8, D], BF16)
                gsaT = sb(ph, "gsaT", [128, 8], F32)
                bgl = sb(ph, "bgl", [128, 4], F32)
                ys = sb(ph, "ys", [128, 4, 512], F32)
                gf = sb(ph, "gf", [128, 4, 512], F32)
                gb = sb(ph, "gb", [128, 4, 512], BF16)
                sg = [sb(ph, "sg%d" % i, [128, 512], F32) for i in range(2)]
                ob = sb(ph, "ob", [128, 4, 512], BF16)
                osq = sb(ph, "osq", [128, 4, 512], BF16)
                st4 = sb(ph, "st4", [128, 8], F32)
                junkb = sb(ph, "junkb", [128, 512], BF16)
                yan = sb(ph, "yan", [128, 512], BF16)
                yaT = sb(ph, "yaT", [128, 4, 128], BF16)
                hb4 = [sb(ph, "hb4_%d" % i, [128, D], F32) for i in range(2)]
                P.dma("sp", lambda e: e.dma_start(out=gsaT[:], in_=gsaT_d[l]), writes=["gsaT"])
                P.dma("sp", lambda e: e.dma_start(out=bgl[:], in_=bgluT_d[l]), writes=["bgl"])
                load_w_bf(ph, "Wg", Wg, wglu_d[l], 4, 512, wst=wst)
                load_w_bf(ph, "Wo", Wo, w_o_d[l], 8, D, gainT=gsaT, gkey="gsaT", wst=wst)
                WGK = wkeys("Wg", 4, 512)
                WOK = wkeys("Wo", 8, D)
                for tg in range(NG):
                    tsl = slice(tg * 512, (tg + 1) * 512)
                    P.dma("sp", lambda e: e.dma_start(out=ys[:], in_=ysT_d.rearrange("(c p) t -> p c t", p=128)[:, :, tsl]),
                          writes=["ys"])
                    P.op("act", lambda e: e.activation(out=gf[:], in_=ys[:], func=AF.Gelu_apprx_tanh), reads=["ys"], writes=["gf"])
                    P.op("pool", lambda e: e.tensor_copy(out=gb[:], in_=gf[:]), reads=["gf"], writes=["gb"])
                    for m in range(4):
                        b = m % 2
                        for c in range(4):
                            P.op("pe", lambda e: e.matmul(pb[b][:, :], lhsT=Wg[:, c, m * 128:(m + 1) * 128], rhs=gb[:, c, :],
                                                          start=(c == 0), stop=(c == 3)),
                                 reads=["gb", ("Wg", c, 0)], writes=[("pb", b)])
                        P.op("act", lambda e: e.activation(out=sg[b][:], in_=pb[b][:, :], func=AF.Sigmoid, bias=bgl[:, m:m + 1]),
                             reads=[("pb", b), "bgl"], writes=["sg%d" % b])
                        P.op("dve", lambda e: e.tensor_tensor(out=ob[:, m, :], in0=gf[:, m, :], in1=sg[b][:], op=ALU.mult),
                             reads=["gf", "sg%d" % b], writes=[("ob", m)])
                        P.op("pool", lambda e: e.tensor_tensor(out=osq[:, m, :], in0=ob[:, m, :], in1=ob[:, m, :], op=ALU.mult),
                             reads=[("ob", m)], writes=[("osq", m)])
                    OBK = [("ob", m) for m in range(4)]
                    OSK = [("osq", m) for m in range(4)]
                    for ti in range(4):
                        t = tg * 4 + ti
                        csl = slice(ti * 128, (ti + 1) * 128)
                        hb = hb4[t % 2]
                        hk = "hb4_%d" % (t % 2)
                        P.dma("sp", lambda e: e.dma_start(out=hb[:], in_=hsrc[t * 128:(t + 1) * 128, :]), writes=[hk])
                        for m in range(4):
                            P.op("pe", lambda e: e.matmul(pb[6][:, 0:1], lhsT=osq[:, m, csl], rhs=ones_bf[:, 0:1],
                                                          start=(m == 0), stop=(m == 3)),
                                 reads=OSK + ["ones_bf"], writes=[("pb", 6)])
                        for hf in range(2):
                            for m in range(4):
                                P.op("pe", lambda e: e.matmul(pb[2 + hf][:, :], lhsT=ob[:, m, csl], rhs=Wo[:, m, hf * 512:(hf + 1) * 512],
                                                              start=(m == 0), stop=(m == 3)),
                                     reads=OBK + [("Wo", m, hf * 512)], writes=[("pb", 2 + hf)])
                        rstd_from_ss(pb[6][:, 0:1], st4[:, 0:1], 512, ("pb", 6), "st4a")
                        P.op("act", lambda e: e.activation(out=junkb[:], in_=yatt[:, t, :], func=AF.Square, accum_out=st4[:, 1:2]),
                             reads=[("yatt", t, hd) for hd in range(NH)] + [("yatt", t)], writes=["junkb", "st4b"])
                        rstd_from_ss(st4[:, 1:2], st4[:, 2:3], 512, "st4b", "st4c")
                        P.op("act", lambda e: e.activation(out=yan[:], in_=yatt[:, t, :], func=AF.Copy, scale=st4[:, 2:3]),
                             reads=["st4c", ("yatt", t)], writes=["yan"])
                        for c in range(4):
                            P.op("pe", lambda e: e.transpose(out=pT[:, c * 128:(c + 1) * 128], in_=yan[:, c * 128:(c + 1) * 128],
                                                             identity=ident[:]), reads=["yan", "ident"], writes=[("pT", c)])
                        P.op("dve", lambda e: e.tensor_copy(out=yaT[:], in_=pT[:, 0:512].rearrange("p (c t) -> p c t", c=4)),
                             reads=[("pT", c) for c in range(4)], writes=["yaT"])
                        for hf in range(2):
                            for c in range(4):
                                P.op("pe", lambda e: e.matmul(pb[4 + hf][:, :], lhsT=yaT[:, c, :], rhs=Wo[:, 4 + c, hf * 512:(hf + 1) * 512],
                                                              start=(c == 0), stop=(c == 3)),
                                     reads=["yaT", ("Wo", 4 + c, hf * 512)], writes=[("pb", 4 + hf)])
                        for hf in range(2):
                            hs = slice(hf * 512, (hf + 1) * 512)
                            P.op("dve", lambda e: e.scalar_tensor_tensor(out=hb[:, hs], in0=pb[2 + hf][:, :], scalar=st4[:, 0:1],
                                                                         in1=hb[:, hs], op0=ALU.mult, op1=ALU.add),
                                 reads=[("pb", 2 + hf), "st4a", hk], writes=[hk])
                            P.op("dve", lambda e: e.tensor_tensor(out=hb[:, hs], in0=pb[4 + hf][:, :], in1=hb[:, hs], op=ALU.add),
                                 reads=[("pb", 4 + hf), hk], writes=[hk])
                        P.dma("sp", lambda e: e.dma_start(out=h_d[t * 128:(t + 1) * 128, :], in_=hb[:]), reads=[hk],
                              writes=[("h_d", t)])
                P.barrier()
            hsrc = h_d

        ya_st.close()
        va_st.close()
        if "5" in phases:
            final = last_final and (l == L - 1)
            with ExitStack() as ph:
                Wq = sb(ph, "Wq", [128, 8, D], BF16)
                kTb = sb(ph, "kTb", [128, 16, 128], BF16)
                g2b = sb(ph, "g2b", [128, D], F32)
                hb5 = [sb(ph, "hb5_%d" % i, [128, D], F32) for i in range(2)]
                hng = [sb(ph, "hng%d" % i, [128, D], F32) for i in range(1)]
                junkb = sb(ph, "junkb5", [128, D], BF16)
                hngb = [sb(ph, "hngb%d" % i, [128, D], BF16) for i in range(2)]
                NZ = 4
                zb = [sb(ph, "zb%d" % i, [128, D], BF16) for i in range(NZ)]
                hT = sb(ph, "hT", [128, 8, 128], BF16)
                qTt = sb(ph, "qTt", [128, 8, 128], BF16)
                sc = sb(ph, "sc", [128, 16, 128], F32)
                wk = sb(ph, "wk", [128, 256], F32)
                tv = sb(ph, "tv", [128, 16, 16], F32)
                tix = sb(ph, "tix", [128, 16, 16], U32)
                tif = sb(ph, "tif", [128, 16, 16], F32)
                cand = sb(ph, "cand", [128, 8, 256], F32)
                best = sb(ph, "best", [128, 8, 16], F32)
                pos = sb(ph, "pos", [128, 8, 16], U32)
                pa = sb(ph, "pa", [128, 8, 16], I32)
                pbb = sb(ph, "pbb", [128, 8, 16], I32)
                paf = sb(ph, "paf", [128, 8, 16], F32)
                pbf = sb(ph, "pbf", [128, 8, 16], F32)
                oh = sb(ph, "oh", [128, 8, 16, 16], F32)
                sel1 = sb(ph, "sel1", [128, 8, 16], F32)
                sel2 = sb(ph, "sel2", [128, 8, 16], F32)
                idxf = sb(ph, "idxf", [128, 128], F32)
                idx = [sb(ph, "idx%d" % i, [128, 128], I32) for i in range(2)]
                gate = [sb(ph, "gate%d" % i, [128, 8, 16], F32) for i in range(2)]
                gsum = sb(ph, "gsum", [128, 8], F32)
                act_ = [sb(ph, "act%d" % i, [128, 128], F32) for i in range(2)]
                gl_ = [sb(ph, "gl%d" % i, [128, 128], F32) for i in range(2)]
                coef = [sb(ph, "coef%d" % i, [128, 128], F32) for i in range(2)]
                outb = [sb(ph, "outb%d" % i, [128, D], F32) for i in range(1)]
                st5 = sb(ph, "st5", [128, 8], F32)
                st6 = sb(ph, "st6", [128, 8], F32)
                NB, KB, NDG = 24, 4, 8
                P.dma("sp", lambda e: e.dma_start(out=g2b[:], in_=g2_d[l].to_broadcast([128, D])), writes=["g2b"])
                with ExitStack() as wph:
                    kst = sb(wph, "kst", [128, 16, 128], F32)
                    P.dma("sp", lambda e: e.dma_start(out=kst[:], in_=keysT_d[l]), writes=["kst"])
                    P.op("act", lambda e: e.activation(out=kTb[:], in_=kst[:], func=AF.Copy), reads=["kst"], writes=["kTb"])
                    wst = sb(wph, "wst", [128, 8, 512], F32)
                    load_w_bf(wph, "Wq", Wq, w_q_d[l], 8, D, wst=wst)
                    P.barrier()
                uvb = [sb(ph, "uvb%d" % i, [128, 2 * D], BF16) for i in range(NB)]
                dg = [sb(ph, "dg%d" % i, [128, 128], BF16) for i in range(NDG)]

                def front_end(t):
                    p2 = t % 2
                    hb, hk = hb5[p2], "hb5_%d" % p2
                    hg, hgk = hng[0], "hng0"
                    P.dma("sp", lambda e: e.dma_start(out=hb[:], in_=hsrc[t * 128:(t + 1) * 128, :]), reads=[("h_d", t)], writes=[hk])
                    P.op("act", lambda e: e.activation(out=junkb[:], in_=hb[:], func=AF.Square, accum_out=st5[:, 0:1]),
                         reads=[hk], writes=["junkb5", "st5a"])
                    yield
                    yield
                    rstd_from_ss(st5[:, 0:1], st5[:, 1:2], D, "st5a", "st5b")
                    P.op("dve", lambda e: e.scalar_tensor_tensor(out=hg[:], in0=hb[:], scalar=st5[:, 1:2], in1=g2b[:],
                                                                 op0=ALU.mult, op1=ALU.mult),
                         reads=[hk, "st5b", "g2b"], writes=[hgk])
                    yield
                    xs5, xs5k = hngb[p2], "hngb%d" % p2
                    P.op("act", lambda e: e.activation(out=xs5[:], in_=hg[:], func=AF.Copy), reads=[hgk], writes=[xs5k])
                    yield
                    yield
                    for c in range(8):
                        P.op("pe", lambda e: e.transpose(out=pT[:, c * 128:(c + 1) * 128], in_=xs5[:, c * 128:(c + 1) * 128],
                                                         identity=ident[:]), reads=[xs5k, "ident"], writes=[("pT", c)])
                    P.op("dve", lambda e: e.tensor_copy(out=hT[:], in_=pT[:].rearrange("p (c t) -> p c t", c=8)),
                         reads=[("pT", c) for c in range(8)], writes=["hT"])
                    yield
                    yield
                    for hd in range(NH):
                        b = hd // 4
                        for c in range(8):
                            P.op("pe", lambda e: e.matmul(pb[b][:, (hd % 4) * 128:(hd % 4 + 1) * 128],
                                                          lhsT=Wq[:, c, hd * 128:(hd + 1) * 128], rhs=hT[:, c, :],
                                                          start=(c == 0), stop=(c == 7)),
                                 reads=["hT", ("Wq", c, (hd // 4) * 512)], writes=[("pb", b, hd % 4)])
                        yield
                    for b in range(2):
                        evac(qTt[:, 4 * b:4 * b + 4, :], pb[b][:, :].rearrange("p (h t) -> p h t", h=4),
                             [("pb", b, i) for i in range(4)], [("qTt", b)])
                    yield
                    for half8 in range(2):
                        for bl in range(8):
                            blk = half8 * 8 + bl
                            hd = blk // 2
                            b = 2 + bl // 4
                            P.op("pe", lambda e: e.matmul(pb[b][:, (bl % 4) * 128:(bl % 4 + 1) * 128],
                                                          lhsT=qTt[:, hd, :], rhs=kTb[:, blk, :], start=True, stop=True),
                                 reads=[("qTt", hd // 4), "kTb"], writes=[("pb", b, bl % 4)])
                        for b in range(2):
                            g4 = half8 * 2 + b
                            evac(sc[:, 4 * g4:4 * g4 + 4, :], pb[2 + b][:, :].rearrange("p (h t) -> p h t", h=4),
                                 [("pb", 2 + b, i) for i in range(4)], [("sc", g4)])
                        yield
                    for blk in range(16):
                        sk = ("sc", blk // 4)
                        P.op("dve", lambda e: e.max(out=tv[:, blk, 0:8], in_=sc[:, blk, :]), reads=[sk], writes=[("tv", blk, 0)])
                        yield
                        P.op("dve", lambda e: e.match_replace(out=wk[:, 0:128], in_to_replace=tv[:, blk, 0:8],
                                                              in_values=sc[:, blk, :], imm_value=-1e30),
                             reads=[sk, ("tv", blk, 0)], writes=["wk"])
                        yield
                        P.op("dve", lambda e: e.max(out=tv[:, blk, 8:16], in_=wk[:, 0:128]), reads=["wk"], writes=[("tv", blk, 1)])
                        yield
                        P.op("dve", lambda e: e.max_index(out=tix[:, blk, 0:8], in_max=tv[:, blk, 0:8], in_values=sc[:, blk, :]),
                             reads=[sk, ("tv", blk, 0)], writes=[("tix", blk, 0)])
                        yield
                        P.op("dve", lambda e: e.max_index(out=tix[:, blk, 8:16], in_max=tv[:, blk, 8:16], in_values=sc[:, blk, :]),
                             reads=[sk, ("tv", blk, 1)], writes=[("tix", blk, 1)])
                        yield
                        yield
                    TVK = [("tv", b, i) for b in range(16) for i in range(2)]
                    TIK = [("tix", b, i) for b in range(16) for i in range(2)]
                    P.op("dve", lambda e: e.tensor_copy(out=tif[:], in_=tix[:]), reads=TIK, writes=["tif"])
                    yield
                    tv4 = tv[:].rearrange("p (h j) k -> p h j k", j=2)
                    tif4 = tif[:].rearrange("p (h j) k -> p h j k", j=2)
                    for hd in range(NH):
                        P.op("dve", lambda e: e.tensor_tensor(out=cand[:, hd, :].rearrange("p (a b) -> p a b", a=16),
                                                              in0=tv4[:, hd, 0, :].unsqueeze(2).to_broadcast([128, 16, 16]),
                                                              in1=tv4[:, hd, 1:2, :].to_broadcast([128, 16, 16]), op=ALU.add),
                             reads=TVK, writes=[("cand", hd)])
                        yield
                        P.op("dve", lambda e: e.max(out=best[:, hd, 0:8], in_=cand[:, hd, :]), reads=[("cand", hd)], writes=[("best", hd, 0)])
                        yield
                        P.op("dve", lambda e: e.match_replace(out=wk[:, :], in_to_replace=best[:, hd, 0:8], in_values=cand[:, hd, :],
                                                              imm_value=-1e30), reads=[("cand", hd), ("best", hd, 0)], writes=["wk"])
                        yield
                        P.op("dve", lambda e: e.max(out=best[:, hd, 8:16], in_=wk[:, :]), reads=["wk"], writes=[("best", hd, 1)])
                        yield
                        P.op("dve", lambda e: e.max_index(out=pos[:, hd, 0:8], in_max=best[:, hd, 0:8], in_values=cand[:, hd, :]),
                             reads=[("cand", hd), ("best", hd, 0)], writes=[("pos", hd, 0)])
                        yield
                        P.op("dve", lambda e: e.max_index(out=pos[:, hd, 8:16], in_max=best[:, hd, 8:16], in_values=cand[:, hd, :]),
                             reads=[("cand", hd), ("best", hd, 1)], writes=[("pos", hd, 1)])
                        yield
                        yield
                    BK = [("best", h_, i) for h_ in range(NH) for i in range(2)]
                    PK = [("pos", h_, i) for h_ in range(NH) for i in range(2)]
                    posi = pos[:].bitcast(I32)
                    P.op("dve", lambda e: e.tensor_scalar(out=pa[:], in0=posi, scalar1=4, scalar2=None, op0=ALU.arith_shift_right),
                         reads=PK, writes=["pa"])
                    yield
                    P.op("dve", lambda e: e.tensor_scalar(out=pbb[:], in0=posi, scalar1=15, scalar2=None, op0=ALU.bitwise_and),
                         reads=PK, writes=["pbb"])
                    yield
                    P.op("dve", lambda e: e.tensor_copy(out=paf[:], in_=pa[:]), reads=["pa"], writes=["paf"])
                    yield
                    P.op("dve", lambda e: e.tensor_copy(out=pbf[:], in_=pbb[:]), reads=["pbb"], writes=["pbf"])
                    yield
                    yield
                    io4 = iota16[:].unsqueeze(1).unsqueeze(1).to_broadcast([128, 8, 16, 16])
                    for pf, pfk, half, sel, selk in ((paf, "paf", 0, sel1, "sel1"), (pbf, "pbf", 1, sel2, "sel2")):
                        P.op("dve", lambda e: e.tensor_tensor(out=oh[:], in0=pf[:].unsqueeze(3).to_broadcast([128, 8, 16, 16]),
                                                              in1=io4, op=ALU.is_equal), reads=[pfk, "iota16"], writes=["oh"])
                        yield
                        P.op("dve", lambda e: e.tensor_tensor(out=oh[:], in0=oh[:],
                                                              in1=tif4[:, :, half, :].unsqueeze(2).to_broadcast([128, 8, 16, 16]),
                                                              op=ALU.mult), reads=["oh", "tif"], writes=["oh"])
                        yield
                        P.op("dve", lambda e: e.tensor_reduce(out=sel[:], in_=oh[:], axis=AX.X, op=ALU.add), reads=["oh"], writes=[selk])
                        yield
                        yield
                    ix, ixk = idx[p2], "idx%d" % p2
                    P.op("dve", lambda e: e.scalar_tensor_tensor(out=idxf[:], in0=sel1[:].rearrange("p h k -> p (h k)"), scalar=128.0,
                                                                 in1=sel2[:].rearrange("p h k -> p (h k)"), op0=ALU.mult, op1=ALU.add),
                         reads=["sel1", "sel2"], writes=["idxf"])
                    yield
                    if l > 0:
                        P.op("dve", lambda e: e.tensor_scalar(out=idxf[:], in0=idxf[:], scalar1=float(l * NEXP), scalar2=None,
                                                              op0=ALU.add), reads=["idxf"], writes=["idxf"])
                        yield
                    P.op("dve", lambda e: e.tensor_copy(out=ix[:], in_=idxf[:]), reads=["idxf"], writes=[ixk])
                    yield
                    yield
                    gt, gtk = gate[p2], "gate%d" % p2
                    P.op("dve", lambda e: e.tensor_tensor(out=gt[:], in0=best[:], in1=best[:, :, 0:1].to_broadcast([128, 8, 16]),
                                                          op=ALU.subtract), reads=BK, writes=[gtk])
                    yield
                    P.op("act", lambda e: e.activation(out=gt[:], in_=gt[:], func=AF.Exp), reads=[gtk], writes=[gtk])
                    yield
                    P.op("dve", lambda e: e.tensor_reduce(out=gsum[:], in_=gt[:], axis=AX.X, op=ALU.add), reads=[gtk], writes=["gsum"])
                    yield
                    P.op("dve", lambda e: e.reciprocal(out=gsum[:], in_=gsum[:]), reads=["gsum"], writes=["gsum"])
                    yield
                    P.op("dve", lambda e: e.tensor_tensor(out=gt[:], in0=gt[:], in1=gsum[:].unsqueeze(2).to_broadcast([128, 8, 16]),
                                                          op=ALU.mult), reads=[gtk, "gsum"], writes=[gtk])
                    yield
                    if dbg:
                        P.dma("sp", lambda e: e.dma_start(out=idx_dbg[t * 128:(t + 1) * 128, :], in_=ix[:]), reads=[ixk], writes=[("idbg", t)])
                        P.dma("sp", lambda e: e.dma_start(out=gate_dbg[t * 128:(t + 1) * 128, :], in_=gt[:].rearrange("p h k -> p (h k)")),
                              reads=[gtk], writes=[("gdbg", t)])
                        P.dma("sp", lambda e: e.dma_start(out=sc_dbg[t * 128:(t + 1) * 128, :], in_=sc[:].rearrange("p h k -> p (h k)")),
                              reads=[("sc", b_) for b_ in range(4)], writes=[("sdbg", t)])
                    yield

                cnt5 = {"u": 0, "d": 0, "z": 0}

                def k_loop(t, fe_next):
                    p2 = t % 2
                    hb, hk = hb5[p2], "hb5_%d" % p2
                    hg, hgk = hng[0], "hng0"
                    hgb, hgbk = hngb[p2], "hngb%d" % p2
                    ix, ixk = idx[p2], "idx%d" % p2
                    gt, gtk = gate[p2], "gate%d" % p2
                    gtf = gt[:].rearrange("p h k -> p (h k)")
                    av, avk = act_[p2], "act%d" % p2
                    gl, glk = gl_[p2], "gl%d" % p2
                    cf, cfk = coef[p2], "coef%d" % p2
                    NBT = 128 // KB
                    bufs = {}

                    def stA(n):
                        bufs[n] = []
                        for k in range(n * KB, (n + 1) * KB):
                            u_ = cnt5["u"] % NB
                            cnt5["u"] += 1
                            bufs[n].append(u_)
                            P.dma("pool", lambda e: e.indirect_dma_start(out=uvb[u_][:], out_offset=None, in_=uv_d,
                                                                         in_offset=bass.IndirectOffsetOnAxis(ap=ix[:, k:k + 1], axis=0)),
                                  reads=[ixk], writes=["uvb%d" % u_])

                    def stB(n):
                        k0 = n * KB
                        for i, k in enumerate(range(k0, k0 + KB)):
                            u_ = bufs[n][i]
                            z_ = cnt5["z"] % NZ
                            cnt5["z"] += 1
                            P.op("dve", lambda e: e.tensor_tensor(out=zb[z_][:], in0=uvb[u_][:, 0:D], in1=hgb[:], op=ALU.mult),
                                 reads=["uvb%d" % u_, hgbk], writes=["zb%d" % z_])
                            P.op("act", lambda e: e.activation(out=zb[z_][:], in_=zb[z_][:], func=AF.Copy, accum_out=av[:, k:k + 1]),
                                 reads=["zb%d" % z_], writes=["zb%d" % z_, (avk, k0, i)])
                        ks = slice(k0, k0 + KB)
                        P.op("act", lambda e: e.activation(out=gl[:, ks], in_=av[:, ks], func=AF.Gelu_apprx_tanh),
                             reads=[(avk, k0, i) for i in range(KB)], writes=[(glk, k0)])

                    def stC(n):
                        k0 = n * KB
                        ks = slice(k0, k0 + KB)
                        P.op("dve", lambda e: e.tensor_tensor(out=cf[:, ks], in0=gl[:, ks], in1=gtf[:, ks], op=ALU.mult),
                             reads=[(glk, k0), gtk], writes=[(cfk, k0)])
                        for i, k in enumerate(range(k0, k0 + KB)):
                            u_ = bufs[n][i]
                            d_ = cnt5["d"] % NDG
                            cnt5["d"] += 1
                            P.op("dve", lambda e: e.tensor_scalar(out=dg[d_][:], in0=ident[:], scalar1=cf[:, k:k + 1], scalar2=None,
                                                                  op0=ALU.mult), reads=["ident", (cfk, k0)], writes=["dg%d" % d_])
                            for hf in range(2):
                                P.op("pe", lambda e: e.matmul(pb[4 + hf][:, :], lhsT=dg[d_][:], rhs=uvb[u_][:, D + hf * 512:D + (hf + 1) * 512],
                                                              start=(k == 0), stop=(k == 127)),
                                     reads=["dg%d" % d_, "uvb%d" % u_], writes=[("pb", 4 + hf)])

                    LA = 3
                    for n in range(min(LA, NBT)):
                        stA(n)
                    for n in range(NBT):
                        if n + LA < NBT:
                            stA(n + LA)
                        stB(n)
                        if n >= 1:
                            stC(n - 1)
                        for _ in range(7):
                            next(fe_next, None)
                    stC(NBT - 1)
                    for _ in fe_next:
                        pass
                    ob, obk = outb[0], "outb0"
                    for hf in range(2):
                        hs = slice(hf * 512, (hf + 1) * 512)
                        P.op("dve", lambda e: e.tensor_tensor(out=ob[:, hs], in0=pb[4 + hf][:, :], in1=hb[:, hs], op=ALU.add),
                             reads=[("pb", 4 + hf), hk], writes=[(obk, hf)])
                    OBK2 = [(obk, 0), (obk, 1)]
                    if final:
                        P.op("act", lambda e: e.activation(out=junkb[:], in_=ob[:], func=AF.Square, accum_out=st6[:, 0:1]),
                             reads=OBK2, writes=["junkb5", "st6a"])
                        rstd_from_ss(st6[:, 0:1], st6[:, 1:2], D, "st6a", "st6b")
                        P.op("dve", lambda e: e.scalar_tensor_tensor(out=ob[:], in0=ob[:], scalar=st6[:, 1:2], in1=nfb[:],
                                                                     op0=ALU.mult, op1=ALU.mult),
                             reads=OBK2 + ["st6b", "nfb"], writes=OBK2)
                        P.dma("sp", lambda e: e.dma_start(out=out_d[t * 128:(t + 1) * 128, :], in_=ob[:]), reads=OBK2,
                              writes=[("out_d", t)])
                    else:
                        P.dma("sp", lambda e: e.dma_start(out=h_d[t * 128:(t + 1) * 128, :], in_=ob[:]), reads=OBK2,
                              writes=[("h_d", t)])

                for _ in front_end(0):
                    pass
                for t in range(NT):
                    fe_next = front_end(t + 1) if t + 1 < NT else iter(())
                    if "x" in phases:
                        for _ in fe_next:
                            pass
                        continue
                    k_loop(t, fe_next)
                P.barrier()
            hsrc = h_d
    P.barrier()
    top.close()
    return nc, P


def _consts():
    ident = np.eye(128, dtype=np.float32)
    k = np.arange(128)
    negmask = np.where(k[:, None] > k[None, :], -30000.0, 0.0).astype(np.float32)
    gmask = (k[:, None] // 16 == np.arange(8)[None, :]).astype(np.float32)
    iota16 = np.broadcast_to(np.arange(16, dtype=np.float32), (128, 16)).copy()
    return {"ident": ident, "negmask": negmask, "gmask": gmask, "iota16": iota16}


def layout_weights(inp, L):
    f = lambda a: np.ascontiguousarray(np.asarray(a, dtype=np.float32))
    w = {}
    w["g1T"] = f(inp["norm1_g"].reshape(L, 8, 128).transpose(0, 2, 1))
    w["w_in"] = f(inp["w_in"])
    w["b_f"] = f(inp["fox_b_f"].reshape(L, 8, 1))
    lam_re, lam_im = np.asarray(inp["ssm_lambda_re"]), np.asarray(inp["ssm_lambda_im"])
    ldt = np.asarray(inp["ssm_log_dt"])
    rep = lambda a: f(np.repeat(a[:, :, None, :], 16, axis=2).reshape(L, 4, 128, 64))
    w["lamB_re"] = rep(lam_re)
    w["lamB_im"] = rep(lam_im)
    w["dtB"] = f(np.repeat(ldt[:, :, None], 16, axis=2).reshape(L, 4, 128, 1))
    w["bT_re"] = f(np.asarray(inp["ssm_b_re"]).transpose(0, 1, 3, 2).reshape(L, 4, 128, 64))
    w["bT_im"] = f(np.asarray(inp["ssm_b_im"]).transpose(0, 1, 3, 2).reshape(L, 4, 128, 64))
    tl = lambda a: f(a.reshape(L, 16, 2, 64).transpose(0, 2, 3, 1).reshape(L, 128, 16))
    w["lamT_re"] = tl(lam_re)
    w["lamT_im"] = tl(lam_im)
    w["dtT"] = tl(np.repeat(ldt[:, :, None], 64, axis=2))
    def cblk(c):
        c = np.asarray(c).reshape(L, 16, 2, 16, 64)
        o = np.zeros((L, 2, 64, 16, 2, 16), np.float32)
        for g2 in range(2):
            o[:, g2, :, :, g2, :] = c[:, :, g2].transpose(0, 3, 1, 2)
        return f(o.reshape(L, 128, 16, 32))
    w["cT_re"] = cblk(inp["ssm_c_re"])
    w["cT_im"] = cblk(inp["ssm_c_im"])
    d = np.asarray(inp["ssm_d"]).reshape(L, 4, 4, 32)
    dm = np.zeros((L, 4, 32, 4, 4, 32), np.float32)
    for i in range(4):
        for m in range(32):
            dm[:, i, m, :, i, m] = d[:, :, i, m]
    w["dmat"] = f(dm.reshape(L, 128, 16, 32))
    w["w_glu"] = f(inp["ssm_w_glu"])
    w["bgluT"] = f(np.asarray(inp["ssm_b_glu"]).reshape(L, 4, 128).transpose(0, 2, 1))
    gsa = np.concatenate([np.asarray(inp["g_ssm_out"]), np.asarray(inp["g_attn_out"])], axis=1)
    w["gsaT"] = f(gsa.reshape(L, 8, 128).transpose(0, 2, 1))
    w["w_o"] = f(inp["w_o"])
    w["g2"] = f(np.asarray(inp["norm2_g"]).reshape(L, 1, D))
    w["w_q"] = f(inp["peer_w_q"])
    kk = np.asarray(inp["peer_keys"]).transpose(0, 2, 4, 1, 3)
    kz = np.zeros((L, 2, 64, 8, 2, 128), np.float32)
    for j in range(2):
        kz[:, j, :, :, j, :] = kk[:, j]
    w["keysT"] = f(kz.reshape(L, 128, 16, 128))
    w["peer_u"] = f(np.asarray(inp["peer_u"]).reshape(L * NEXP, D))
    w["peer_v"] = f(np.asarray(inp["peer_v"]).reshape(L * NEXP, D))
    w["norm_f"] = f(np.asarray(inp["norm_f"]).reshape(1, D))
    w.update(_consts())
    return w


def kernel(**inputs):
    x = np.asarray(inputs["x"], dtype=np.float32)
    B, S, _ = x.shape
    L = int(np.asarray(inputs["w_in"]).shape[0])
    w = layout_weights(inputs, L)
    nc, _ = build(L, S)
    in_maps = []
    for b in range(B):
        m = dict(w)
        m["x"] = np.ascontiguousarray(x[b])
        in_maps.append(m)
    res = run_bass_kernel_spmd(nc, in_maps, core_ids=list(range(B)))
    return np.stack([np.asarray(r["out"], dtype=np.float32) for r in res.results], axis=0)
```

```python
import math
from contextlib import ExitStack
import numpy as np
import concourse.bass as bass
import concourse.mybir as mybir
from concourse.bass_utils import run_bass_kernel_spmd

F32 = mybir.dt.float32
BF16 = mybir.dt.bfloat16
I32 = mybir.dt.int32
U32 = mybir.dt.uint32
ALU = mybir.AluOpType
AF = mybir.ActivationFunctionType
AX = mybir.AxisListType

D = 1024
NH = 8
EPS = 1e-6
NEXP = 16384


class Prog:
    NDMA = 24

    def __init__(self, nc):
        self.nc = nc
        self.eng = {"pe": nc.tensor, "act": nc.scalar, "dve": nc.vector,
                    "pool": nc.gpsimd, "sp": nc.sync}
        self.sem = {k: nc.alloc_semaphore("sem_" + k) for k in self.eng}
        self.cnt = {k: 0 for k in self.eng}
        self.dsem = [nc.alloc_semaphore("dsem%d" % i) for i in range(2 * self.NDMA)]
        self.dval = [0] * (2 * self.NDMA)
        self.dnext = {"sp": 0, "pool": 0}
        self.seen = {k: {} for k in self.eng}
        self.lastw = {}
        self.readers = {}
        self.ninst = 0

    def _wait(self, e, tok):
        sem, val, uid = tok
        if e == "pe" and uid == "epe":
            return
        if self.seen[e].get(uid, 0) >= val:
            return
        self.seen[e][uid] = val
        self.eng[e].wait_ge(sem, val)
        self.ninst += 1

    def _deps(self, e, reads, writes):
        for k in reads:
            t = self.lastw.get(k)
            if t is not None:
                self._wait(e, t)
        for k in writes:
            t = self.lastw.get(k)
            if t is not None:
                self._wait(e, t)
            for t in self.readers.get(k, ()):
                self._wait(e, t)

    def _commit(self, tok, reads, writes):
        for k in reads:
            self.readers.setdefault(k, []).append(tok)
        for k in writes:
            self.lastw[k] = tok
            self.readers[k] = []
        self.ninst += 1

    def op(self, e, fn, reads=(), writes=()):
        self._deps(e, reads, writes)
        inst = fn(self.eng[e])
        self.cnt[e] += 1
        inst.then_inc(self.sem[e], 1)
        tok = (self.sem[e], self.cnt[e], "e" + e)
        self._commit(tok, reads, writes)
        return tok

    def dma(self, e, fn, reads=(), writes=()):
        k = self.dnext[e] + (self.NDMA if e == "pool" else 0)
        self.dnext[e] = (self.dnext[e] + 1) % self.NDMA
        if self.dval[k] > 0:
            self._wait(e, (self.dsem[k], self.dval[k], "d%d" % k))
        self._deps(e, reads, writes)
        inst = fn(self.eng[e])
        self.dval[k] += 16
        inst.then_inc(self.dsem[k], 16)
        tok = (self.dsem[k], self.dval[k], "d%d" % k)
        self._commit(tok, reads, writes)
        return tok

    def barrier(self):
        for e in self.eng:
            for o in self.eng:
                if self.cnt[o] > 0:
                    self._wait(e, (self.sem[o], self.cnt[o], "e" + o))
            for k in range(2 * self.NDMA):
                if self.dval[k] > 0:
                    self._wait(e, (self.dsem[k], self.dval[k], "d%d" % k))
        self.lastw = {}
        self.readers = {}


def build(L, S, dbg=False, phases="12345", last_final=True, stop=99):
    nc = bass.Bass("TRN2", target_bir_lowering=False)
    P = Prog(nc)
    NT, NG = S // 128, S // 512
    LOGS = int(math.log2(S))
    assert 1 << LOGS == S and NG >= 1

    def din(name, shape, dt=F32):
        return nc.dram_tensor(name, list(shape), dt, kind="ExternalInput").ap()

    def dscr(name, shape, dt):
        return nc.dram_tensor(name, list(shape), dt, kind=("ExternalOutput" if dbg else "Internal")).ap()

    uniq = [0]

    def sb(st, name, shape, dt):
        uniq[0] += 1
        return st.enter_context(nc.sbuf_tensor("s%d_%s" % (uniq[0], name), list(shape), dt))

    x_d = din("x", [S, D])
    g1T_d = din("g1T", [L, 128, 8])
    w_in_d = din("w_in", [L, D, 2056])
    bf_d = din("b_f", [L, 8, 1])
    lamB_re_d = din("lamB_re", [L, 4, 128, 64])
    lamB_im_d = din("lamB_im", [L, 4, 128, 64])
    dtB_d = din("dtB", [L, 4, 128, 1])
    bT_re_d = din("bT_re", [L, 4, 128, 64])
    bT_im_d = din("bT_im", [L, 4, 128, 64])
    lamT_re_d = din("lamT_re", [L, 128, 16])
    lamT_im_d = din("lamT_im", [L, 128, 16])
    dtT_d = din("dtT", [L, 128, 16])
    cT_re_d = din("cT_re", [L, 128, 16, 32])
    cT_im_d = din("cT_im", [L, 128, 16, 32])
    dmat_d = din("dmat", [L, 128, 16, 32])
    wglu_d = din("w_glu", [L, 512, 512])
    bgluT_d = din("bgluT", [L, 128, 4])
    gsaT_d = din("gsaT", [L, 128, 8])
    w_o_d = din("w_o", [L, D, D])
    g2_d = din("g2", [L, 1, D])
    w_q_d = din("w_q", [L, D, D])
    keysT_d = din("keysT", [L, 128, 16, 128])
    pu_d = din("peer_u", [L * NEXP, D])
    pv_d = din("peer_v", [L * NEXP, D])
    nf_d = din("norm_f", [1, D])
    ident_d = din("ident", [128, 128])
    negmask_d = din("negmask", [128, 128])
    gmask_d = din("gmask", [128, 8])
    iota16_d = din("iota16", [128, 16])
    out_d = nc.dram_tensor("out", [S, D], F32, kind="ExternalOutput").ap()

    h_d = dscr("h_scr", [S, D], F32)
    uT_d = dscr("uT_scr", [512, S], BF16)
    qT_d = dscr("qT_scr", [8, 70, S], BF16)
    kT_d = dscr("kT_scr", [8, 70, S], BF16)
    ysT_d = dscr("ysT_scr", [512, S], F32)
    ya_d = dscr("ya_scr", [S, 512], BF16) if dbg else None
    uv_d = nc.dram_tensor("uv_scr", [L * NEXP, 2 * D], BF16, kind="Internal").ap()
    idx_dbg = dscr("idx_dbg", [S, 128], I32) if dbg else None
    gate_dbg = dscr("gate_dbg", [S, 128], F32) if dbg else None
    sc_dbg = dscr("sc_dbg", [S, 2048], F32) if dbg else None

    top = ExitStack()
    ident_f = sb(top, "ident_f", [128, 128], F32)
    ident = sb(top, "ident", [128, 128], BF16)
    negm_f = sb(top, "negm_f", [128, 128], F32)
    negm = sb(top, "negm", [128, 128], BF16)
    gmask = sb(top, "gmask", [128, 8], F32)
    iota16 = sb(top, "iota16", [128, 16], F32)
    eps_t = sb(top, "eps_t", [128, 1], F32)
    one_t = sb(top, "one_t", [128, 1], F32)
    ones_bf = sb(top, "ones_bf", [128, 1], BF16)
    nfb = sb(top, "nfb", [128, D], F32)

    P.dma("sp", lambda e: e.dma_start(out=ident_f[:], in_=ident_d), writes=["ident_f"])
    P.dma("sp", lambda e: e.dma_start(out=negm_f[:], in_=negmask_d), writes=["negm_f"])
    P.dma("sp", lambda e: e.dma_start(out=gmask[:], in_=gmask_d), writes=["gmask"])
    P.dma("sp", lambda e: e.dma_start(out=iota16[:], in_=iota16_d), writes=["iota16"])
    P.dma("sp", lambda e: e.dma_start(out=nfb[:], in_=nf_d.to_broadcast([128, D])), writes=["nfb"])
    P.op("act", lambda e: e.activation(out=ident[:], in_=ident_f[:], func=AF.Copy), reads=["ident_f"], writes=["ident"])
    P.op("act", lambda e: e.activation(out=negm[:], in_=negm_f[:], func=AF.Copy), reads=["negm_f"], writes=["negm"])
    P.op("dve", lambda e: e.memset(eps_t[:], EPS), writes=["eps_t"])
    P.op("dve", lambda e: e.memset(one_t[:], 1.0), writes=["one_t"])
    P.op("dve", lambda e: e.memset(ones_bf[:], 1.0), writes=["ones_bf"])

    pb = [nc.alloc_psum_tensor("pb%d" % i, [128, 512], F32) for i in range(7)]
    pT = nc.alloc_psum_tensor("pT", [128, 1024], BF16)

    evac_rr = [0]

    def evac(out_ap, in_ap, reads, writes, scale=None):
        evac_rr[0] ^= 1
        if evac_rr[0]:
            P.op("act", lambda e: e.activation(out=out_ap, in_=in_ap, func=AF.Copy), reads=reads, writes=writes)
        else:
            P.op("dve", lambda e: e.tensor_copy(out=out_ap, in_=in_ap), reads=reads, writes=writes)

    def rstd_from_ss(ss, rs, n, key_ss, key_rs, src_reads=()):
        P.op("act", lambda e: e.activation(out=rs, in_=ss, func=AF.Sqrt, bias=eps_t[:, 0:1], scale=1.0 / n),
             reads=[key_ss, "eps_t"] + list(src_reads), writes=[key_rs])
        P.op("dve", lambda e: e.reciprocal(out=rs, in_=rs), reads=[key_rs], writes=[key_rs])

    def load_w_bf(st, name, dst, src_ap, nchunk, ncols, gainT=None, gkey=None, wst=None):
        for c0 in range(0, ncols, 512):
            c1 = min(ncols, c0 + 512)
            n = c1 - c0
            P.dma("sp", lambda e: e.dma_start(out=wst[:, 0:nchunk, 0:n],
                                              in_=src_ap.rearrange("(c p) n -> p c n", p=128)[:, :, c0:c1]),
                  writes=["wst"])
            for c in range(nchunk):
                eng = "act" if c % 2 == 0 else "pool"
                if gainT is not None:
                    if eng == "act":
                        P.op("act", lambda e: e.activation(out=dst[:, c, c0:c1], in_=wst[:, c, 0:n], func=AF.Copy,
                                                           scale=gainT[:, c:c + 1]),
                             reads=["wst", gkey], writes=[(name, c, c0)])
                    else:
                        P.op("pool", lambda e: e.tensor_scalar(out=dst[:, c, c0:c1], in0=wst[:, c, 0:n],
                                                               scalar1=gainT[:, c:c + 1], scalar2=None, op0=ALU.mult),
                             reads=["wst", gkey], writes=[(name, c, c0)])
                else:
                    if eng == "act":
                        P.op("act", lambda e: e.activation(out=dst[:, c, c0:c1], in_=wst[:, c, 0:n], func=AF.Copy),
                             reads=["wst"], writes=[(name, c, c0)])
                    else:
                        P.op("pool", lambda e: e.tensor_copy(out=dst[:, c, c0:c1], in_=wst[:, c, 0:n]),
                             reads=["wst"], writes=[(name, c, c0)])

    def wkeys(name, nchunk, ncols):
        return [(name, c, c0) for c in range(nchunk) for c0 in range(0, ncols, 512)]

    def p0_gen(l0, cin, cout):
        NCB = len(cin)
        ci = 0
        for tb, src_d in ((0, pu_d), (1, pv_d)):
            for s_ in range(NEXP // 512):
                r0 = l0 * NEXP + s_ * 512
                bi = ci % NCB
                ci += 1
                P.dma("sp", lambda e: e.dma_start(out=cin[bi][:], in_=src_d[r0:r0 + 512, :].rearrange("(p i) d -> p i d", i=4)),
                      writes=["cin%d" % bi])
                if ci % 2 == 0:
                    P.op("act", lambda e: e.activation(out=cout[bi][:], in_=cin[bi][:], func=AF.Copy),
                         reads=["cin%d" % bi], writes=["cout%d" % bi])
                else:
                    P.op("dve", lambda e: e.tensor_copy(out=cout[bi][:], in_=cin[bi][:]),
                         reads=["cin%d" % bi], writes=["cout%d" % bi])
                P.dma("sp", lambda e: e.dma_start(out=uv_d[r0:r0 + 512, tb * D:(tb + 1) * D].rearrange("(p i) d -> p i d", i=4),
                                                  in_=cout[bi][:]), reads=["cout%d" % bi], writes=[("uv_d", l0, tb, s_)])
                yield

    for l in range(L):
        hsrc = x_d if l == 0 else h_d
        va_st = ExitStack()
        Vaug = sb(va_st, "Vaug", [128, NT, NH, 65], BF16)
        P.op("pool", lambda e: e.memset(Vaug[:], 1.0), writes=["Vaug"])
        if "1" in phases:
          with ExitStack() as phA:
            fT = sb(phA, "fT", [8, S], F32)
            with ExitStack() as ph:
                Wbf = sb(ph, "Wbf", [128, 8, 2056], BF16)
                wst = sb(ph, "wst", [128, 8, 512], F32)
                g1T = sb(ph, "g1T", [128, 8], F32)
                hbuf = [sb(ph, "hbuf%d" % i, [128, D], F32) for i in range(2)]
                junk = sb(ph, "junk", [128, D], F32)
                xs = [sb(ph, "xs%d" % i, [128, D], BF16) for i in range(2)]
                xnT = [sb(ph, "xnT%d" % i, [128, 8, 512], BF16) for i in range(2)]
                ust = [sb(ph, "ust%d" % i, [128, 512], BF16) for i in range(3)]
                ss1 = sb(ph, "ss1", [128, 4], F32)
                bfb = sb(ph, "bfb", [8, 1], F32)

                P.dma("sp", lambda e: e.dma_start(out=g1T[:], in_=g1T_d[l]), writes=["g1T"])
                P.dma("sp", lambda e: e.dma_start(out=bfb[:], in_=bf_d[l]), writes=["bfb"])
                load_w_bf(ph, "Wbf", Wbf, w_in_d[l], 8, 2056, gainT=g1T, gkey="g1T", wst=wst)
                WK = wkeys("Wbf", 8, 2056)
                ust_i = [0]
                pbi = [0]

                def nextpb():
                    pbi[0] = (pbi[0] + 1) % 6
                    return pbi[0]

                def pre(tg):
                    xg = xnT[tg % 2]
                    xk = "xnT%d" % (tg % 2)
                    for ti in range(4):
                        t = tg * 4 + ti
                        ht = hbuf[t % 2]
                        hk = "hbuf%d" % (t % 2)
                        xsb = xs[t % 2]
                        xsk = "xs%d" % (t % 2)
                        P.dma("sp", lambda e: e.dma_start(out=ht[:], in_=hsrc[t * 128:(t + 1) * 128, :]), writes=[hk])
                        P.op("act", lambda e: e.activation(out=junk[:], in_=ht[:], func=AF.Square,
                                                           accum_out=ss1[:, 0:1]), reads=[hk], writes=["junk", "ss1a"])
                        rstd_from_ss(ss1[:, 0:1], ss1[:, 1:2], D, "ss1a", "ss1b")
                        P.op("act", lambda e: e.activation(out=xsb[:], in_=ht[:], func=AF.Copy, scale=ss1[:, 1:2]),
                             reads=[hk, "ss1b"], writes=[xsk])
                        for c in range(8):
                            P.op("pe", lambda e: e.transpose(out=pT[:, c * 128:(c + 1) * 128],
                                                             in_=xsb[:, c * 128:(c + 1) * 128], identity=ident[:]),
                                 reads=[xsk, "ident"], writes=[("pT", c)])
                        P.op("dve", lambda e: e.tensor_copy(out=xg[:, :, ti * 128:(ti + 1) * 128],
                                                            in_=pT[:].rearrange("p (c t) -> p c t", c=8)),
                             reads=[("pT", c) for c in range(8)], writes=[(xk, ti)])
                def mm(tg):
                    xg = xnT[tg % 2]
                    xk = "xnT%d" % (tg % 2)
                    XK = [(xk, ti) for ti in range(4)]
                    tsl = slice(tg * 512, (tg + 1) * 512)
                    for m in range(4):
                        b = nextpb()
                        for c in range(8):
                            P.op("pe", lambda e: e.matmul(pb[b][:, :], lhsT=Wbf[:, c, m * 128:(m + 1) * 128],
                                                          rhs=xg[:, c, :], start=(c == 0), stop=(c == 7)),
                                 reads=XK + [("Wbf", c, 0)], writes=[("pb", b)])
                        u = ust_i[0] = (ust_i[0] + 1) % 3
                        evac(ust[u][:, :], pb[b][:, :], [("pb", b)], ["ust%d" % u])
                        P.dma("sp", lambda e: e.dma_start(out=uT_d[m * 128:(m + 1) * 128, tsl], in_=ust[u][:, :]),
                              reads=["ust%d" % u], writes=[("uT_d", m)])
                    for hd in range(NH):
                        for nm, off, dst in (("q", 512, qT_d), ("k", 1024, kT_d)):
                            b = nextpb()
                            c0 = off + 64 * hd
                            for c in range(8):
                                P.op("pe", lambda e: e.matmul(pb[b][0:64, :], lhsT=Wbf[:, c, c0:c0 + 64],
                                                              rhs=xg[:, c, :], start=(c == 0), stop=(c == 7)),
                                     reads=XK + [("Wbf", c, (c0 // 512) * 512)], writes=[("pb", b)])
                            u = ust_i[0] = (ust_i[0] + 1) % 3
                            evac(ust[u][0:64, :], pb[b][0:64, :], [("pb", b)], ["ust%d" % u])
                            P.dma("sp", lambda e: e.dma_start(out=dst[hd, 0:64, tsl], in_=ust[u][0:64, :]),
                                  reads=["ust%d" % u], writes=[(nm + "T_d", hd)])
                    b = nextpb()
                    for c in range(8):
                        P.op("pe", lambda e: e.matmul(pb[b][0:8, :], lhsT=Wbf[:, c, 2048:2056], rhs=xg[:, c, :],
                                                      start=(c == 0), stop=(c == 7)),
                             reads=XK + [("Wbf", c, 2048)], writes=[("pb", b)])
                    P.op("act", lambda e: e.activation(out=fT[:, tsl], in_=pb[b][0:8, :], func=AF.Identity,
                                                       bias=bfb[:, 0:1]), reads=[("pb", b), "bfb"], writes=["fT"])
                    for ti in range(4):
                        t = tg * 4 + ti
                        b = nextpb()
                        for c in range(8):
                            P.op("pe", lambda e: e.matmul(pb[b][:, :], lhsT=xg[:, c, ti * 128:(ti + 1) * 128],
                                                          rhs=Wbf[:, c, 1536:2048], start=(c == 0), stop=(c == 7)),
                                 reads=XK + [("Wbf", c, 1536)], writes=[("pb", b)])
                        evac(Vaug[:, t, :, 0:64], pb[b][:, :].rearrange("p (h d) -> p h d", h=NH),
                             [("pb", b)], [("Vaug", t)])
                pre(0)
                for tg in range(NG):
                    if tg + 1 < NG:
                        pre(tg + 1)
                    mm(tg)
                P.barrier()
            with ExitStack() as ph:
                ft2 = sb(ph, "ft2", [8, S], F32)
                cpb = sb(ph, "cpb", [8, 3, S], BF16)
                cnb = sb(ph, "cnb", [8, 3, S], BF16)
                oneb = sb(ph, "oneb", [8, S], BF16)
                P.op("act", lambda e: e.activation(out=ft2[:], in_=fT[:], func=AF.Exp, scale=-1.0),
                     reads=["fT"], writes=["ft2"])
                P.op("act", lambda e: e.activation(out=ft2[:], in_=ft2[:], func=AF.Ln, bias=one_t[0:8, 0:1]),
                     reads=["ft2", "one_t"], writes=["ft2"])
                P.op("dve", lambda e: e.tensor_scalar(out=ft2[:], in0=ft2[:], scalar1=-8.0, scalar2=None, op0=ALU.mult),
                     reads=["ft2"], writes=["ft2"])
                P.op("dve", lambda e: e.memset(fT[:], 1.0), reads=[], writes=["fT"])
                P.op("dve", lambda e: e.tensor_tensor_scan(ft2[:], fT[:], ft2[:], 0.0, ALU.mult, ALU.add),
                     reads=["fT", "ft2"], writes=["ft2"])
                for i in range(3):
                    P.op("dve", lambda e: e.tensor_copy(out=cpb[:, i, :], in_=ft2[:]), reads=["ft2"], writes=[("cpb", i)])
                    if i < 2:
                        P.op("dve", lambda e: e.tensor_tensor(out=ft2[:], in0=ft2[:], in1=cpb[:, i, :], op=ALU.subtract),
                             reads=["ft2", ("cpb", i)], writes=["ft2"])
                    P.op("dve", lambda e: e.tensor_scalar(out=cnb[:, i, :], in0=cpb[:, i, :], scalar1=-1.0, scalar2=None,
                                                          op0=ALU.mult), reads=[("cpb", i)], writes=[("cnb", i)])
                P.op("dve", lambda e: e.memset(oneb[:], 1.0), writes=["oneb"])
                for i in range(3):
                    P.dma("sp", lambda e: e.dma_start(out=qT_d[:, 64 + i, :], in_=cpb[:, i, :]),
                          reads=[("cpb", i)], writes=[("qT_dc", i)])
                    P.dma("sp", lambda e: e.dma_start(out=qT_d[:, 67 + i, :], in_=oneb[:]),
                          reads=["oneb"], writes=[("qT_dc", 3 + i)])
                    P.dma("sp", lambda e: e.dma_start(out=kT_d[:, 64 + i, :], in_=oneb[:]),
                          reads=["oneb"], writes=[("kT_dc", i)])
                    P.dma("sp", lambda e: e.dma_start(out=kT_d[:, 67 + i, :], in_=cnb[:, i, :]),
                          reads=[("cnb", i)], writes=[("kT_dc", 3 + i)])
                P.barrier()

        if "2" in phases:
            with ExitStack() as ph:
                uTs = sb(ph, "uTs", [128, 4, S], BF16)
                XA = sb(ph, "XA", [128, 2, S], F32)
                XB = sb(ph, "XB", [128, 2, S], F32)
                Sb = sb(ph, "Sb", [128, 2, S], BF16)
                yst = sb(ph, "yst", [32, S], F32)
                LBr = sb(ph, "LBr", [128, 16, 128], BF16)
                LBi = sb(ph, "LBi", [128, 16, 128], BF16)
                Cre = sb(ph, "Cre", [128, 16, 32], BF16)
                Cim = sb(ph, "Cim", [128, 16, 32], BF16)
                Dm = sb(ph, "Dm", [128, 16, 32], BF16)
                cst = sb(ph, "cst", [128, 16, 32], F32)
                Apr = sb(ph, "Apr", [128, LOGS, 16], F32)
                Api = sb(ph, "Api", [128, LOGS, 16], F32)
                Apn = sb(ph, "Apn", [128, LOGS, 16], F32)
                pr = {nm: sb(ph, "pr_" + nm, [128, 64], F32) for nm in
                      ("lr", "li", "br", "bi", "t0", "t1", "t2", "t3", "ar", "ai", "zr", "zi", "bbr", "bbi")}
                dtb = sb(ph, "dtb", [128, 1], F32)
                BL, LBL = 16, 4
                NBK = S // BL
                EA = sb(ph, "EA", [128, 2, NBK], F32)
                EB = sb(ph, "EB", [128, 2, NBK], F32)
                PWr = sb(ph, "PWr", [128, 16, BL], F32)
                PWi = sb(ph, "PWi", [128, 16, BL], F32)
                pwt0 = sb(ph, "pwt0", [128, 16, BL // 2], F32)
                pwt1 = sb(ph, "pwt1", [128, 16, BL // 2], F32)
                pri = sb(ph, "pri", [128, 64], I32)

                P.dma("sp", lambda e: e.dma_start(out=uTs[:], in_=uT_d.rearrange("(c p) t -> p c t", p=128)),
                      writes=["uTs"])
                for nm, src, dst, sc in (("Cre", cT_re_d, Cre, 1.0), ("Cim", cT_im_d, Cim, -1.0), ("Dm", dmat_d, Dm, 1.0)):
                    P.dma("sp", lambda e: e.dma_start(out=cst[:], in_=src[l]), writes=["cst"])
                    P.op("act", lambda e: e.activation(out=dst[:], in_=cst[:], func=AF.Copy, scale=sc),
                         reads=["cst"], writes=[nm])

                def dv(fn, reads, writes):
                    P.op("dve", fn, reads=reads, writes=writes)

                def abar(lr, li, dt_ap, ar, ai, y, fr, tf, ti, sn, k):
                    if dt_ap.shape[-1] == 1:
                        dv(lambda e: e.tensor_scalar(out=y, in0=li, scalar1=dt_ap, scalar2=1.0 / (2 * math.pi),
                                                     op0=ALU.mult, op1=ALU.mult), [k + "li", k + "dt"], [k + "t0"])
                        dv(lambda e: e.tensor_scalar(out=ar, in0=lr, scalar1=dt_ap, scalar2=None, op0=ALU.mult),
                           [k + "lr", k + "dt"], [k + "ar"])
                    else:
                        dv(lambda e: e.tensor_tensor(out=y, in0=li, in1=dt_ap, op=ALU.mult), [k + "li", k + "dt"], [k + "t0"])
                        dv(lambda e: e.tensor_scalar(out=y, in0=y, scalar1=1.0 / (2 * math.pi), scalar2=None,
                                                     op0=ALU.mult), [k + "t0"], [k + "t0"])
                        dv(lambda e: e.tensor_tensor(out=ar, in0=lr, in1=dt_ap, op=ALU.mult), [k + "lr", k + "dt"], [k + "ar"])
                    P.op("act", lambda e: e.activation(out=ar, in_=ar, func=AF.Exp), reads=[k + "ar"], writes=[k + "ar"])
                    for shift, dst, dk in ((0.0, sn, "sn"), (0.25, ai, "ai")):
                        dv(lambda e: e.tensor_scalar(out=fr, in0=y, scalar1=shift, scalar2=None, op0=ALU.add),
                           [k + "t0"], [k + "t1"])
                        dv(lambda e: e.tensor_copy(out=ti, in_=fr), [k + "t1"], [k + "ti"])
                        dv(lambda e: e.tensor_copy(out=tf, in_=ti), [k + "ti"], [k + "t2"])
                        dv(lambda e: e.tensor_tensor(out=fr, in0=fr, in1=tf, op=ALU.subtract), [k + "t1", k + "t2"], [k + "t1"])
                        dv(lambda e: e.tensor_scalar(out=tf, in0=fr, scalar1=0.5, scalar2=None, op0=ALU.is_gt),
                           [k + "t1"], [k + "t2"])
                        dv(lambda e: e.tensor_tensor(out=fr, in0=fr, in1=tf, op=ALU.subtract), [k + "t1", k + "t2"], [k + "t1"])
                        dv(lambda e: e.tensor_scalar(out=tf, in0=fr, scalar1=-0.5, scalar2=None, op0=ALU.is_lt),
                           [k + "t1"], [k + "t2"])
                        dv(lambda e: e.tensor_tensor(out=fr, in0=fr, in1=tf, op=ALU.add), [k + "t1", k + "t2"], [k + "t1"])
                        P.op("act", lambda e: e.activation(out=dst, in_=fr, func=AF.Sin, scale=2 * math.pi),
                             reads=[k + "t1"], writes=[k + dk])
                    dv(lambda e: e.tensor_tensor(out=fr, in0=ai, in1=ar, op=ALU.mult), [k + "ai", k + "ar", k + "t1"], [k + "t1"])
                    dv(lambda e: e.tensor_tensor(out=ai, in0=sn, in1=ar, op=ALU.mult), [k + "t3", k + "ar", k + "ai"], [k + "ai"])
                    dv(lambda e: e.tensor_copy(out=ar, in_=fr), [k + "t1", k + "ar"], [k + "ar"])

                P.op("dve", lambda e: e.memset(LBr[:], 0.0), writes=["LBr"])
                P.op("dve", lambda e: e.memset(LBi[:], 0.0), writes=["LBi"])
                for q in range(4):
                    kq = "B_"
                    P.dma("sp", lambda e: e.dma_start(out=pr["lr"][:], in_=lamB_re_d[l, q]), writes=[kq + "lr"])
                    P.dma("sp", lambda e: e.dma_start(out=pr["li"][:], in_=lamB_im_d[l, q]), writes=[kq + "li"])
                    P.dma("sp", lambda e: e.dma_start(out=pr["br"][:], in_=bT_re_d[l, q]), writes=[kq + "br"])
                    P.dma("sp", lambda e: e.dma_start(out=pr["bi"][:], in_=bT_im_d[l, q]), writes=[kq + "bi"])
                    P.dma("sp", lambda e: e.dma_start(out=dtb[:], in_=dtB_d[l, q]), writes=[kq + "dtraw"])
                    P.op("act", lambda e: e.activation(out=dtb[:], in_=dtb[:], func=AF.Exp), reads=[kq + "dtraw"],
                         writes=[kq + "dt"])
                    abar(pr["lr"][:], pr["li"][:], dtb[:, 0:1], pr["ar"][:], pr["ai"][:], pr["t0"][:], pr["t1"][:],
                         pr["t2"][:], pri[:], pr["t3"][:], kq)
                    lr, li, ar, ai = pr["lr"][:], pr["li"][:], pr["ar"][:], pr["ai"][:]
                    t0, t1, t2, t3 = pr["t0"][:], pr["t1"][:], pr["t2"][:], pr["t3"][:]
                    zr, zi = pr["zr"][:], pr["zi"][:]
                    K = lambda s: kq + s
                    dv(lambda e: e.tensor_tensor(out=t0, in0=lr, in1=lr, op=ALU.mult), [K("lr"), K("t0")], [K("t0")])
                    dv(lambda e: e.tensor_tensor(out=t1, in0=li, in1=li, op=ALU.mult), [K("li"), K("t1")], [K("t1")])
                    dv(lambda e: e.tensor_tensor(out=t0, in0=t0, in1=t1, op=ALU.add), [K("t0"), K("t1")], [K("t0")])
                    dv(lambda e: e.reciprocal(out=t0, in_=t0), [K("t0")], [K("t0")])
                    dv(lambda e: e.tensor_scalar(out=t1, in0=ar, scalar1=-1.0, scalar2=None, op0=ALU.add), [K("ar"), K("t1")], [K("t1")])
                    dv(lambda e: e.tensor_tensor(out=zr, in0=t1, in1=lr, op=ALU.mult), [K("t1"), K("lr")], [K("zr")])
                    dv(lambda e: e.tensor_tensor(out=t2, in0=ai, in1=li, op=ALU.mult), [K("ai"), K("li")], [K("t2")])
                    dv(lambda e: e.tensor_tensor(out=zr, in0=zr, in1=t2, op=ALU.add), [K("zr"), K("t2")], [K("zr")])
                    dv(lambda e: e.tensor_tensor(out=zr, in0=zr, in1=t0, op=ALU.mult), [K("zr"), K("t0")], [K("zr")])
                    dv(lambda e: e.tensor_tensor(out=zi, in0=ai, in1=lr, op=ALU.mult), [K("ai"), K("lr")], [K("zi")])
                    dv(lambda e: e.tensor_tensor(out=t2, in0=t1, in1=li, op=ALU.mult), [K("t1"), K("li")], [K("t2")])
                    dv(lambda e: e.tensor_tensor(out=zi, in0=zi, in1=t2, op=ALU.subtract), [K("zi"), K("t2")], [K("zi")])
                    dv(lambda e: e.tensor_tensor(out=zi, in0=zi, in1=t0, op=ALU.mult), [K("zi"), K("t0")], [K("zi")])
                    br, bi, bbr, bbi = pr["br"][:], pr["bi"][:], pr["bbr"][:], pr["bbi"][:]
                    dv(lambda e: e.tensor_tensor(out=bbr, in0=zr, in1=br, op=ALU.mult), [K("zr"), K("br")], [K("bbr")])
                    dv(lambda e: e.tensor_tensor(out=t2, in0=zi, in1=bi, op=ALU.mult), [K("zi"), K("bi")], [K("t2")])
                    dv(lambda e: e.tensor_tensor(out=bbr, in0=bbr, in1=t2, op=ALU.subtract), [K("bbr"), K("t2")], [K("bbr")])
                    dv(lambda e: e.tensor_tensor(out=bbi, in0=zr, in1=bi, op=ALU.mult), [K("zr"), K("bi")], [K("bbi")])
                    dv(lambda e: e.tensor_tensor(out=t2, in0=zi, in1=br, op=ALU.mult), [K("zi"), K("br")], [K("t2")])
                    dv(lambda e: e.tensor_tensor(out=bbi, in0=bbi, in1=t2, op=ALU.add), [K("bbi"), K("t2")], [K("bbi")])
                    for i in range(4):
                        j = 4 * q + i
                        for g2 in range(2):
                            gi = 2 * i + g2
                            dv(lambda e: e.tensor_scalar(out=LBr[:, j, 64 * g2:64 * g2 + 64], in0=bbr,
                                                         scalar1=gmask[:, gi:gi + 1], scalar2=None, op0=ALU.mult),
                               [K("bbr"), "gmask", "LBr"], [("LBr", j, g2)])
                            dv(lambda e: e.tensor_scalar(out=LBi[:, j, 64 * g2:64 * g2 + 64], in0=bbi,
                                                         scalar1=gmask[:, gi:gi + 1], scalar2=None, op0=ALU.mult),
                               [K("bbi"), "gmask", "LBi"], [("LBi", j, g2)])
                aT = {nm: sb(ph, "aT_" + nm, [128, 16], F32) for nm in ("lr", "li", "dt", "ar", "ai", "t0", "t1", "t2", "t3")}
                aTi = sb(ph, "aTi", [128, 16], I32)
                P.dma("sp", lambda e: e.dma_start(out=aT["lr"][:], in_=lamT_re_d[l]), writes=["T_lr"])
                P.dma("sp", lambda e: e.dma_start(out=aT["li"][:], in_=lamT_im_d[l]), writes=["T_li"])
                P.dma("sp", lambda e: e.dma_start(out=aT["dt"][:], in_=dtT_d[l]), writes=["T_dtraw"])
                P.op("act", lambda e: e.activation(out=aT["dt"][:], in_=aT["dt"][:], func=AF.Exp), reads=["T_dtraw"],
                     writes=["T_dt"])
                abar(aT["lr"][:], aT["li"][:], aT["dt"][:], aT["ar"][:], aT["ai"][:], aT["t0"][:], aT["t1"][:],
                     aT["t2"][:], aTi[:], aT["t3"][:], "T_")
                dv(lambda e: e.tensor_copy(out=Apr[:, 0, :], in_=aT["ar"][:]), ["T_ar"], [("Apr", 0)])
                dv(lambda e: e.tensor_copy(out=Api[:, 0, :], in_=aT["ai"][:]), ["T_ai"], [("Api", 0)])
                for k in range(1, LOGS):
                    a_r, a_i = Apr[:, k - 1, :], Api[:, k - 1, :]
                    t0, t1 = aT["t0"][:], aT["t1"][:]
                    dv(lambda e: e.tensor_tensor(out=t0, in0=a_r, in1=a_r, op=ALU.mult), [("Apr", k - 1), "T_t0"], ["T_t0"])
                    dv(lambda e: e.tensor_tensor(out=t1, in0=a_i, in1=a_i, op=ALU.mult), [("Api", k - 1), "T_t1"], ["T_t1"])
                    dv(lambda e: e.tensor_tensor(out=Apr[:, k, :], in0=t0, in1=t1, op=ALU.subtract), ["T_t0", "T_t1"], [("Apr", k)])
                    dv(lambda e: e.tensor_tensor(out=t0, in0=a_r, in1=a_i, op=ALU.mult), [("Apr", k - 1), ("Api", k - 1), "T_t0"], ["T_t0"])
                    dv(lambda e: e.tensor_scalar(out=Api[:, k, :], in0=t0, scalar1=2.0, scalar2=None, op0=ALU.mult), ["T_t0"], [("Api", k)])
                dv(lambda e: e.tensor_scalar(out=Apn[:], in0=Api[:], scalar1=-1.0, scalar2=None, op0=ALU.mult),
                   [("Api", k) for k in range(LOGS)], ["Apn"])
                APK = [("Apr", k) for k in range(LOGS)] + [("Api", k) for k in range(LOGS)] + ["Apn"]
                dv(lambda e: e.tensor_copy(out=PWr[:, :, 0:1], in_=Apr[:, 0, :].unsqueeze(2)), APK, ["PW"])
                dv(lambda e: e.tensor_copy(out=PWi[:, :, 0:1], in_=Api[:, 0, :].unsqueeze(2)), APK + ["PW"], ["PW"])
                for k in range(LBL):
                    d = 1 << k
                    arb = Apr[:, k, :].unsqueeze(2).to_broadcast([128, 16, d])
                    aib = Api[:, k, :].unsqueeze(2).to_broadcast([128, 16, d])
                    t0, t1 = pwt0[:, :, 0:d], pwt1[:, :, 0:d]
                    dv(lambda e: e.tensor_tensor(out=t0, in0=PWr[:, :, 0:d], in1=arb, op=ALU.mult), APK + ["PW", "pwt0"], ["pwt0"])
                    dv(lambda e: e.tensor_tensor(out=t1, in0=PWi[:, :, 0:d], in1=aib, op=ALU.mult), APK + ["PW", "pwt1"], ["pwt1"])
                    dv(lambda e: e.tensor_tensor(out=PWr[:, :, d:2 * d], in0=t0, in1=t1, op=ALU.subtract), ["pwt0", "pwt1", "PW"], ["PW"])
                    dv(lambda e: e.tensor_tensor(out=t0, in0=PWr[:, :, 0:d], in1=aib, op=ALU.mult), APK + ["PW", "pwt0"], ["pwt0"])
                    dv(lambda e: e.tensor_tensor(out=t1, in0=PWi[:, :, 0:d], in1=arb, op=ALU.mult), APK + ["PW", "pwt1"], ["pwt1"])
                    dv(lambda e: e.tensor_tensor(out=PWi[:, :, d:2 * d], in0=t0, in1=t1, op=ALU.add), ["pwt0", "pwt1", "PW"], ["PW"])

                for j in range(16):
                    q, i = j // 4, j % 4
                    for ri, LB, lk in ((0, LBr, "LBr"), (1, LBi, "LBi")):
                        for n in range(NG):
                            b = 1 + (n + ri) % 4
                            P.op("pe", lambda e: e.matmul(pb[b][:, :], lhsT=LB[:, j, :], rhs=uTs[:, q, n * 512:(n + 1) * 512],
                                                          start=True, stop=True),
                                 reads=["uTs", (lk, j, 0), (lk, j, 1), lk], writes=[("pb", b)])
                            P.op("act", lambda e: e.activation(out=XA[:, ri, n * 512:(n + 1) * 512], in_=pb[b][:, :], func=AF.Copy),
                                 reads=[("pb", b)], writes=[("XA", ri)])
                    cur, curk, oth, othk = XA, "XA", XB, "XB"
                    v4 = lambda T, ri: T[:, ri, :].rearrange("p (b j) -> p b j", j=BL)
                    for k in range(LBL):
                        d = 1 << k
                        new, newk = oth, othk
                        ar_k, ai_k, an_k = Apr[:, k, j:j + 1], Api[:, k, j:j + 1], Apn[:, k, j:j + 1]
                        P.op("dve", lambda e: e.tensor_copy(out=new[:].rearrange("p r (b j) -> p r b j", j=BL)[:, :, :, 0:d],
                                                            in_=cur[:].rearrange("p r (b j) -> p r b j", j=BL)[:, :, :, 0:d]),
                             reads=[(curk, 0), (curk, 1)], writes=[(newk, 0), (newk, 1)])
                        P.op("dve", lambda e: e.scalar_tensor_tensor(out=v4(new, 0)[:, :, d:BL], in0=v4(cur, 0)[:, :, 0:BL - d], scalar=ar_k,
                                                                     in1=v4(cur, 0)[:, :, d:BL], op0=ALU.mult, op1=ALU.add),
                             reads=[(curk, 0)] + APK, writes=[(newk, 0)])
                        P.op("dve", lambda e: e.scalar_tensor_tensor(out=v4(new, 1)[:, :, d:BL], in0=v4(cur, 1)[:, :, 0:BL - d], scalar=ar_k,
                                                                     in1=v4(cur, 1)[:, :, d:BL], op0=ALU.mult, op1=ALU.add),
                             reads=[(curk, 1)] + APK, writes=[(newk, 1)])
                        P.op("dve", lambda e: e.scalar_tensor_tensor(out=v4(new, 0)[:, :, d:BL], in0=v4(cur, 1)[:, :, 0:BL - d], scalar=an_k,
                                                                     in1=v4(new, 0)[:, :, d:BL], op0=ALU.mult, op1=ALU.add),
                             reads=[(curk, 1), (newk, 0)] + APK, writes=[(newk, 0)])
                        P.op("dve", lambda e: e.scalar_tensor_tensor(out=v4(new, 1)[:, :, d:BL], in0=v4(cur, 0)[:, :, 0:BL - d], scalar=ai_k,
                                                                     in1=v4(new, 1)[:, :, d:BL], op0=ALU.mult, op1=ALU.add),
                             reads=[(curk, 0), (newk, 1)] + APK, writes=[(newk, 1)])
                        cur, curk, oth, othk = new, newk, cur, curk
                    ecur, ecurk, eoth, eothk = EA, "EA", EB, "EB"
                    P.op("dve", lambda e: e.tensor_copy(out=ecur[:], in_=cur[:].rearrange("p r (b j) -> p r b j", j=BL)[:, :, :, BL - 1]),
                         reads=[(curk, 0), (curk, 1)], writes=[(ecurk, 0), (ecurk, 1)])
                    for k in range(LOGS - LBL):
                        d = 1 << k
                        kk = LBL + k
                        ar_k, ai_k, an_k = Apr[:, kk, j:j + 1], Api[:, kk, j:j + 1], Apn[:, kk, j:j + 1]
                        P.op("dve", lambda e: e.tensor_copy(out=eoth[:, :, 0:d], in_=ecur[:, :, 0:d]),
                             reads=[(ecurk, 0), (ecurk, 1)], writes=[(eothk, 0), (eothk, 1)])
                        P.op("dve", lambda e: e.scalar_tensor_tensor(out=eoth[:, 0, d:NBK], in0=ecur[:, 0, 0:NBK - d], scalar=ar_k,
                                                                     in1=ecur[:, 0, d:NBK], op0=ALU.mult, op1=ALU.add),
                             reads=[(ecurk, 0)] + APK, writes=[(eothk, 0)])
                        P.op("dve", lambda e: e.scalar_tensor_tensor(out=eoth[:, 1, d:NBK], in0=ecur[:, 1, 0:NBK - d], scalar=ar_k,
                                                                     in1=ecur[:, 1, d:NBK], op0=ALU.mult, op1=ALU.add),
                             reads=[(ecurk, 1)] + APK, writes=[(eothk, 1)])
                        P.op("dve", lambda e: e.scalar_tensor_tensor(out=eoth[:, 0, d:NBK], in0=ecur[:, 1, 0:NBK - d], scalar=an_k,
                                                                     in1=eoth[:, 0, d:NBK], op0=ALU.mult, op1=ALU.add),
                             reads=[(ecurk, 1), (eothk, 0)] + APK, writes=[(eothk, 0)])
                        P.op("dve", lambda e: e.scalar_tensor_tensor(out=eoth[:, 1, d:NBK], in0=ecur[:, 0, 0:NBK - d], scalar=ai_k,
                                                                     in1=eoth[:, 1, d:NBK], op0=ALU.mult, op1=ALU.add),
                             reads=[(ecurk, 0), (eothk, 1)] + APK, writes=[(eothk, 1)])
                        ecur, ecurk, eoth, eothk = eoth, eothk, ecur, ecurk
                    NB1 = NBK - 1
                    bc_pw = lambda T: T[:, j, :].unsqueeze(1).to_broadcast([128, NB1, BL])
                    bc_c = lambda ri: ecur[:, ri, 0:NB1].unsqueeze(2).to_broadcast([128, NB1, BL])
                    blk1 = lambda T, ri: T[:, ri, BL:S].rearrange("p (b j) -> p b j", j=BL)
                    ECK = [(ecurk, 0), (ecurk, 1)]
                    P.op("act", lambda e: e.activation(out=Sb[:, :, 0:BL], in_=cur[:, :, 0:BL], func=AF.Copy),
                         reads=[(curk, 0), (curk, 1)], writes=[("Sb", 0, "h"), ("Sb", 1, "h")])
                    for eng, ri, c1, c2, lastop in (("dve", 0, 0, 1, ALU.subtract), ("pool", 1, 1, 0, ALU.add)):
                        tmp = blk1(oth, ri)
                        P.op(eng, lambda e: e.tensor_tensor(out=tmp, in0=bc_pw(PWr), in1=bc_c(c1), op=ALU.mult),
                             reads=ECK + ["PW"], writes=[(othk, ri)])
                        P.op(eng, lambda e: e.tensor_tensor(out=blk1(cur, ri), in0=blk1(cur, ri), in1=tmp, op=ALU.add),
                             reads=[(othk, ri), (curk, ri)], writes=[(curk, ri)])
                        P.op(eng, lambda e: e.tensor_tensor(out=tmp, in0=bc_pw(PWi), in1=bc_c(c2), op=ALU.mult),
                             reads=ECK + ["PW", (othk, ri)], writes=[(othk, ri)])
                        P.op(eng, lambda e: e.tensor_tensor(out=blk1(Sb, ri), in0=blk1(cur, ri), in1=tmp, op=lastop),
                             reads=[(othk, ri), (curk, ri)], writes=[("Sb", ri)])
                    SK = [("Sb", 0), ("Sb", 1), ("Sb", 0, "h"), ("Sb", 1, "h")]
                    for n in range(NG):
                        b = 5 + n % 2
                        sl = slice(n * 512, (n + 1) * 512)
                        P.op("pe", lambda e: e.matmul(pb[b][0:32, :], lhsT=Cre[:, j, :], rhs=Sb[:, 0, sl], start=True, stop=False),
                             reads=SK + ["Cre"], writes=[("pb", b)])
                        P.op("pe", lambda e: e.matmul(pb[b][0:32, :], lhsT=Cim[:, j, :], rhs=Sb[:, 1, sl], start=False, stop=False),
                             reads=SK + ["Cim"], writes=[("pb", b)])
                        P.op("pe", lambda e: e.matmul(pb[b][0:32, :], lhsT=Dm[:, j, :], rhs=uTs[:, q, sl], start=False, stop=True),
                             reads=["uTs", "Dm"], writes=[("pb", b)])
                        P.op("act", lambda e: e.activation(out=yst[:, sl], in_=pb[b][0:32, :], func=AF.Copy),
                             reads=[("pb", b)], writes=["yst"])
                    P.dma("sp", lambda e: e.dma_start(out=ysT_d[32 * j:32 * j + 32, :], in_=yst[:, :]),
                          reads=["yst"], writes=[("ysT_d", j)])
                P.barrier()

        ya_st = ExitStack()
        yatt = sb(ya_st, "yatt", [128, NT, 512], BF16)
        if "3" in phases:
            with ExitStack() as ph:
                qa = [sb(ph, "qa%d" % i, [70, S], BF16) for i in range(2)]
                ka = [sb(ph, "ka%d" % i, [70, S], BF16) for i in range(2)]
                PTb = [sb(ph, "PT%d" % i, [128, 512], BF16) for i in range(3)]
                rl = sb(ph, "rl", [128, 4], F32)
                cin = [sb(ph, "cin%d" % i, [128, 4, D], F32) for i in range(2)]
                cout = [sb(ph, "cout%d" % i, [128, 4, D], BF16) for i in range(2)]
                p0g = p0_gen(l, cin, cout) if "5" in phases else iter(())
                p0_every = max(1, (NH * NG * (2 * NG + 2)) // 70)
                items = [(hd, G, j) for hd in range(NH) for G in range(NG) for j in range(4 * G + 4)]
                SB_ = [0, 1, 6]

                def st1(n):
                    hd, G, j = items[n]
                    qh, kh = qa[hd % 2], ka[hd % 2]
                    qk_, kk_ = "qa%d" % (hd % 2), "ka%d" % (hd % 2)
                    if G == 0 and j == 0:
                        P.dma("sp", lambda e: e.dma_start(out=qh[:], in_=qT_d[hd]), writes=[qk_])
                        P.dma("sp", lambda e: e.dma_start(out=kh[:], in_=kT_d[hd]), writes=[kk_])
                    i0 = max(0, j - 4 * G)
                    diag = j >= 4 * G
                    c0 = i0 * 128
                    sp_b = SB_[n % 3]
                    PT, ptk = PTb[n % 3], "PT%d" % (n % 3)
                    P.op("pe", lambda e: e.matmul(pb[sp_b][:, c0:512], lhsT=kh[:, j * 128:(j + 1) * 128],
                                                  rhs=qh[:, G * 512 + c0:(G + 1) * 512], start=True, stop=not diag),
                         reads=[qk_, kk_], writes=[("pb", sp_b)])
                    if diag:
                        P.op("pe", lambda e: e.matmul(pb[sp_b][:, c0:c0 + 128], lhsT=ident[:], rhs=negm[:],
                                                      start=False, stop=True),
                             reads=["ident", "negm"], writes=[("pb", sp_b)])
                    P.op("act", lambda e: e.activation(out=PT[:, c0:512], in_=pb[sp_b][:, c0:512], func=AF.Exp,
                                                       scale=0.125), reads=[("pb", sp_b)], writes=[ptk])

                def st2(n):
                    hd, G, j = items[n]
                    i0 = max(0, j - 4 * G)
                    PT, ptk = PTb[n % 3], "PT%d" % (n % 3)
                    for i in range(i0, 4):
                        P.op("pe", lambda e: e.matmul(pb[2 + i][:, 0:65], lhsT=PT[:, i * 128:(i + 1) * 128],
                                                      rhs=Vaug[:, j, hd, :], start=(j == 0), stop=(j == 4 * G + i)),
                             reads=[ptk, ("Vaug", j), "Vaug"], writes=[("pb", 2 + i)])
                    if j == 4 * G + 3:
                        for i in range(4):
                            t = 4 * G + i
                            P.op("dve", lambda e: e.reciprocal(out=rl[:, i:i + 1], in_=pb[2 + i][:, 64:65]),
                                 reads=[("pb", 2 + i)], writes=[("rl", i)])
                            P.op("dve", lambda e: e.tensor_scalar(out=yatt[:, t, hd * 64:(hd + 1) * 64], in0=pb[2 + i][:, 0:64],
                                                                  scalar1=rl[:, i:i + 1], scalar2=None, op0=ALU.mult),
                                 reads=[("pb", 2 + i), ("rl", i)], writes=[("yatt", t, hd)])

                for n in range(len(items)):
                    st1(n)
                    if n >= 1:
                        st2(n - 1)
                    if (n + 1) % p0_every == 0:
                        next(p0g, None)
                st2(len(items) - 1)
                for _ in p0g:
                    pass
                if dbg:
                    for t in range(NT):
                        P.dma("sp", lambda e: e.dma_start(out=ya_d[t * 128:(t + 1) * 128, :], in_=yatt[:, t, :]),
                              reads=[("yatt", t, hd) for hd in range(NH)], writes=[("ya_d", t)])
                P.barrier()

        if "4" in phases:
            with ExitStack() as ph:
                wst = sb(ph, "wst", [128, 8, 512], F32)
                Wg = sb(ph, "Wg", [128, 4, 512], BF16)
                Wo = sb(ph, "Wo", [128, 8, D], BF16)
                gsaT = sb(ph, "gsaT", [128, 8], F32)
                bgl = sb(ph, "bgl", [128, 4], F32)
                ys = sb(ph, "ys", [128, 4, 512], F32)
                gf = sb(ph, "gf", [128, 4, 512], F32)
                gb = sb(ph, "gb", [128, 4, 512], BF16)
                sg = [sb(ph, "sg%d" % i, [128, 512], F32) for i in range(2)]
                ob = sb(ph, "ob", [128, 4, 512], BF16)
                osq = sb(ph, "osq", [128, 4, 512], BF16)
                st4 = sb(ph, "st4", [128, 8], F32)
                junkb = sb(ph, "junkb", [128, 512], BF16)
                yan = sb(ph, "yan", [128, 512], BF16)
                yaT = sb(ph, "yaT", [128, 4, 128], BF16)
                hb4 = [sb(ph, "hb4_%d" % i, [128, D], F32) for i in range(2)]
                P.dma("sp", lambda e: e.dma_start(out=gsaT[:], in_=gsaT_d[l]), writes=["gsaT"])
                P.dma("sp", lambda e: e.dma_start(out=bgl[:], in_=bgluT_d[l]), writes=["bgl"])
                load_w_bf(ph, "Wg", Wg, wglu_d[l], 4, 512, wst=wst)
                load_w_bf(ph, "Wo", Wo, w_o_d[l], 8, D, gainT=gsaT, gkey="gsaT", wst=wst)
                WGK = wkeys("Wg", 4, 512)
                WOK = wkeys("Wo", 8, D)
                for tg in range(NG):
                    tsl = slice(tg * 512, (tg + 1) * 512)
                    P.dma("sp", lambda e: e.dma_start(out=ys[:], in_=ysT_d.rearrange("(c p) t -> p c t", p=128)[:, :, tsl]),
                          writes=["ys"])
                    P.op("act", lambda e: e.activation(out=gf[:], in_=ys[:], func=AF.Gelu_apprx_tanh), reads=["ys"], writes=["gf"])
                    P.op("pool", lambda e: e.tensor_copy(out=gb[:], in_=gf[:]), reads=["gf"], writes=["gb"])
                    for m in range(4):
                        b = m % 2
                        for c in range(4):
                            P.op("pe", lambda e: e.matmul(pb[b][:, :], lhsT=Wg[:, c, m * 128:(m + 1) * 128], rhs=gb[:, c, :],
                                                          start=(c == 0), stop=(c == 3)),
                                 reads=["gb", ("Wg", c, 0)], writes=[("pb", b)])
                        P.op("act", lambda e: e.activation(out=sg[b][:], in_=pb[b][:, :], func=AF.Sigmoid, bias=bgl[:, m:m + 1]),
                             reads=[("pb", b), "bgl"], writes=["sg%d" % b])
                        P.op("dve", lambda e: e.tensor_tensor(out=ob[:, m, :], in0=gf[:, m, :], in1=sg[b][:], op=ALU.mult),
                             reads=["gf", "sg%d" % b], writes=[("ob", m)])
                        P.op("pool", lambda e: e.tensor_tensor(out=osq[:, m, :], in0=ob[:, m, :], in1=ob[:, m, :], op=ALU.mult),
                             reads=[("ob", m)], writes=[("osq", m)])
                    OBK = [("ob", m) for m in range(4)]
                    OSK = [("osq", m) for m in range(4)]
                    for ti in range(4):
                        t = tg * 4 + ti
                        csl = slice(ti * 128, (ti + 1) * 128)
                        hb = hb4[t % 2]
                        hk = "hb4_%d" % (t % 2)
                        P.dma("sp", lambda e: e.dma_start(out=hb[:], in_=hsrc[t * 128:(t + 1) * 128, :]), writes=[hk])
                        for m in range(4):
                            P.op("pe", lambda e: e.matmul(pb[6][:, 0:1], lhsT=osq[:, m, csl], rhs=ones_bf[:, 0:1],
                                                          start=(m == 0), stop=(m == 3)),
                                 reads=OSK + ["ones_bf"], writes=[("pb", 6)])
                        for hf in range(2):
                            for m in range(4):
                                P.op("pe", lambda e: e.matmul(pb[2 + hf][:, :], lhsT=ob[:, m, csl], rhs=Wo[:, m, hf * 512:(hf + 1) * 512],
                                                              start=(m == 0), stop=(m == 3)),
                                     reads=OBK + [("Wo", m, hf * 512)], writes=[("pb", 2 + hf)])
                        rstd_from_ss(pb[6][:, 0:1], st4[:, 0:1], 512, ("pb", 6), "st4a")
                        P.op("act", lambda e: e.activation(out=junkb[:], in_=yatt[:, t, :], func=AF.Square, accum_out=st4[:, 1:2]),
                             reads=[("yatt", t, hd) for hd in range(NH)] + [("yatt", t)], writes=["junkb", "st4b"])
                        rstd_from_ss(st4[:, 1:2], st4[:, 2:3], 512, "st4b", "st4c")
                        P.op("act", lambda e: e.activation(out=yan[:], in_=yatt[:, t, :], func=AF.Copy, scale=st4[:, 2:3]),
                             reads=["st4c", ("yatt", t)], writes=["yan"])
                        for c in range(4):
                            P.op("pe", lambda e: e.transpose(out=pT[:, c * 128:(c + 1) * 128], in_=yan[:, c * 128:(c + 1) * 128],
                                                             identity=ident[:]), reads=["yan", "ident"], writes=[("pT", c)])
                        P.op("dve", lambda e: e.tensor_copy(out=yaT[:], in_=pT[:, 0:512].rearrange("p (c t) -> p c t", c=4)),
                             reads=[("pT", c) for c in range(4)], writes=["yaT"])
                        for hf in range(2):
                            for c in range(4):
                                P.op("pe", lambda e: e.matmul(pb[4 + hf][:, :], lhsT=yaT[:, c, :], rhs=Wo[:, 4 + c, hf * 512:(hf + 1) * 512],
                                                              start=(c == 0), stop=(c == 3)),
                                     reads=["yaT", ("Wo", 4 + c, hf * 512)], writes=[("pb", 4 + hf)])
                        for hf in range(2):
                            hs = slice(hf * 512, (hf + 1) * 512)
                            P.op("dve", lambda e: e.scalar_tensor_tensor(out=hb[:, hs], in0=pb[2 + hf][:, :], scalar=st4[:, 0:1],
                                                                         in1=hb[:, hs], op0=ALU.mult, op1=ALU.add),
                                 reads=[("pb", 2 + hf), "st4a", hk], writes=[hk])
                            P.op("dve", lambda e: e.tensor_tensor(out=hb[:, hs], in0=pb[4 + hf][:, :], in1=hb[:, hs], op=ALU.add),
                                 reads=[("pb", 4 + hf), hk], writes=[hk])
                        P.dma("sp", lambda e: e.dma_start(out=h_d[t * 128:(t + 1) * 128, :], in_=hb[:]), reads=[hk],
                              writes=[("h_d", t)])
                P.barrier()
            hsrc = h_d

        ya_st.close()
        va_st.close()
        if "5" in phases:
            final = last_final and (l == L - 1)
            with ExitStack() as ph:
                Wq = sb(ph, "Wq", [128, 8, D], BF16)
                kTb = sb(ph, "kTb", [128, 16, 128], BF16)
                g2b = sb(ph, "g2b", [128, D], F32)
                hb5 = [sb(ph, "hb5_%d" % i, [128, D], F32) for i in range(2)]
                hng = [sb(ph, "hng%d" % i, [128, D], F32) for i in range(1)]
                junkb = sb(ph, "junkb5", [128, D], BF16)
                hngb = [sb(ph, "hngb%d" % i, [128, D], BF16) for i in range(2)]
                NZ = 4
                zb = [sb(ph, "zb%d" % i, [128, D], BF16) for i in range(NZ)]
                hT = sb(ph, "hT", [128, 8, 128], BF16)
                qTt = sb(ph, "qTt", [128, 8, 128], BF16)
                sc = sb(ph, "sc", [128, 16, 128], F32)
                wk = sb(ph, "wk", [128, 256], F32)
                tv = sb(ph, "tv", [128, 16, 16], F32)
                tix = sb(ph, "tix", [128, 16, 16], U32)
                tif = sb(ph, "tif", [128, 16, 16], F32)
                cand = sb(ph, "cand", [128, 8, 256], F32)
                best = sb(ph, "best", [128, 8, 16], F32)
                pos = sb(ph, "pos", [128, 8, 16], U32)
                pa = sb(ph, "pa", [128, 8, 16], I32)
                pbb = sb(ph, "pbb", [128, 8, 16], I32)
                paf = sb(ph, "paf", [128, 8, 16], F32)
                pbf = sb(ph, "pbf", [128, 8, 16], F32)
                oh = sb(ph, "oh", [128, 8, 16, 16], F32)
                sel1 = sb(ph, "sel1", [128, 8, 16], F32)
                sel2 = sb(ph, "sel2", [128, 8, 16], F32)
                idxf = sb(ph, "idxf", [128, 128], F32)
                idx = [sb(ph, "idx%d" % i, [128, 128], I32) for i in range(2)]
                gate = [sb(ph, "gate%d" % i, [128, 8, 16], F32) for i in range(2)]
                gsum = sb(ph, "gsum", [128, 8], F32)
                act_ = [sb(ph, "act%d" % i, [128, 128], F32) for i in range(2)]
                gl_ = [sb(ph, "gl%d" % i, [128, 128], F32) for i in range(2)]
                coef = [sb(ph, "coef%d" % i, [128, 128], F32) for i in range(2)]
                outb = [sb(ph, "outb%d" % i, [128, D], F32) for i in range(1)]
                st5 = sb(ph, "st5", [128, 8], F32)
                st6 = sb(ph, "st6", [128, 8], F32)
                NB, KB, NDG = 24, 4, 8
                P.dma("sp", lambda e: e.dma_start(out=g2b[:], in_=g2_d[l].to_broadcast([128, D])), writes=["g2b"])
                with ExitStack() as wph:
                    kst = sb(wph, "kst", [128, 16, 128], F32)
                    P.dma("sp", lambda e: e.dma_start(out=kst[:], in_=keysT_d[l]), writes=["kst"])
                    P.op("act", lambda e: e.activation(out=kTb[:], in_=kst[:], func=AF.Copy), reads=["kst"], writes=["kTb"])
                    wst = sb(wph, "wst", [128, 8, 512], F32)
                    load_w_bf(wph, "Wq", Wq, w_q_d[l], 8, D, wst=wst)
                    P.barrier()
                uvb = [sb(ph, "uvb%d" % i, [128, 2 * D], BF16) for i in range(NB)]
                dg = [sb(ph, "dg%d" % i, [128, 128], BF16) for i in range(NDG)]

                def front_end(t):
                    p2 = t % 2
                    hb, hk = hb5[p2], "hb5_%d" % p2
                    hg, hgk = hng[0], "hng0"
                    P.dma("sp", lambda e: e.dma_start(out=hb[:], in_=hsrc[t * 128:(t + 1) * 128, :]), reads=[("h_d", t)], writes=[hk])
                    P.op("act", lambda e: e.activation(out=junkb[:], in_=hb[:], func=AF.Square, accum_out=st5[:, 0:1]),
                         reads=[hk], writes=["junkb5", "st5a"])
                    yield
                    yield
                    rstd_from_ss(st5[:, 0:1], st5[:, 1:2], D, "st5a", "st5b")
                    P.op("dve", lambda e: e.scalar_tensor_tensor(out=hg[:], in0=hb[:], scalar=st5[:, 1:2], in1=g2b[:],
                                                                 op0=ALU.mult, op1=ALU.mult),
                         reads=[hk, "st5b", "g2b"], writes=[hgk])
                    yield
                    xs5, xs5k = hngb[p2], "hngb%d" % p2
                    P.op("act", lambda e: e.activation(out=xs5[:], in_=hg[:], func=AF.Copy), reads=[hgk], writes=[xs5k])
                    yield
                    yield
                    for c in range(8):
                        P.op("pe", lambda e: e.transpose(out=pT[:, c * 128:(c + 1) * 128], in_=xs5[:, c * 128:(c + 1) * 128],
                                                         identity=ident[:]), reads=[xs5k, "ident"], writes=[("pT", c)])
                    P.op("dve", lambda e: e.tensor_copy(out=hT[:], in_=pT[:].rearrange("p (c t) -> p c t", c=8)),
                         reads=[("pT", c) for c in range(8)], writes=["hT"])
                    yield
                    yield
                    for hd in range(NH):
                        b = hd // 4
                        for c in range(8):
                            P.op("pe", lambda e: e.matmul(pb[b][:, (hd % 4) * 128:(hd % 4 + 1) * 128],
                                                          lhsT=Wq[:, c, hd * 128:(hd + 1) * 128], rhs=hT[:, c, :],
                                                          start=(c == 0), stop=(c == 7)),
                                 reads=["hT", ("Wq", c, (hd // 4) * 512)], writes=[("pb", b, hd % 4)])
                        yield
                    for b in range(2):
                        evac(qTt[:, 4 * b:4 * b + 4, :], pb[b][:, :].rearrange("p (h t) -> p h t", h=4),
                             [("pb", b, i) for i in range(4)], [("qTt", b)])
                    yield
                    for half8 in range(2):
                        for bl in range(8):
                            blk = half8 * 8 + bl
                            hd = blk // 2
                            b = 2 + bl // 4
                            P.op("pe", lambda e: e.matmul(pb[b][:, (bl % 4) * 128:(bl % 4 + 1) * 128],
                                                          lhsT=qTt[:, hd, :], rhs=kTb[:, blk, :], start=True, stop=True),
                                 reads=[("qTt", hd // 4), "kTb"], writes=[("pb", b, bl % 4)])
                        for b in range(2):
                            g4 = half8 * 2 + b
                            evac(sc[:, 4 * g4:4 * g4 + 4, :], pb[2 + b][:, :].rearrange("p (h t) -> p h t", h=4),
                                 [("pb", 2 + b, i) for i in range(4)], [("sc", g4)])
                        yield
                    for blk in range(16):
                        sk = ("sc", blk // 4)
                        P.op("dve", lambda e: e.max(out=tv[:, blk, 0:8], in_=sc[:, blk, :]), reads=[sk], writes=[("tv", blk, 0)])
                        yield
                        P.op("dve", lambda e: e.match_replace(out=wk[:, 0:128], in_to_replace=tv[:, blk, 0:8],
                                                              in_values=sc[:, blk, :], imm_value=-1e30),
                             reads=[sk, ("tv", blk, 0)], writes=["wk"])
                        yield
                        P.op("dve", lambda e: e.max(out=tv[:, blk, 8:16], in_=wk[:, 0:128]), reads=["wk"], writes=[("tv", blk, 1)])
                        yield
                        P.op("dve", lambda e: e.max_index(out=tix[:, blk, 0:8], in_max=tv[:, blk, 0:8], in_values=sc[:, blk, :]),
                             reads=[sk, ("tv", blk, 0)], writes=[("tix", blk, 0)])
                        yield
                        P.op("dve", lambda e: e.max_index(out=tix[:, blk, 8:16], in_max=tv[:, blk, 8:16], in_values=sc[:, blk, :]),
                             reads=[sk, ("tv", blk, 1)], writes=[("tix", blk, 1)])
                        yield
                        yield
                    TVK = [("tv", b, i) for b in range(16) for i in range(2)]
                    TIK = [("tix", b, i) for b in range(16) for i in range(2)]
                    P.op("dve", lambda e: e.tensor_copy(out=tif[:], in_=tix[:]), reads=TIK, writes=["tif"])
                    yield
                    tv4 = tv[:].rearrange("p (h j) k -> p h j k", j=2)
                    tif4 = tif[:].rearrange("p (h j) k -> p h j k", j=2)
                    for hd in range(NH):
                        P.op("dve", lambda e: e.tensor_tensor(out=cand[:, hd, :].rearrange("p (a b) -> p a b", a=16),
                                                              in0=tv4[:, hd, 0, :].unsqueeze(2).to_broadcast([128, 16, 16]),
                                                              in1=tv4[:, hd, 1:2, :].to_broadcast([128, 16, 16]), op=ALU.add),
                             reads=TVK, writes=[("cand", hd)])
                        yield
                        P.op("dve", lambda e: e.max(out=best[:, hd, 0:8], in_=cand[:, hd, :]), reads=[("cand", hd)], writes=[("best", hd, 0)])
                        yield
                        P.op("dve", lambda e: e.match_replace(out=wk[:, :], in_to_replace=best[:, hd, 0:8], in_values=cand[:, hd, :],
                                                              imm_value=-1e30), reads=[("cand", hd), ("best", hd, 0)], writes=["wk"])
                        yield
                        P.op("dve", lambda e: e.max(out=best[:, hd, 8:16], in_=wk[:, :]), reads=["wk"], writes=[("best", hd, 1)])
                        yield
                        P.op("dve", lambda e: e.max_index(out=pos[:, hd, 0:8], in_max=best[:, hd, 0:8], in_values=cand[:, hd, :]),
                             reads=[("cand", hd), ("best", hd, 0)], writes=[("pos", hd, 0)])
                        yield
                        P.op("dve", lambda e: e.max_index(out=pos[:, hd, 8:16], in_max=best[:, hd, 8:16], in_values=cand[:, hd, :]),
                             reads=[("cand", hd), ("best", hd, 1)], writes=[("pos", hd, 1)])
                        yield
                        yield
                    BK = [("best", h_, i) for h_ in range(NH) for i in range(2)]
                    PK = [("pos", h_, i) for h_ in range(NH) for i in range(2)]
                    posi = pos[:].bitcast(I32)
                    P.op("dve", lambda e: e.tensor_scalar(out=pa[:], in0=posi, scalar1=4, scalar2=None, op0=ALU.arith_shift_right),
                         reads=PK, writes=["pa"])
                    yield
                    P.op("dve", lambda e: e.tensor_scalar(out=pbb[:], in0=posi, scalar1=15, scalar2=None, op0=ALU.bitwise_and),
                         reads=PK, writes=["pbb"])
                    yield
                    P.op("dve", lambda e: e.tensor_copy(out=paf[:], in_=pa[:]), reads=["pa"], writes=["paf"])
                    yield
                    P.op("dve", lambda e: e.tensor_copy(out=pbf[:], in_=pbb[:]), reads=["pbb"], writes=["pbf"])
                    yield
                    yield
                    io4 = iota16[:].unsqueeze(1).unsqueeze(1).to_broadcast([128, 8, 16, 16])
                    for pf, pfk, half, sel, selk in ((paf, "paf", 0, sel1, "sel1"), (pbf, "pbf", 1, sel2, "sel2")):
                        P.op("dve", lambda e: e.tensor_tensor(out=oh[:], in0=pf[:].unsqueeze(3).to_broadcast([128, 8, 16, 16]),
                                                              in1=io4, op=ALU.is_equal), reads=[pfk, "iota16"], writes=["oh"])
                        yield
                        P.op("dve", lambda e: e.tensor_tensor(out=oh[:], in0=oh[:],
                                                              in1=tif4[:, :, half, :].unsqueeze(2).to_broadcast([128, 8, 16, 16]),
                                                              op=ALU.mult), reads=["oh", "tif"], writes=["oh"])
                        yield
                        P.op("dve", lambda e: e.tensor_reduce(out=sel[:], in_=oh[:], axis=AX.X, op=ALU.add), reads=["oh"], writes=[selk])
                        yield
                        yield
                    ix, ixk = idx[p2], "idx%d" % p2
                    P.op("dve", lambda e: e.scalar_tensor_tensor(out=idxf[:], in0=sel1[:].rearrange("p h k -> p (h k)"), scalar=128.0,
                                                                 in1=sel2[:].rearrange("p h k -> p (h k)"), op0=ALU.mult, op1=ALU.add),
                         reads=["sel1", "sel2"], writes=["idxf"])
                    yield
                    if l > 0:
                        P.op("dve", lambda e: e.tensor_scalar(out=idxf[:], in0=idxf[:], scalar1=float(l * NEXP), scalar2=None,
                                                              op0=ALU.add), reads=["idxf"], writes=["idxf"])
                        yield
                    P.op("dve", lambda e: e.tensor_copy(out=ix[:], in_=idxf[:]), reads=["idxf"], writes=[ixk])
                    yield
                    yield
                    gt, gtk = gate[p2], "gate%d" % p2
                    P.op("dve", lambda e: e.tensor_tensor(out=gt[:], in0=best[:], in1=best[:, :, 0:1].to_broadcast([128, 8, 16]),
                                                          op=ALU.subtract), reads=BK, writes=[gtk])
                    yield
                    P.op("act", lambda e: e.activation(out=gt[:], in_=gt[:], func=AF.Exp), reads=[gtk], writes=[gtk])
                    yield
                    P.op("dve", lambda e: e.tensor_reduce(out=gsum[:], in_=gt[:], axis=AX.X, op=ALU.add), reads=[gtk], writes=["gsum"])
                    yield
                    P.op("dve", lambda e: e.reciprocal(out=gsum[:], in_=gsum[:]), reads=["gsum"], writes=["gsum"])
                    yield
                    P.op("dve", lambda e: e.tensor_tensor(out=gt[:], in0=gt[:], in1=gsum[:].unsqueeze(2).to_broadcast([128, 8, 16]),
                                                          op=ALU.mult), reads=[gtk, "gsum"], writes=[gtk])
                    yield
                    if dbg:
                        P.dma("sp", lambda e: e.dma_start(out=idx_dbg[t * 128:(t + 1) * 128, :], in_=ix[:]), reads=[ixk], writes=[("idbg", t)])
                        P.dma("sp", lambda e: e.dma_start(out=gate_dbg[t * 128:(t + 1) * 128, :], in_=gt[:].rearrange("p h k -> p (h k)")),
                              reads=[gtk], writes=[("gdbg", t)])
                        P.dma("sp", lambda e: e.dma_start(out=sc_dbg[t * 128:(t + 1) * 128, :], in_=sc[:].rearrange("p h k -> p (h k)")),
                              reads=[("sc", b_) for b_ in range(4)], writes=[("sdbg", t)])
                    yield

                cnt5 = {"u": 0, "d": 0, "z": 0}

                def k_loop(t, fe_next):
                    p2 = t % 2
                    hb, hk = hb5[p2], "hb5_%d" % p2
                    hg, hgk = hng[0], "hng0"
                    hgb, hgbk = hngb[p2], "hngb%d" % p2
                    ix, ixk = idx[p2], "idx%d" % p2
                    gt, gtk = gate[p2], "gate%d" % p2
                    gtf = gt[:].rearrange("p h k -> p (h k)")
                    av, avk = act_[p2], "act%d" % p2
                    gl, glk = gl_[p2], "gl%d" % p2
                    cf, cfk = coef[p2], "coef%d" % p2
                    NBT = 128 // KB
                    bufs = {}

                    def stA(n):
                        bufs[n] = []
                        for k in range(n * KB, (n + 1) * KB):
                            u_ = cnt5["u"] % NB
                            cnt5["u"] += 1
                            bufs[n].append(u_)
                            P.dma("pool", lambda e: e.indirect_dma_start(out=uvb[u_][:], out_offset=None, in_=uv_d,
                                                                         in_offset=bass.IndirectOffsetOnAxis(ap=ix[:, k:k + 1], axis=0)),
                                  reads=[ixk], writes=["uvb%d" % u_])

                    def stB(n):
                        k0 = n * KB
                        for i, k in enumerate(range(k0, k0 + KB)):
                            u_ = bufs[n][i]
                            z_ = cnt5["z"] % NZ
                            cnt5["z"] += 1
                            P.op("dve", lambda e: e.tensor_tensor(out=zb[z_][:], in0=uvb[u_][:, 0:D], in1=hgb[:], op=ALU.mult),
                                 reads=["uvb%d" % u_, hgbk], writes=["zb%d" % z_])
                            P.op("act", lambda e: e.activation(out=zb[z_][:], in_=zb[z_][:], func=AF.Copy, accum_out=av[:, k:k + 1]),
                                 reads=["zb%d" % z_], writes=["zb%d" % z_, (avk, k0, i)])
                        ks = slice(k0, k0 + KB)
                        P.op("act", lambda e: e.activation(out=gl[:, ks], in_=av[:, ks], func=AF.Gelu_apprx_tanh),
                             reads=[(avk, k0, i) for i in range(KB)], writes=[(glk, k0)])

                    def stC(n):
                        k0 = n * KB
                        ks = slice(k0, k0 + KB)
                        P.op("dve", lambda e: e.tensor_tensor(out=cf[:, ks], in0=gl[:, ks], in1=gtf[:, ks], op=ALU.mult),
                             reads=[(glk, k0), gtk], writes=[(cfk, k0)])
                        for i, k in enumerate(range(k0, k0 + KB)):
                            u_ = bufs[n][i]
                            d_ = cnt5["d"] % NDG
                            cnt5["d"] += 1
                            P.op("dve", lambda e: e.tensor_scalar(out=dg[d_][:], in0=ident[:], scalar1=cf[:, k:k + 1], scalar2=None,
                                                                  op0=ALU.mult), reads=["ident", (cfk, k0)], writes=["dg%d" % d_])
                            for hf in range(2):
                                P.op("pe", lambda e: e.matmul(pb[4 + hf][:, :], lhsT=dg[d_][:], rhs=uvb[u_][:, D + hf * 512:D + (hf + 1) * 512],
                                                              start=(k == 0), stop=(k == 127)),
                                     reads=["dg%d" % d_, "uvb%d" % u_], writes=[("pb", 4 + hf)])

                    LA = 3
                    for n in range(min(LA, NBT)):
                        stA(n)
                    for n in range(NBT):
                        if n + LA < NBT:
                            stA(n + LA)
                        stB(n)
                        if n >= 1:
                            stC(n - 1)
                        for _ in range(7):
                            next(fe_next, None)
                    stC(NBT - 1)
                    for _ in fe_next:
                        pass
                    ob, obk = outb[0], "outb0"
                    for hf in range(2):
                        hs = slice(hf * 512, (hf + 1) * 512)
                        P.op("dve", lambda e: e.tensor_tensor(out=ob[:, hs], in0=pb[4 + hf][:, :], in1=hb[:, hs], op=ALU.add),
                             reads=[("pb", 4 + hf), hk], writes=[(obk, hf)])
                    OBK2 = [(obk, 0), (obk, 1)]
                    if final:
                        P.op("act", lambda e: e.activation(out=junkb[:], in_=ob[:], func=AF.Square, accum_out=st6[:, 0:1]),
                             reads=OBK2, writes=["junkb5", "st6a"])
                        rstd_from_ss(st6[:, 0:1], st6[:, 1:2], D, "st6a", "st6b")
                        P.op("dve", lambda e: e.scalar_tensor_tensor(out=ob[:], in0=ob[:], scalar=st6[:, 1:2], in1=nfb[:],
                                                                     op0=ALU.mult, op1=ALU.mult),
                             reads=OBK2 + ["st6b", "nfb"], writes=OBK2)
                        P.dma("sp", lambda e: e.dma_start(out=out_d[t * 128:(t + 1) * 128, :], in_=ob[:]), reads=OBK2,
                              writes=[("out_d", t)])
                    else:
                        P.dma("sp", lambda e: e.dma_start(out=h_d[t * 128:(t + 1) * 128, :], in_=ob[:]), reads=OBK2,
                              writes=[("h_d", t)])

                for _ in front_end(0):
                    pass
                for t in range(NT):
                    fe_next = front_end(t + 1) if t + 1 < NT else iter(())
                    if "x" in phases:
                        for _ in fe_next:
                            pass
                        continue
                    k_loop(t, fe_next)
                P.barrier()
            hsrc = h_d
    P.barrier()
    top.close()
    return nc, P


def _consts():
    ident = np.eye(128, dtype=np.float32)
    k = np.arange(128)
    negmask = np.where(k[:, None] > k[None, :], -30000.0, 0.0).astype(np.float32)
    gmask = (k[:, None] // 16 == np.arange(8)[None, :]).astype(np.float32)
    iota16 = np.broadcast_to(np.arange(16, dtype=np.float32), (128, 16)).copy()
    return {"ident": ident, "negmask": negmask, "gmask": gmask, "iota16": iota16}


def layout_weights(inp, L):
    f = lambda a: np.ascontiguousarray(np.asarray(a, dtype=np.float32))
    w = {}
    w["g1T"] = f(inp["norm1_g"].reshape(L, 8, 128).transpose(0, 2, 1))
    w["w_in"] = f(inp["w_in"])
    w["b_f"] = f(inp["fox_b_f"].reshape(L, 8, 1))
    lam_re, lam_im = np.asarray(inp["ssm_lambda_re"]), np.asarray(inp["ssm_lambda_im"])
    ldt = np.asarray(inp["ssm_log_dt"])
    rep = lambda a: f(np.repeat(a[:, :, None, :], 16, axis=2).reshape(L, 4, 128, 64))
    w["lamB_re"] = rep(lam_re)
    w["lamB_im"] = rep(lam_im)
    w["dtB"] = f(np.repeat(ldt[:, :, None], 16, axis=2).reshape(L, 4, 128, 1))
    w["bT_re"] = f(np.asarray(inp["ssm_b_re"]).transpose(0, 1, 3, 2).reshape(L, 4, 128, 64))
    w["bT_im"] = f(np.asarray(inp["ssm_b_im"]).transpose(0, 1, 3, 2).reshape(L, 4, 128, 64))
    tl = lambda a: f(a.reshape(L, 16, 2, 64).transpose(0, 2, 3, 1).reshape(L, 128, 16))
    w["lamT_re"] = tl(lam_re)
    w["lamT_im"] = tl(lam_im)
    w["dtT"] = tl(np.repeat(ldt[:, :, None], 64, axis=2))
    def cblk(c):
        c = np.asarray(c).reshape(L, 16, 2, 16, 64)
        o = np.zeros((L, 2, 64, 16, 2, 16), np.float32)
        for g2 in range(2):
            o[:, g2, :, :, g2, :] = c[:, :, g2].transpose(0, 3, 1, 2)
        return f(o.reshape(L, 128, 16, 32))
    w["cT_re"] = cblk(inp["ssm_c_re"])
    w["cT_im"] = cblk(inp["ssm_c_im"])
    d = np.asarray(inp["ssm_d"]).reshape(L, 4, 4, 32)
    dm = np.zeros((L, 4, 32, 4, 4, 32), np.float32)
    for i in range(4):
        for m in range(32):
            dm[:, i, m, :, i, m] = d[:, :, i, m]
    w["dmat"] = f(dm.reshape(L, 128, 16, 32))
    w["w_glu"] = f(inp["ssm_w_glu"])
    w["bgluT"] = f(np.asarray(inp["ssm_b_glu"]).reshape(L, 4, 128).transpose(0, 2, 1))
    gsa = np.concatenate([np.asarray(inp["g_ssm_out"]), np.asarray(inp["g_attn_out"])], axis=1)
    w["gsaT"] = f(gsa.reshape(L, 8, 128).transpose(0, 2, 1))
    w["w_o"] = f(inp["w_o"])
    w["g2"] = f(np.asarray(inp["norm2_g"]).reshape(L, 1, D))
    w["w_q"] = f(inp["peer_w_q"])
    kk = np.asarray(inp["peer_keys"]).transpose(0, 2, 4, 1, 3)
    kz = np.zeros((L, 2, 64, 8, 2, 128), np.float32)
    for j in range(2):
        kz[:, j, :, :, j, :] = kk[:, j]
    w["keysT"] = f(kz.reshape(L, 128, 16, 128))
    w["peer_u"] = f(np.asarray(inp["peer_u"]).reshape(L * NEXP, D))
    w["peer_v"] = f(np.asarray(inp["peer_v"]).reshape(L * NEXP, D))
    w["norm_f"] = f(np.asarray(inp["norm_f"]).reshape(1, D))
    w.update(_consts())
    return w


def kernel(**inputs):
    x = np.asarray(inputs["x"], dtype=np.float32)
    B, S, _ = x.shape
    L = int(np.asarray(inputs["w_in"]).shape[0])
    w = layout_weights(inputs, L)
    nc, _ = build(L, S)
    in_maps = []
    for b in range(B):
        m = dict(w)
        m["x"] = np.ascontiguousarray(x[b])
        in_maps.append(m)
    res = run_bass_kernel_spmd(nc, in_maps, core_ids=list(range(B)))
    return np.stack([np.asarray(r["out"], dtype=np.float32) for r in res.results], axis=0)
```

```python
import math
from contextlib import ExitStack
import numpy as np
import concourse.bass as bass
import concourse.mybir as mybir
from concourse.bass_utils import run_bass_kernel_spmd

F32 = mybir.dt.float32
BF16 = mybir.dt.bfloat16
I32 = mybir.dt.int32
U32 = mybir.dt.uint32
ALU = mybir.AluOpType
AF = mybir.ActivationFunctionType
AX = mybir.AxisListType

D = 1024
NH = 8
EPS = 1e-6
NEXP = 16384


class Prog:
    NDMA = 24

    def __init__(self, nc):
        self.nc = nc
        self.eng = {"pe": nc.tensor, "act": nc.scalar, "dve": nc.vector,
                    "pool": nc.gpsimd, "sp": nc.sync}
        self.sem = {k: nc.alloc_semaphore("sem_" + k) for k in self.eng}
        self.cnt = {k: 0 for k in self.eng}
        self.dsem = [nc.alloc_semaphore("dsem%d" % i) for i in range(2 * self.NDMA)]
        self.dval = [0] * (2 * self.NDMA)
        self.dnext = {"sp": 0, "pool": 0}
        self.seen = {k: {} for k in self.eng}
        self.lastw = {}
        self.readers = {}
        self.ninst = 0

    def _wait(self, e, tok):
        sem, val, uid = tok
        if e == "pe" and uid == "epe":
            return
        if self.seen[e].get(uid, 0) >= val:
            return
        self.seen[e][uid] = val
        self.eng[e].wait_ge(sem, val)
        self.ninst += 1

    def _deps(self, e, reads, writes):
        for k in reads:
            t = self.lastw.get(k)
            if t is not None:
                self._wait(e, t)
        for k in writes:
            t = self.lastw.get(k)
            if t is not None:
                self._wait(e, t)
            for t in self.readers.get(k, ()):
                self._wait(e, t)

    def _commit(self, tok, reads, writes):
        for k in reads:
            self.readers.setdefault(k, []).append(tok)
        for k in writes:
            self.lastw[k] = tok
            self.readers[k] = []
        self.ninst += 1

    def op(self, e, fn, reads=(), writes=()):
        self._deps(e, reads, writes)
        inst = fn(self.eng[e])
        self.cnt[e] += 1
        inst.then_inc(self.sem[e], 1)
        tok = (self.sem[e], self.cnt[e], "e" + e)
        self._commit(tok, reads, writes)
        return tok

    def dma(self, e, fn, reads=(), writes=()):
        k = self.dnext[e] + (self.NDMA if e == "pool" else 0)
        self.dnext[e] = (self.dnext[e] + 1) % self.NDMA
        if self.dval[k] > 0:
            self._wait(e, (self.dsem[k], self.dval[k], "d%d" % k))
        self._deps(e, reads, writes)
        inst = fn(self.eng[e])
        self.dval[k] += 16
        inst.then_inc(self.dsem[k], 16)
        tok = (self.dsem[k], self.dval[k], "d%d" % k)
        self._commit(tok, reads, writes)
        return tok

    def barrier(self):
        for e in self.eng:
            for o in self.eng:
                if self.cnt[o] > 0:
                    self._wait(e, (self.sem[o], self.cnt[o], "e" + o))
            for k in range(2 * self.NDMA):
                if self.dval[k] > 0:
                    self._wait(e, (self.dsem[k], self.dval[k], "d%d" % k))
        self.lastw = {}
        self.readers = {}


def build(L, S, dbg=False, phases="12345", last_final=True, stop=99):
    nc = bass.Bass("TRN2", target_bir_lowering=False)
    P = Prog(nc)
    NT, NG = S // 128, S // 512
    LOGS = int(math.log2(S))
    assert 1 << LOGS == S and NG >= 1

    def din(name, shape, dt=F32):
        return nc.dram_tensor(name, list(shape), dt, kind="ExternalInput").ap()

    def dscr(name, shape, dt):
        return nc.dram_tensor(name, list(shape), dt, kind=("ExternalOutput" if dbg else "Internal")).ap()

    uniq = [0]

    def sb(st, name, shape, dt):
        uniq[0] += 1
        return st.enter_context(nc.sbuf_tensor("s%d_%s" % (uniq[0], name), list(shape), dt))

    x_d = din("x", [S, D])
    g1T_d = din("g1T", [L, 128, 8])
    w_in_d = din("w_in", [L, D, 2056])
    bf_d = din("b_f", [L, 8, 1])
    lamB_re_d = din("lamB_re", [L, 4, 128, 64])
    lamB_im_d = din("lamB_im", [L, 4, 128, 64])
    dtB_d = din("dtB", [L, 4, 128, 1])
    bT_re_d = din("bT_re", [L, 4, 128, 64])
    bT_im_d = din("bT_im", [L, 4, 128, 64])
    lamT_re_d = din("lamT_re", [L, 128, 16])
    lamT_im_d = din("lamT_im", [L, 128, 16])
    dtT_d = din("dtT", [L, 128, 16])
    cT_re_d = din("cT_re", [L, 128, 16, 32])
    cT_im_d = din("cT_im", [L, 128, 16, 32])
    dmat_d = din("dmat", [L, 128, 16, 32])
    wglu_d = din("w_glu", [L, 512, 512])
    bgluT_d = din("bgluT", [L, 128, 4])
    gsaT_d = din("gsaT", [L, 128, 8])
    w_o_d = din("w_o", [L, D, D])
    g2_d = din("g2", [L, 1, D])
    w_q_d = din("w_q", [L, D, D])
    keysT_d = din("keysT", [L, 128, 16, 128])
    pu_d = din("peer_u", [L * NEXP, D])
    pv_d = din("peer_v", [L * NEXP, D])
    nf_d = din("norm_f", [1, D])
    ident_d = din("ident", [128, 128])
    negmask_d = din("negmask", [128, 128])
    gmask_d = din("gmask", [128, 8])
    iota16_d = din("iota16", [128, 16])
    out_d = nc.dram_tensor("out", [S, D], F32, kind="ExternalOutput").ap()

    h_d = dscr("h_scr", [S, D], F32)
    uT_d = dscr("uT_scr", [512, S], BF16)
    qT_d = dscr("qT_scr", [8, 70, S], BF16)
    kT_d = dscr("kT_scr", [8, 70, S], BF16)
    ysT_d = dscr("ysT_scr", [512, S], F32)
    ya_d = dscr("ya_scr", [S, 512], BF16) if dbg else None
    uv_d = nc.dram_tensor("uv_scr", [L * NEXP, 2 * D], BF16, kind="Internal").ap()
    idx_dbg = dscr("idx_dbg", [S, 128], I32) if dbg else None
    gate_dbg = dscr("gate_dbg", [S, 128], F32) if dbg else None
    sc_dbg = dscr("sc_dbg", [S, 2048], F32) if dbg else None

    top = ExitStack()
    ident_f = sb(top, "ident_f", [128, 128], F32)
    ident = sb(top, "ident", [128, 128], BF16)
    negm_f = sb(top, "negm_f", [128, 128], F32)
    negm = sb(top, "negm", [128, 128], BF16)
    gmask = sb(top, "gmask", [128, 8], F32)
    iota16 = sb(top, "iota16", [128, 16], F32)
    eps_t = sb(top, "eps_t", [128, 1], F32)
    one_t = sb(top, "one_t", [128, 1], F32)
    ones_bf = sb(top, "ones_bf", [128, 1], BF16)
    nfb = sb(top, "nfb", [128, D], F32)

    P.dma("sp", lambda e: e.dma_start(out=ident_f[:], in_=ident_d), writes=["ident_f"])
    P.dma("sp", lambda e: e.dma_start(out=negm_f[:], in_=negmask_d), writes=["negm_f"])
    P.dma("sp", lambda e: e.dma_start(out=gmask[:], in_=gmask_d), writes=["gmask"])
    P.dma("sp", lambda e: e.dma_start(out=iota16[:], in_=iota16_d), writes=["iota16"])
    P.dma("sp", lambda e: e.dma_start(out=nfb[:], in_=nf_d.to_broadcast([128, D])), writes=["nfb"])
    P.op("act", lambda e: e.activation(out=ident[:], in_=ident_f[:], func=AF.Copy), reads=["ident_f"], writes=["ident"])
    P.op("act", lambda e: e.activation(out=negm[:], in_=negm_f[:], func=AF.Copy), reads=["negm_f"], writes=["negm"])
    P.op("dve", lambda e: e.memset(eps_t[:], EPS), writes=["eps_t"])
    P.op("dve", lambda e: e.memset(one_t[:], 1.0), writes=["one_t"])
    P.op("dve", lambda e: e.memset(ones_bf[:], 1.0), writes=["ones_bf"])

    pb = [nc.alloc_psum_tensor("pb%d" % i, [128, 512], F32) for i in range(7)]
    pT = nc.alloc_psum_tensor("pT", [128, 1024], BF16)

    evac_rr = [0]

    def evac(out_ap, in_ap, reads, writes, scale=None):
        evac_rr[0] ^= 1
        if evac_rr[0]:
            P.op("act", lambda e: e.activation(out=out_ap, in_=in_ap, func=AF.Copy), reads=reads, writes=writes)
        else:
            P.op("dve", lambda e: e.tensor_copy(out=out_ap, in_=in_ap), reads=reads, writes=writes)

    def rstd_from_ss(ss, rs, n, key_ss, key_rs, src_reads=()):
        P.op("act", lambda e: e.activation(out=rs, in_=ss, func=AF.Sqrt, bias=eps_t[:, 0:1], scale=1.0 / n),
             reads=[key_ss, "eps_t"] + list(src_reads), writes=[key_rs])
        P.op("dve", lambda e: e.reciprocal(out=rs, in_=rs), reads=[key_rs], writes=[key_rs])

    def load_w_bf(st, name, dst, src_ap, nchunk, ncols, gainT=None, gkey=None, wst=None):
        for c0 in range(0, ncols, 512):
            c1 = min(ncols, c0 + 512)
            n = c1 - c0
            P.dma("sp", lambda e: e.dma_start(out=wst[:, 0:nchunk, 0:n],
                                              in_=src_ap.rearrange("(c p) n -> p c n", p=128)[:, :, c0:c1]),
                  writes=["wst"])
            for c in range(nchunk):
                eng = "act" if c % 2 == 0 else "pool"
                if gainT is not None:
                    if eng == "act":
                        P.op("act", lambda e: e.activation(out=dst[:, c, c0:c1], in_=wst[:, c, 0:n], func=AF.Copy,
                                                           scale=gainT[:, c:c + 1]),
                             reads=["wst", gkey], writes=[(name, c, c0)])
                    else:
                        P.op("pool", lambda e: e.tensor_scalar(out=dst[:, c, c0:c1], in0=wst[:, c, 0:n],
                                                               scalar1=gainT[:, c:c + 1], scalar2=None, op0=ALU.mult),
                             reads=["wst", gkey], writes=[(name, c, c0)])
                else:
                    if eng == "act":
                        P.op("act", lambda e: e.activation(out=dst[:, c, c0:c1], in_=wst[:, c, 0:n], func=AF.Copy),
                             reads=["wst"], writes=[(name, c, c0)])
                    else:
                        P.op("pool", lambda e: e.tensor_copy(out=dst[:, c, c0:c1], in_=wst[:, c, 0:n]),
                             reads=["wst"], writes=[(name, c, c0)])

    def wkeys(name, nchunk, ncols):
        return [(name, c, c0) for c in range(nchunk) for c0 in range(0, ncols, 512)]

    def p0_gen(l0, cin, cout):
        NCB = len(cin)
        ci = 0
        for tb, src_d in ((0, pu_d), (1, pv_d)):
            for s_ in range(NEXP // 512):
                r0 = l0 * NEXP + s_ * 512
                bi = ci % NCB
                ci += 1
                P.dma("sp", lambda e: e.dma_start(out=cin[bi][:], in_=src_d[r0:r0 + 512, :].rearrange("(p i) d -> p i d", i=4)),
                      writes=["cin%d" % bi])
                if ci % 2 == 0:
                    P.op("act", lambda e: e.activation(out=cout[bi][:], in_=cin[bi][:], func=AF.Copy),
                         reads=["cin%d" % bi], writes=["cout%d" % bi])
                else:
                    P.op("dve", lambda e: e.tensor_copy(out=cout[bi][:], in_=cin[bi][:]),
                         reads=["cin%d" % bi], writes=["cout%d" % bi])
                P.dma("sp", lambda e: e.dma_start(out=uv_d[r0:r0 + 512, tb * D:(tb + 1) * D].rearrange("(p i) d -> p i d", i=4),
                                                  in_=cout[bi][:]), reads=["cout%d" % bi], writes=[("uv_d", l0, tb, s_)])
                yield

    for l in range(L):
        hsrc = x_d if l == 0 else h_d
        va_st = ExitStack()
        Vaug = sb(va_st, "Vaug", [128, NT, NH, 65], BF16)
        P.op("pool", lambda e: e.memset(Vaug[:], 1.0), writes=["Vaug"])
        if "1" in phases:
          with ExitStack() as phA:
            fT = sb(phA, "fT", [8, S], F32)
            with ExitStack() as ph:
                Wbf = sb(ph, "Wbf", [128, 8, 2056], BF16)
                wst = sb(ph, "wst", [128, 8, 512], F32)
                g1T = sb(ph, "g1T", [128, 8], F32)
                hbuf = [sb(ph, "hbuf%d" % i, [128, D], F32) for i in range(2)]
                junk = sb(ph, "junk", [128, D], F32)
                xs = [sb(ph, "xs%d" % i, [128, D], BF16) for i in range(2)]
                xnT = [sb(ph, "xnT%d" % i, [128, 8, 512], BF16) for i in range(2)]
                ust = [sb(ph, "ust%d" % i, [128, 512], BF16) for i in range(3)]
                ss1 = sb(ph, "ss1", [128, 4], F32)
                bfb = sb(ph, "bfb", [8, 1], F32)

                P.dma("sp", lambda e: e.dma_start(out=g1T[:], in_=g1T_d[l]), writes=["g1T"])
                P.dma("sp", lambda e: e.dma_start(out=bfb[:], in_=bf_d[l]), writes=["bfb"])
                load_w_bf(ph, "Wbf", Wbf, w_in_d[l], 8, 2056, gainT=g1T, gkey="g1T", wst=wst)
                WK = wkeys("Wbf", 8, 2056)
                ust_i = [0]
                pbi = [0]

                def nextpb():
                    pbi[0] = (pbi[0] + 1) % 6
                    return pbi[0]

                def pre(tg):
                    xg = xnT[tg % 2]
                    xk = "xnT%d" % (tg % 2)
                    for ti in range(4):
                        t = tg * 4 + ti
                        ht = hbuf[t % 2]
                        hk = "hbuf%d" % (t % 2)
                        xsb = xs[t % 2]
                        xsk = "xs%d" % (t % 2)
                        P.dma("sp", lambda e: e.dma_start(out=ht[:], in_=hsrc[t * 128:(t + 1) * 128, :]), writes=[hk])
                        P.op("act", lambda e: e.activation(out=junk[:], in_=ht[:], func=AF.Square,
                                                           accum_out=ss1[:, 0:1]), reads=[hk], writes=["junk", "ss1a"])
                        rstd_from_ss(ss1[:, 0:1], ss1[:, 1:2], D, "ss1a", "ss1b")
                        P.op("act", lambda e: e.activation(out=xsb[:], in_=ht[:], func=AF.Copy, scale=ss1[:, 1:2]),
                             reads=[hk, "ss1b"], writes=[xsk])
                        for c in range(8):
                            P.op("pe", lambda e: e.transpose(out=pT[:, c * 128:(c + 1) * 128],
                                                             in_=xsb[:, c * 128:(c + 1) * 128], identity=ident[:]),
                                 reads=[xsk, "ident"], writes=[("pT", c)])
                        P.op("dve", lambda e: e.tensor_copy(out=xg[:, :, ti * 128:(ti + 1) * 128],
                                                            in_=pT[:].rearrange("p (c t) -> p c t", c=8)),
                             reads=[("pT", c) for c in range(8)], writes=[(xk, ti)])
                def mm(tg):
                    xg = xnT[tg % 2]
                    xk = "xnT%d" % (tg % 2)
                    XK = [(xk, ti) for ti in range(4)]
                    tsl = slice(tg * 512, (tg + 1) * 512)
                    for m in range(4):
                        b = nextpb()
                        for c in range(8):
                            P.op("pe", lambda e: e.matmul(pb[b][:, :], lhsT=Wbf[:, c, m * 128:(m + 1) * 128],
                                                          rhs=xg[:, c, :], start=(c == 0), stop=(c == 7)),
                                 reads=XK + [("Wbf", c, 0)], writes=[("pb", b)])
                        u = ust_i[0] = (ust_i[0] + 1) % 3
                        evac(ust[u][:, :], pb[b][:, :], [("pb", b)], ["ust%d" % u])
                        P.dma("sp", lambda e: e.dma_start(out=uT_d[m * 128:(m + 1) * 128, tsl], in_=ust[u][:, :]),
                              reads=["ust%d" % u], writes=[("uT_d", m)])
                    for hd in range(NH):
                        for nm, off, dst in (("q", 512, qT_d), ("k", 1024, kT_d)):
                            b = nextpb()
                            c0 = off + 64 * hd
                            for c in range(8):
                                P.op("pe", lambda e: e.matmul(pb[b][0:64, :], lhsT=Wbf[:, c, c0:c0 + 64],
                                                              rhs=xg[:, c, :], start=(c == 0), stop=(c == 7)),
                                     reads=XK + [("Wbf", c, (c0 // 512) * 512)], writes=[("pb", b)])
                            u = ust_i[0] = (ust_i[0] + 1) % 3
                            evac(ust[u][0:64, :], pb[b][0:64, :], [("pb", b)], ["ust%d" % u])
                            P.dma("sp", lambda e: e.dma_start(out=dst[hd, 0:64, tsl], in_=ust[u][0:64, :]),
                                  reads=["ust%d" % u], writes=[(nm + "T_d", hd)])
                    b = nextpb()
                    for c in range(8):
                        P.op("pe", lambda e: e.matmul(pb[b][0:8, :], lhsT=Wbf[:, c, 2048:2056], rhs=xg[:, c, :],
                                                      start=(c == 0), stop=(c == 7)),
                             reads=XK + [("Wbf", c, 2048)], writes=[("pb", b)])
                    P.op("act", lambda e: e.activation(out=fT[:, tsl], in_=pb[b][0:8, :], func=AF.Identity,
                                                       bias=bfb[:, 0:1]), reads=[("pb", b), "bfb"], writes=["fT"])
                    for ti in range(4):
                        t = tg * 4 + ti
                        b = nextpb()
                        for c in range(8):
                            P.op("pe", lambda e: e.matmul(pb[b][:, :], lhsT=xg[:, c, ti * 128:(ti + 1) * 128],
                                                          rhs=Wbf[:, c, 1536:2048], start=(c == 0), stop=(c == 7)),
                                 reads=XK + [("Wbf", c, 1536)], writes=[("pb", b)])
                        evac(Vaug[:, t, :, 0:64], pb[b][:, :].rearrange("p (h d) -> p h d", h=NH),
                             [("pb", b)], [("Vaug", t)])
                pre(0)
                for tg in range(NG):
                    if tg + 1 < NG:
                        pre(tg + 1)
                    mm(tg)
                P.barrier()
            with ExitStack() as ph:
                ft2 = sb(ph, "ft2", [8, S], F32)
                cpb = sb(ph, "cpb", [8, 3, S], BF16)
                cnb = sb(ph, "cnb", [8, 3, S], BF16)
                oneb = sb(ph, "oneb", [8, S], BF16)
                P.op("act", lambda e: e.activation(out=ft2[:], in_=fT[:], func=AF.Exp, scale=-1.0),
                     reads=["fT"], writes=["ft2"])
                P.op("act", lambda e: e.activation(out=ft2[:], in_=ft2[:], func=AF.Ln, bias=one_t[0:8, 0:1]),
                     reads=["ft2", "one_t"], writes=["ft2"])
                P.op("dve", lambda e: e.tensor_scalar(out=ft2[:], in0=ft2[:], scalar1=-8.0, scalar2=None, op0=ALU.mult),
                     reads=["ft2"], writes=["ft2"])
                P.op("dve", lambda e: e.memset(fT[:], 1.0), reads=[], writes=["fT"])
                P.op("dve", lambda e: e.tensor_tensor_scan(ft2[:], fT[:], ft2[:], 0.0, ALU.mult, ALU.add),
                     reads=["fT", "ft2"], writes=["ft2"])
                for i in range(3):
                    P.op("dve", lambda e: e.tensor_copy(out=cpb[:, i, :], in_=ft2[:]), reads=["ft2"], writes=[("cpb", i)])
                    if i < 2:
                        P.op("dve", lambda e: e.tensor_tensor(out=ft2[:], in0=ft2[:], in1=cpb[:, i, :], op=ALU.subtract),
                             reads=["ft2", ("cpb", i)], writes=["ft2"])
                    P.op("dve", lambda e: e.tensor_scalar(out=cnb[:, i, :], in0=cpb[:, i, :], scalar1=-1.0, scalar2=None,
                                                          op0=ALU.mult), reads=[("cpb", i)], writes=[("cnb", i)])
                P.op("dve", lambda e: e.memset(oneb[:], 1.0), writes=["oneb"])
                for i in range(3):
                    P.dma("sp", lambda e: e.dma_start(out=qT_d[:, 64 + i, :], in_=cpb[:, i, :]),
                          reads=[("cpb", i)], writes=[("qT_dc", i)])
                    P.dma("sp", lambda e: e.dma_start(out=qT_d[:, 67 + i, :], in_=oneb[:]),
                          reads=["oneb"], writes=[("qT_dc", 3 + i)])
                    P.dma("sp", lambda e: e.dma_start(out=kT_d[:, 64 + i, :], in_=oneb[:]),
                          reads=["oneb"], writes=[("kT_dc", i)])
                    P.dma("sp", lambda e: e.dma_start(out=kT_d[:, 67 + i, :], in_=cnb[:, i, :]),
                          reads=[("cnb", i)], writes=[("kT_dc", 3 + i)])
                P.barrier()

        if "2" in phases:
            with ExitStack() as ph:
                uTs = sb(ph, "uTs", [128, 4, S], BF16)
                XA = sb(ph, "XA", [128, 2, S], F32)
                XB = sb(ph, "XB", [128, 2, S], F32)
                Sb = sb(ph, "Sb", [128, 2, S], BF16)
                yst = sb(ph, "yst", [32, S], F32)
                LBr = sb(ph, "LBr", [128, 16, 128], BF16)
                LBi = sb(ph, "LBi", [128, 16, 128], BF16)
                Cre = sb(ph, "Cre", [128, 16, 32], BF16)
                Cim = sb(ph, "Cim", [128, 16, 32], BF16)
                Dm = sb(ph, "Dm", [128, 16, 32], BF16)
                cst = sb(ph, "cst", [128, 16, 32], F32)
                Apr = sb(ph, "Apr", [128, LOGS, 16], F32)
                Api = sb(ph, "Api", [128, LOGS, 16], F32)
                Apn = sb(ph, "Apn", [128, LOGS, 16], F32)
                pr = {nm: sb(ph, "pr_" + nm, [128, 64], F32) for nm in
                      ("lr", "li", "br", "bi", "t0", "t1", "t2", "t3", "ar", "ai", "zr", "zi", "bbr", "bbi")}
                dtb = sb(ph, "dtb", [128, 1], F32)
                BL, LBL = 8, 3
                NBK = S // BL
                EA = sb(ph, "EA", [128, 2, NBK], F32)
                EB = sb(ph, "EB", [128, 2, NBK], F32)
                PWr = sb(ph, "PWr", [128, 16, BL], F32)
                PWi = sb(ph, "PWi", [128, 16, BL], F32)
                pwt0 = sb(ph, "pwt0", [128, 16, BL // 2], F32)
                pwt1 = sb(ph, "pwt1", [128, 16, BL // 2], F32)
                pri = sb(ph, "pri", [128, 64], I32)

                P.dma("sp", lambda e: e.dma_start(out=uTs[:], in_=uT_d.rearrange("(c p) t -> p c t", p=128)),
                      writes=["uTs"])
                for nm, src, dst, sc in (("Cre", cT_re_d, Cre, 1.0), ("Cim", cT_im_d, Cim, -1.0), ("Dm", dmat_d, Dm, 1.0)):
                    P.dma("sp", lambda e: e.dma_start(out=cst[:], in_=src[l]), writes=["cst"])
                    P.op("act", lambda e: e.activation(out=dst[:], in_=cst[:], func=AF.Copy, scale=sc),
                         reads=["cst"], writes=[nm])

                def dv(fn, reads, writes):
                    P.op("dve", fn, reads=reads, writes=writes)

                def abar(lr, li, dt_ap, ar, ai, y, fr, tf, ti, sn, k):
                    if dt_ap.shape[-1] == 1:
                        dv(lambda e: e.tensor_scalar(out=y, in0=li, scalar1=dt_ap, scalar2=1.0 / (2 * math.pi),
                                                     op0=ALU.mult, op1=ALU.mult), [k + "li", k + "dt"], [k + "t0"])
                        dv(lambda e: e.tensor_scalar(out=ar, in0=lr, scalar1=dt_ap, scalar2=None, op0=ALU.mult),
                           [k + "lr", k + "dt"], [k + "ar"])
                    else:
                        dv(lambda e: e.tensor_tensor(out=y, in0=li, in1=dt_ap, op=ALU.mult), [k + "li", k + "dt"], [k + "t0"])
                        dv(lambda e: e.tensor_scalar(out=y, in0=y, scalar1=1.0 / (2 * math.pi), scalar2=None,
                                                     op0=ALU.mult), [k + "t0"], [k + "t0"])
                        dv(lambda e: e.tensor_tensor(out=ar, in0=lr, in1=dt_ap, op=ALU.mult), [k + "lr", k + "dt"], [k + "ar"])
                    P.op("act", lambda e: e.activation(out=ar, in_=ar, func=AF.Exp), reads=[k + "ar"], writes=[k + "ar"])
                    for shift, dst, dk in ((0.0, sn, "sn"), (0.25, ai, "ai")):
                        dv(lambda e: e.tensor_scalar(out=fr, in0=y, scalar1=shift, scalar2=None, op0=ALU.add),
                           [k + "t0"], [k + "t1"])
                        dv(lambda e: e.tensor_copy(out=ti, in_=fr), [k + "t1"], [k + "ti"])
                        dv(lambda e: e.tensor_copy(out=tf, in_=ti), [k + "ti"], [k + "t2"])
                        dv(lambda e: e.tensor_tensor(out=fr, in0=fr, in1=tf, op=ALU.subtract), [k + "t1", k + "t2"], [k + "t1"])
                        dv(lambda e: e.tensor_scalar(out=tf, in0=fr, scalar1=0.5, scalar2=None, op0=ALU.is_gt),
                           [k + "t1"], [k + "t2"])
                        dv(lambda e: e.tensor_tensor(out=fr, in0=fr, in1=tf, op=ALU.subtract), [k + "t1", k + "t2"], [k + "t1"])
                        dv(lambda e: e.tensor_scalar(out=tf, in0=fr, scalar1=-0.5, scalar2=None, op0=ALU.is_lt),
                           [k + "t1"], [k + "t2"])
                        dv(lambda e: e.tensor_tensor(out=fr, in0=fr, in1=tf, op=ALU.add), [k + "t1", k + "t2"], [k + "t1"])
                        P.op("act", lambda e: e.activation(out=dst, in_=fr, func=AF.Sin, scale=2 * math.pi),
                             reads=[k + "t1"], writes=[k + dk])
                    dv(lambda e: e.tensor_tensor(out=fr, in0=ai, in1=ar, op=ALU.mult), [k + "ai", k + "ar", k + "t1"], [k + "t1"])
                    dv(lambda e: e.tensor_tensor(out=ai, in0=sn, in1=ar, op=ALU.mult), [k + "t3", k + "ar", k + "ai"], [k + "ai"])
                    dv(lambda e: e.tensor_copy(out=ar, in_=fr), [k + "t1", k + "ar"], [k + "ar"])

                P.op("dve", lambda e: e.memset(LBr[:], 0.0), writes=["LBr"])
                P.op("dve", lambda e: e.memset(LBi[:], 0.0), writes=["LBi"])
                for q in range(4):
                    kq = "B_"
                    P.dma("sp", lambda e: e.dma_start(out=pr["lr"][:], in_=lamB_re_d[l, q]), writes=[kq + "lr"])
                    P.dma("sp", lambda e: e.dma_start(out=pr["li"][:], in_=lamB_im_d[l, q]), writes=[kq + "li"])
                    P.dma("sp", lambda e: e.dma_start(out=pr["br"][:], in_=bT_re_d[l, q]), writes=[kq + "br"])
                    P.dma("sp", lambda e: e.dma_start(out=pr["bi"][:], in_=bT_im_d[l, q]), writes=[kq + "bi"])
                    P.dma("sp", lambda e: e.dma_start(out=dtb[:], in_=dtB_d[l, q]), writes=[kq + "dtraw"])
                    P.op("act", lambda e: e.activation(out=dtb[:], in_=dtb[:], func=AF.Exp), reads=[kq + "dtraw"],
                         writes=[kq + "dt"])
                    abar(pr["lr"][:], pr["li"][:], dtb[:, 0:1], pr["ar"][:], pr["ai"][:], pr["t0"][:], pr["t1"][:],
                         pr["t2"][:], pri[:], pr["t3"][:], kq)
                    lr, li, ar, ai = pr["lr"][:], pr["li"][:], pr["ar"][:], pr["ai"][:]
                    t0, t1, t2, t3 = pr["t0"][:], pr["t1"][:], pr["t2"][:], pr["t3"][:]
                    zr, zi = pr["zr"][:], pr["zi"][:]
                    K = lambda s: kq + s
                    dv(lambda e: e.tensor_tensor(out=t0, in0=lr, in1=lr, op=ALU.mult), [K("lr"), K("t0")], [K("t0")])
                    dv(lambda e: e.tensor_tensor(out=t1, in0=li, in1=li, op=ALU.mult), [K("li"), K("t1")], [K("t1")])
                    dv(lambda e: e.tensor_tensor(out=t0, in0=t0, in1=t1, op=ALU.add), [K("t0"), K("t1")], [K("t0")])
                    dv(lambda e: e.reciprocal(out=t0, in_=t0), [K("t0")], [K("t0")])
                    dv(lambda e: e.tensor_scalar(out=t1, in0=ar, scalar1=-1.0, scalar2=None, op0=ALU.add), [K("ar"), K("t1")], [K("t1")])
                    dv(lambda e: e.tensor_tensor(out=zr, in0=t1, in1=lr, op=ALU.mult), [K("t1"), K("lr")], [K("zr")])
                    dv(lambda e: e.tensor_tensor(out=t2, in0=ai, in1=li, op=ALU.mult), [K("ai"), K("li")], [K("t2")])
                    dv(lambda e: e.tensor_tensor(out=zr, in0=zr, in1=t2, op=ALU.add), [K("zr"), K("t2")], [K("zr")])
                    dv(lambda e: e.tensor_tensor(out=zr, in0=zr, in1=t0, op=ALU.mult), [K("zr"), K("t0")], [K("zr")])
                    dv(lambda e: e.tensor_tensor(out=zi, in0=ai, in1=lr, op=ALU.mult), [K("ai"), K("lr")], [K("zi")])
                    dv(lambda e: e.tensor_tensor(out=t2, in0=t1, in1=li, op=ALU.mult), [K("t1"), K("li")], [K("t2")])
                    dv(lambda e: e.tensor_tensor(out=zi, in0=zi, in1=t2, op=ALU.subtract), [K("zi"), K("t2")], [K("zi")])
                    dv(lambda e: e.tensor_tensor(out=zi, in0=zi, in1=t0, op=ALU.mult), [K("zi"), K("t0")], [K("zi")])
                    br, bi, bbr, bbi = pr["br"][:], pr["bi"][:], pr["bbr"][:], pr["bbi"][:]
                    dv(lambda e: e.tensor_tensor(out=bbr, in0=zr, in1=br, op=ALU.mult), [K("zr"), K("br")], [K("bbr")])
                    dv(lambda e: e.tensor_tensor(out=t2, in0=zi, in1=bi, op=ALU.mult), [K("zi"), K("bi")], [K("t2")])
                    dv(lambda e: e.tensor_tensor(out=bbr, in0=bbr, in1=t2, op=ALU.subtract), [K("bbr"), K("t2")], [K("bbr")])
                    dv(lambda e: e.tensor_tensor(out=bbi, in0=zr, in1=bi, op=ALU.mult), [K("zr"), K("bi")], [K("bbi")])
                    dv(lambda e: e.tensor_tensor(out=t2, in0=zi, in1=br, op=ALU.mult), [K("zi"), K("br")], [K("t2")])
                    dv(lambda e: e.tensor_tensor(out=bbi, in0=bbi, in1=t2, op=ALU.add), [K("bbi"), K("t2")], [K("bbi")])
                    for i in range(4):
                        j = 4 * q + i
                        for g2 in range(2):
                            gi = 2 * i + g2
                            dv(lambda e: e.tensor_scalar(out=LBr[:, j, 64 * g2:64 * g2 + 64], in0=bbr,
                                                         scalar1=gmask[:, gi:gi + 1], scalar2=None, op0=ALU.mult),
                               [K("bbr"), "gmask", "LBr"], [("LBr", j, g2)])
                            dv(lambda e: e.tensor_scalar(out=LBi[:, j, 64 * g2:64 * g2 + 64], in0=bbi,
                                                         scalar1=gmask[:, gi:gi + 1], scalar2=None, op0=ALU.mult),
                               [K("bbi"), "gmask", "LBi"], [("LBi", j, g2)])
                aT = {nm: sb(ph, "aT_" + nm, [128, 16], F32) for nm in ("lr", "li", "dt", "ar", "ai", "t0", "t1", "t2", "t3")}
                aTi = sb(ph, "aTi", [128, 16], I32)
                P.dma("sp", lambda e: e.dma_start(out=aT["lr"][:], in_=lamT_re_d[l]), writes=["T_lr"])
                P.dma("sp", lambda e: e.dma_start(out=aT["li"][:], in_=lamT_im_d[l]), writes=["T_li"])
                P.dma("sp", lambda e: e.dma_start(out=aT["dt"][:], in_=dtT_d[l]), writes=["T_dtraw"])
                P.op("act", lambda e: e.activation(out=aT["dt"][:], in_=aT["dt"][:], func=AF.Exp), reads=["T_dtraw"],
                     writes=["T_dt"])
                abar(aT["lr"][:], aT["li"][:], aT["dt"][:], aT["ar"][:], aT["ai"][:], aT["t0"][:], aT["t1"][:],
                     aT["t2"][:], aTi[:], aT["t3"][:], "T_")
                dv(lambda e: e.tensor_copy(out=Apr[:, 0, :], in_=aT["ar"][:]), ["T_ar"], [("Apr", 0)])
                dv(lambda e: e.tensor_copy(out=Api[:, 0, :], in_=aT["ai"][:]), ["T_ai"], [("Api", 0)])
                for k in range(1, LOGS):
                    a_r, a_i = Apr[:, k - 1, :], Api[:, k - 1, :]
                    t0, t1 = aT["t0"][:], aT["t1"][:]
                    dv(lambda e: e.tensor_tensor(out=t0, in0=a_r, in1=a_r, op=ALU.mult), [("Apr", k - 1), "T_t0"], ["T_t0"])
                    dv(lambda e: e.tensor_tensor(out=t1, in0=a_i, in1=a_i, op=ALU.mult), [("Api", k - 1), "T_t1"], ["T_t1"])
                    dv(lambda e: e.tensor_tensor(out=Apr[:, k, :], in0=t0, in1=t1, op=ALU.subtract), ["T_t0", "T_t1"], [("Apr", k)])
                    dv(lambda e: e.tensor_tensor(out=t0, in0=a_r, in1=a_i, op=ALU.mult), [("Apr", k - 1), ("Api", k - 1), "T_t0"], ["T_t0"])
                    dv(lambda e: e.tensor_scalar(out=Api[:, k, :], in0=t0, scalar1=2.0, scalar2=None, op0=ALU.mult), ["T_t0"], [("Api", k)])
                dv(lambda e: e.tensor_scalar(out=Apn[:], in0=Api[:], scalar1=-1.0, scalar2=None, op0=ALU.mult),
                   [("Api", k) for k in range(LOGS)], ["Apn"])
                APK = [("Apr", k) for k in range(LOGS)] + [("Api", k) for k in range(LOGS)] + ["Apn"]
                dv(lambda e: e.tensor_copy(out=PWr[:, :, 0:1], in_=Apr[:, 0, :].unsqueeze(2)), APK, ["PW"])
                dv(lambda e: e.tensor_copy(out=PWi[:, :, 0:1], in_=Api[:, 0, :].unsqueeze(2)), APK + ["PW"], ["PW"])
                for k in range(LBL):
                    d = 1 << k
                    arb = Apr[:, k, :].unsqueeze(2).to_broadcast([128, 16, d])
                    aib = Api[:, k, :].unsqueeze(2).to_broadcast([128, 16, d])
                    t0, t1 = pwt0[:, :, 0:d], pwt1[:, :, 0:d]
                    dv(lambda e: e.tensor_tensor(out=t0, in0=PWr[:, :, 0:d], in1=arb, op=ALU.mult), APK + ["PW", "pwt0"], ["pwt0"])
                    dv(lambda e: e.tensor_tensor(out=t1, in0=PWi[:, :, 0:d], in1=aib, op=ALU.mult), APK + ["PW", "pwt1"], ["pwt1"])
                    dv(lambda e: e.tensor_tensor(out=PWr[:, :, d:2 * d], in0=t0, in1=t1, op=ALU.subtract), ["pwt0", "pwt1", "PW"], ["PW"])
                    dv(lambda e: e.tensor_tensor(out=t0, in0=PWr[:, :, 0:d], in1=aib, op=ALU.mult), APK + ["PW", "pwt0"], ["pwt0"])
                    dv(lambda e: e.tensor_tensor(out=t1, in0=PWi[:, :, 0:d], in1=arb, op=ALU.mult), APK + ["PW", "pwt1"], ["pwt1"])
                    dv(lambda e: e.tensor_tensor(out=PWi[:, :, d:2 * d], in0=t0, in1=t1, op=ALU.add), ["pwt0", "pwt1", "PW"], ["PW"])

                for j in range(16):
                    q, i = j // 4, j % 4
                    for ri, LB, lk in ((0, LBr, "LBr"), (1, LBi, "LBi")):
                        for n in range(NG):
                            b = 1 + (n + ri) % 4
                            P.op("pe", lambda e: e.matmul(pb[b][:, :], lhsT=LB[:, j, :], rhs=uTs[:, q, n * 512:(n + 1) * 512],
                                                          start=True, stop=True),
                                 reads=["uTs", (lk, j, 0), (lk, j, 1), lk], writes=[("pb", b)])
                            P.op("act", lambda e: e.activation(out=XA[:, ri, n * 512:(n + 1) * 512], in_=pb[b][:, :], func=AF.Copy),
                                 reads=[("pb", b)], writes=[("XA", ri)])
                    cur, curk, oth, othk = XA, "XA", XB, "XB"
                    v4 = lambda T, ri: T[:, ri, :].rearrange("p (b j) -> p b j", j=BL)
                    for k in range(LBL):
                        d = 1 << k
                        new, newk = oth, othk
                        ar_k, ai_k, an_k = Apr[:, k, j:j + 1], Api[:, k, j:j + 1], Apn[:, k, j:j + 1]
                        P.op("dve", lambda e: e.tensor_copy(out=new[:].rearrange("p r (b j) -> p r b j", j=BL)[:, :, :, 0:d],
                                                            in_=cur[:].rearrange("p r (b j) -> p r b j", j=BL)[:, :, :, 0:d]),
                             reads=[(curk, 0), (curk, 1)], writes=[(newk, 0), (newk, 1)])
                        P.op("dve", lambda e: e.scalar_tensor_tensor(out=v4(new, 0)[:, :, d:BL], in0=v4(cur, 0)[:, :, 0:BL - d], scalar=ar_k,
                                                                     in1=v4(cur, 0)[:, :, d:BL], op0=ALU.mult, op1=ALU.add),
                             reads=[(curk, 0)] + APK, writes=[(newk, 0)])
                        P.op("dve", lambda e: e.scalar_tensor_tensor(out=v4(new, 1)[:, :, d:BL], in0=v4(cur, 1)[:, :, 0:BL - d], scalar=ar_k,
                                                                     in1=v4(cur, 1)[:, :, d:BL], op0=ALU.mult, op1=ALU.add),
                             reads=[(curk, 1)] + APK, writes=[(newk, 1)])
                        P.op("dve", lambda e: e.scalar_tensor_tensor(out=v4(new, 0)[:, :, d:BL], in0=v4(cur, 1)[:, :, 0:BL - d], scalar=an_k,
                                                                     in1=v4(new, 0)[:, :, d:BL], op0=ALU.mult, op1=ALU.add),
                             reads=[(curk, 1), (newk, 0)] + APK, writes=[(newk, 0)])
                        P.op("dve", lambda e: e.scalar_tensor_tensor(out=v4(new, 1)[:, :, d:BL], in0=v4(cur, 0)[:, :, 0:BL - d], scalar=ai_k,
                                                                     in1=v4(new, 1)[:, :, d:BL], op0=ALU.mult, op1=ALU.add),
                             reads=[(curk, 0), (newk, 1)] + APK, writes=[(newk, 1)])
                        cur, curk, oth, othk = new, newk, cur, curk
                    ecur, ecurk, eoth, eothk = EA, "EA", EB, "EB"
                    P.op("dve", lambda e: e.tensor_copy(out=ecur[:], in_=cur[:].rearrange("p r (b j) -> p r b j", j=BL)[:, :, :, BL - 1]),
                         reads=[(curk, 0), (curk, 1)], writes=[(ecurk, 0), (ecurk, 1)])
                    for k in range(LOGS - LBL):
                        d = 1 << k
                        kk = LBL + k
                        ar_k, ai_k, an_k = Apr[:, kk, j:j + 1], Api[:, kk, j:j + 1], Apn[:, kk, j:j + 1]
                        P.op("dve", lambda e: e.tensor_copy(out=eoth[:, :, 0:d], in_=ecur[:, :, 0:d]),
                             reads=[(ecurk, 0), (ecurk, 1)], writes=[(eothk, 0), (eothk, 1)])
                        P.op("dve", lambda e: e.scalar_tensor_tensor(out=eoth[:, 0, d:NBK], in0=ecur[:, 0, 0:NBK - d], scalar=ar_k,
                                                                     in1=ecur[:, 0, d:NBK], op0=ALU.mult, op1=ALU.add),
                             reads=[(ecurk, 0)] + APK, writes=[(eothk, 0)])
                        P.op("dve", lambda e: e.scalar_tensor_tensor(out=eoth[:, 1, d:NBK], in0=ecur[:, 1, 0:NBK - d], scalar=ar_k,
                                                                     in1=ecur[:, 1, d:NBK], op0=ALU.mult, op1=ALU.add),
                             reads=[(ecurk, 1)] + APK, writes=[(eothk, 1)])
                        P.op("dve", lambda e: e.scalar_tensor_tensor(out=eoth[:, 0, d:NBK], in0=ecur[:, 1, 0:NBK - d], scalar=an_k,
                                                                     in1=eoth[:, 0, d:NBK], op0=ALU.mult, op1=ALU.add),
                             reads=[(ecurk, 1), (eothk, 0)] + APK, writes=[(eothk, 0)])
                        P.op("dve", lambda e: e.scalar_tensor_tensor(out=eoth[:, 1, d:NBK], in0=ecur[:, 0, 0:NBK - d], scalar=ai_k,
                                                                     in1=eoth[:, 1, d:NBK], op0=ALU.mult, op1=ALU.add),
                             reads=[(ecurk, 0), (eothk, 1)] + APK, writes=[(eothk, 1)])
                        ecur, ecurk, eoth, eothk = eoth, eothk, ecur, ecurk
                    NB1 = NBK - 1
                    bc_pw = lambda T: T[:, j, :].unsqueeze(1).to_broadcast([128, NB1, BL])
                    bc_c = lambda ri: ecur[:, ri, 0:NB1].unsqueeze(2).to_broadcast([128, NB1, BL])
                    blk1 = lambda T, ri: T[:, ri, BL:S].rearrange("p (b j) -> p b j", j=BL)
                    ECK = [(ecurk, 0), (ecurk, 1)]
                    P.op("act", lambda e: e.activation(out=Sb[:, :, 0:BL], in_=cur[:, :, 0:BL], func=AF.Copy),
                         reads=[(curk, 0), (curk, 1)], writes=[("Sb", 0, "h"), ("Sb", 1, "h")])
                    for eng, ri, c1, c2, lastop in (("dve", 0, 0, 1, ALU.subtract), ("pool", 1, 1, 0, ALU.add)):
                        tmp = blk1(oth, ri)
                        P.op(eng, lambda e: e.tensor_tensor(out=tmp, in0=bc_pw(PWr), in1=bc_c(c1), op=ALU.mult),
                             reads=ECK + ["PW"], writes=[(othk, ri)])
                        P.op(eng, lambda e: e.tensor_tensor(out=blk1(cur, ri), in0=blk1(cur, ri), in1=tmp, op=ALU.add),
                             reads=[(othk, ri), (curk, ri)], writes=[(curk, ri)])
                        P.op(eng, lambda e: e.tensor_tensor(out=tmp, in0=bc_pw(PWi), in1=bc_c(c2), op=ALU.mult),
                             reads=ECK + ["PW", (othk, ri)], writes=[(othk, ri)])
                        P.op(eng, lambda e: e.tensor_tensor(out=blk1(Sb, ri), in0=blk1(cur, ri), in1=tmp, op=lastop),
                             reads=[(othk, ri), (curk, ri)], writes=[("Sb", ri)])
                    SK = [("Sb", 0), ("Sb", 1), ("Sb", 0, "h"), ("Sb", 1, "h")]
                    for n in range(NG):
                        b = 5 + n % 2
                        sl = slice(n * 512, (n + 1) * 512)
                        P.op("pe", lambda e: e.matmul(pb[b][0:32, :], lhsT=Cre[:, j, :], rhs=Sb[:, 0, sl], start=True, stop=False),
                             reads=SK + ["Cre"], writes=[("pb", b)])
                        P.op("pe", lambda e: e.matmul(pb[b][0:32, :], lhsT=Cim[:, j, :], rhs=Sb[:, 1, sl], start=False, stop=False),
                             reads=SK + ["Cim"], writes=[("pb", b)])
                        P.op("pe", lambda e: e.matmul(pb[b][0:32, :], lhsT=Dm[:, j, :], rhs=uTs[:, q, sl], start=False, stop=True),
                             reads=["uTs", "Dm"], writes=[("pb", b)])
                        P.op("act", lambda e: e.activation(out=yst[:, sl], in_=pb[b][0:32, :], func=AF.Copy),
                             reads=[("pb", b)], writes=["yst"])
                    P.dma("sp", lambda e: e.dma_start(out=ysT_d[32 * j:32 * j + 32, :], in_=yst[:, :]),
                          reads=["yst"], writes=[("ysT_d", j)])
                P.barrier()

        ya_st = ExitStack()
        yatt = sb(ya_st, "yatt", [128, NT, 512], BF16)
        if "3" in phases:
            with ExitStack() as ph:
                qa = [sb(ph, "qa%d" % i, [70, S], BF16) for i in range(2)]
                ka = [sb(ph, "ka%d" % i, [70, S], BF16) for i in range(2)]
                PTb = [sb(ph, "PT%d" % i, [128, 512], BF16) for i in range(3)]
                rl = sb(ph, "rl", [128, 4], F32)
                cin = [sb(ph, "cin%d" % i, [128, 4, D], F32) for i in range(2)]
                cout = [sb(ph, "cout%d" % i, [128, 4, D], BF16) for i in range(2)]
                p0g = p0_gen(l, cin, cout) if "5" in phases else iter(())
                p0_every = max(1, (NH * NG * (2 * NG + 2)) // 70)
                items = [(hd, G, j) for hd in range(NH) for G in range(NG) for j in range(4 * G + 4)]
                SB_ = [0, 1, 6]

                def st1(n):
                    hd, G, j = items[n]
                    qh, kh = qa[hd % 2], ka[hd % 2]
                    qk_, kk_ = "qa%d" % (hd % 2), "ka%d" % (hd % 2)
                    if G == 0 and j == 0:
                        for h2 in ([0, 1] if hd == 0 else [hd + 1]):
                            if h2 < NH:
                                q2, k2 = qa[h2 % 2], ka[h2 % 2]
                                P.dma("sp", lambda e: e.dma_start(out=q2[:], in_=qT_d[h2]), writes=["qa%d" % (h2 % 2)])
                                P.dma("sp", lambda e: e.dma_start(out=k2[:], in_=kT_d[h2]), writes=["ka%d" % (h2 % 2)])
                    i0 = max(0, j - 4 * G)
                    diag = j >= 4 * G
                    c0 = i0 * 128
                    sp_b = SB_[n % 3]
                    PT, ptk = PTb[n % 3], "PT%d" % (n % 3)
                    P.op("pe", lambda e: e.matmul(pb[sp_b][:, c0:512], lhsT=kh[:, j * 128:(j + 1) * 128],
                                                  rhs=qh[:, G * 512 + c0:(G + 1) * 512], start=True, stop=not diag),
                         reads=[qk_, kk_], writes=[("pb", sp_b)])
                    if diag:
                        P.op("pe", lambda e: e.matmul(pb[sp_b][:, c0:c0 + 128], lhsT=ident[:], rhs=negm[:],
                                                      start=False, stop=True),
                             reads=["ident", "negm"], writes=[("pb", sp_b)])
                    P.op("act", lambda e: e.activation(out=PT[:, c0:512], in_=pb[sp_b][:, c0:512], func=AF.Exp,
                                                       scale=0.125), reads=[("pb", sp_b)], writes=[ptk])

                def st2(n):
                    hd, G, j = items[n]
                    i0 = max(0, j - 4 * G)
                    PT, ptk = PTb[n % 3], "PT%d" % (n % 3)
                    for i in range(i0, 4):
                        P.op("pe", lambda e: e.matmul(pb[2 + i][:, 0:65], lhsT=PT[:, i * 128:(i + 1) * 128],
                                                      rhs=Vaug[:, j, hd, :], start=(j == 0), stop=(j == 4 * G + i)),
                             reads=[ptk, ("Vaug", j), "Vaug"], writes=[("pb", 2 + i)])
                    if j == 4 * G + 3:
                        for i in range(4):
                            t = 4 * G + i
                            P.op("dve", lambda e: e.reciprocal(out=rl[:, i:i + 1], in_=pb[2 + i][:, 64:65]),
                                 reads=[("pb", 2 + i)], writes=[("rl", i)])
                            P.op("dve", lambda e: e.tensor_scalar(out=yatt[:, t, hd * 64:(hd + 1) * 64], in0=pb[2 + i][:, 0:64],
                                                                  scalar1=rl[:, i:i + 1], scalar2=None, op0=ALU.mult),
                                 reads=[("pb", 2 + i), ("rl", i)], writes=[("yatt", t, hd)])

                for n in range(len(items)):
                    st1(n)
                    if n >= 1:
                        st2(n - 1)
                    if (n + 1) % p0_every == 0:
                        next(p0g, None)
                st2(len(items) - 1)
                for _ in p0g:
                    pass
                if dbg:
                    for t in range(NT):
                        P.dma("sp", lambda e: e.dma_start(out=ya_d[t * 128:(t + 1) * 128, :], in_=yatt[:, t, :]),
                              reads=[("yatt", t, hd) for hd in range(NH)], writes=[("ya_d", t)])
                P.barrier()

        if "4" in phases:
            with ExitStack() as ph:
                wst = sb(ph, "wst", [128, 8, 512], F32)
                Wg = sb(ph, "Wg", [128, 4, 512], BF16)
                Wo = sb(ph, "Wo", [128, 8, D], BF16)
                gsaT = sb(ph, "gsaT", [128, 8], F32)
                bgl = sb(ph, "bgl", [128, 4], F32)
                ys = sb(ph, "ys", [128, 4, 512], F32)
                gf = sb(ph, "gf", [128, 4, 512], F32)
                gb = sb(ph, "gb", [128, 4, 512], BF16)
                sg = [sb(ph, "sg%d" % i, [128, 512], F32) for i in range(2)]
                ob = sb(ph, "ob", [128, 4, 512], BF16)
                osq = sb(ph, "osq", [128, 4, 512], BF16)
                st4 = sb(ph, "st4", [128, 8], F32)
                junkb = sb(ph, "junkb", [128, 512], BF16)
                yan = sb(ph, "yan", [128, 512], BF16)
                yaT = sb(ph, "yaT", [128, 4, 128], BF16)
                hb4 = [sb(ph, "hb4_%d" % i, [128, D], F32) for i in range(2)]
                P.dma("sp", lambda e: e.dma_start(out=gsaT[:], in_=gsaT_d[l]), writes=["gsaT"])
                P.dma("sp", lambda e: e.dma_start(out=bgl[:], in_=bgluT_d[l]), writes=["bgl"])
                load_w_bf(ph, "Wg", Wg, wglu_d[l], 4, 512, wst=wst)
                load_w_bf(ph, "Wo", Wo, w_o_d[l], 8, D, gainT=gsaT, gkey="gsaT", wst=wst)
                WGK = wkeys("Wg", 4, 512)
                WOK = wkeys("Wo", 8, D)
                for tg in range(NG):
                    tsl = slice(tg * 512, (tg + 1) * 512)
                    P.dma("sp", lambda e: e.dma_start(out=ys[:], in_=ysT_d.rearrange("(c p) t -> p c t", p=128)[:, :, tsl]),
                          writes=["ys"])
                    P.op("act", lambda e: e.activation(out=gf[:], in_=ys[:], func=AF.Gelu_apprx_tanh), reads=["ys"], writes=["gf"])
                    P.op("pool", lambda e: e.tensor_copy(out=gb[:], in_=gf[:]), reads=["gf"], writes=["gb"])
                    for m in range(4):
                        b = m % 2
                        for c in range(4):
                            P.op("pe", lambda e: e.matmul(pb[b][:, :], lhsT=Wg[:, c, m * 128:(m + 1) * 128], rhs=gb[:, c, :],
                                                          start=(c == 0), stop=(c == 3)),
                                 reads=["gb", ("Wg", c, 0)], writes=[("pb", b)])
                        P.op("act", lambda e: e.activation(out=sg[b][:], in_=pb[b][:, :], func=AF.Sigmoid, bias=bgl[:, m:m + 1]),
                             reads=[("pb", b), "bgl"], writes=["sg%d" % b])
                        P.op("dve", lambda e: e.tensor_tensor(out=ob[:, m, :], in0=gf[:, m, :], in1=sg[b][:], op=ALU.mult),
                             reads=["gf", "sg%d" % b], writes=[("ob", m)])
                        P.op("pool", lambda e: e.tensor_tensor(out=osq[:, m, :], in0=ob[:, m, :], in1=ob[:, m, :], op=ALU.mult),
                             reads=[("ob", m)], writes=[("osq", m)])
                    OBK = [("ob", m) for m in range(4)]
                    OSK = [("osq", m) for m in range(4)]
                    for ti in range(4):
                        t = tg * 4 + ti
                        csl = slice(ti * 128, (ti + 1) * 128)
                        hb = hb4[t % 2]
                        hk = "hb4_%d" % (t % 2)
                        P.dma("sp", lambda e: e.dma_start(out=hb[:], in_=hsrc[t * 128:(t + 1) * 128, :]), writes=[hk])
                        for m in range(4):
                            P.op("pe", lambda e: e.matmul(pb[6][:, 0:1], lhsT=osq[:, m, csl], rhs=ones_bf[:, 0:1],
                                                          start=(m == 0), stop=(m == 3)),
                                 reads=OSK + ["ones_bf"], writes=[("pb", 6)])
                        for hf in range(2):
                            for m in range(4):
                                P.op("pe", lambda e: e.matmul(pb[2 + hf][:, :], lhsT=ob[:, m, csl], rhs=Wo[:, m, hf * 512:(hf + 1) * 512],
                                                              start=(m == 0), stop=(m == 3)),
                                     reads=OBK + [("Wo", m, hf * 512)], writes=[("pb", 2 + hf)])
                        rstd_from_ss(pb[6][:, 0:1], st4[:, 0:1], 512, ("pb", 6), "st4a")
                        P.op("act", lambda e: e.activation(out=junkb[:], in_=yatt[:, t, :], func=AF.Square, accum_out=st4[:, 1:2]),
                             reads=[("yatt", t, hd) for hd in range(NH)] + [("yatt", t)], writes=["junkb", "st4b"])
                        rstd_from_ss(st4[:, 1:2], st4[:, 2:3], 512, "st4b", "st4c")
                        P.op("act", lambda e: e.activation(out=yan[:], in_=yatt[:, t, :], func=AF.Copy, scale=st4[:, 2:3]),
                             reads=["st4c", ("yatt", t)], writes=["yan"])
                        for c in range(4):
                            P.op("pe", lambda e: e.transpose(out=pT[:, c * 128:(c + 1) * 128], in_=yan[:, c * 128:(c + 1) * 128],
                                                             identity=ident[:]), reads=["yan", "ident"], writes=[("pT", c)])
                        P.op("dve", lambda e: e.tensor_copy(out=yaT[:], in_=pT[:, 0:512].rearrange("p (c t) -> p c t", c=4)),
                             reads=[("pT", c) for c in range(4)], writes=["yaT"])
                        for hf in range(2):
                            for c in range(4):
                                P.op("pe", lambda e: e.matmul(pb[4 + hf][:, :], lhsT=yaT[:, c, :], rhs=Wo[:, 4 + c, hf * 512:(hf + 1) * 512],
                                                              start=(c == 0), stop=(c == 3)),
                                     reads=["yaT", ("Wo", 4 + c, hf * 512)], writes=[("pb", 4 + hf)])
                        for hf in range(2):
                            hs = slice(hf * 512, (hf + 1) * 512)
                            P.op("dve", lambda e: e.scalar_tensor_tensor(out=hb[:, hs], in0=pb[2 + hf][:, :], scalar=st4[:, 0:1],
                                                                         in1=hb[:, hs], op0=ALU.mult, op1=ALU.add),
                                 reads=[("pb", 2 + hf), "st4a", hk], writes=[hk])
                            P.op("dve", lambda e: e.tensor_tensor(out=hb[:, hs], in0=pb[4 + hf][:, :], in1=hb[:, hs], op=ALU.add),
                                 reads=[("pb", 4 + hf), hk], writes=[hk])
                        P.dma("sp", lambda e: e.dma_start(out=h_d[t * 128:(t + 1) * 128, :], in_=hb[:]), reads=[hk],
                              writes=[("h_d", t)])
                P.barrier()
            hsrc = h_d

        ya_st.close()
        va_st.close()
        if "5" in phases:
            final = last_final and (l == L - 1)
            with ExitStack() as ph:
                Wq = sb(ph, "Wq", [128, 8, D], BF16)
                kTb = sb(ph, "kTb", [128, 16, 128], BF16)
                g2b = sb(ph, "g2b", [128, D], F32)
                hb5 = [sb(ph, "hb5_%d" % i, [128, D], F32) for i in range(2)]
                hng = [sb(ph, "hng%d" % i, [128, D], F32) for i in range(1)]
                junkb = sb(ph, "junkb5", [128, D], BF16)
                hngb = [sb(ph, "hngb%d" % i, [128, D], BF16) for i in range(2)]
                NZ = 4
                zb = [sb(ph, "zb%d" % i, [128, D], BF16) for i in range(NZ)]
                hT = sb(ph, "hT", [128, 8, 128], BF16)
                qTt = sb(ph, "qTt", [128, 8, 128], BF16)
                sc = sb(ph, "sc", [128, 16, 128], F32)
                wk = sb(ph, "wk", [128, 256], F32)
                tv = sb(ph, "tv", [128, 16, 16], F32)
                tix = sb(ph, "tix", [128, 16, 16], U32)
                tif = sb(ph, "tif", [128, 16, 16], F32)
                cand = sb(ph, "cand", [128, 8, 256], F32)
                best = sb(ph, "best", [128, 8, 16], F32)
                pos = sb(ph, "pos", [128, 8, 16], U32)
                pa = sb(ph, "pa", [128, 8, 16], I32)
                pbb = sb(ph, "pbb", [128, 8, 16], I32)
                paf = sb(ph, "paf", [128, 8, 16], F32)
                pbf = sb(ph, "pbf", [128, 8, 16], F32)
                oh = sb(ph, "oh", [128, 8, 16, 16], F32)
                sel1 = sb(ph, "sel1", [128, 8, 16], F32)
                sel2 = sb(ph, "sel2", [128, 8, 16], F32)
                idxf = sb(ph, "idxf", [128, 128], F32)
                idx = [sb(ph, "idx%d" % i, [128, 128], I32) for i in range(2)]
                gate = [sb(ph, "gate%d" % i, [128, 8, 16], F32) for i in range(2)]
                gsum = sb(ph, "gsum", [128, 8], F32)
                act_ = [sb(ph, "act%d" % i, [128, 128], F32) for i in range(2)]
                gl_ = [sb(ph, "gl%d" % i, [128, 128], F32) for i in range(2)]
                coef = [sb(ph, "coef%d" % i, [128, 128], F32) for i in range(2)]
                outb = [sb(ph, "outb%d" % i, [128, D], F32) for i in range(1)]
                st5 = sb(ph, "st5", [128, 8], F32)
                st6 = sb(ph, "st6", [128, 8], F32)
                NB, KB, NDG = 24, 4, 8
                P.dma("sp", lambda e: e.dma_start(out=g2b[:], in_=g2_d[l].to_broadcast([128, D])), writes=["g2b"])
                with ExitStack() as wph:
                    kst = sb(wph, "kst", [128, 16, 128], F32)
                    P.dma("sp", lambda e: e.dma_start(out=kst[:], in_=keysT_d[l]), writes=["kst"])
                    P.op("act", lambda e: e.activation(out=kTb[:], in_=kst[:], func=AF.Copy), reads=["kst"], writes=["kTb"])
                    wst = sb(wph, "wst", [128, 8, 512], F32)
                    load_w_bf(wph, "Wq", Wq, w_q_d[l], 8, D, wst=wst)
                    P.barrier()
                uvb = [sb(ph, "uvb%d" % i, [128, 2 * D], BF16) for i in range(NB)]
                dg = [sb(ph, "dg%d" % i, [128, 128], BF16) for i in range(NDG)]

                def front_end(t):
                    p2 = t % 2
                    hb, hk = hb5[p2], "hb5_%d" % p2
                    hg, hgk = hng[0], "hng0"
                    P.dma("sp", lambda e: e.dma_start(out=hb[:], in_=hsrc[t * 128:(t + 1) * 128, :]), reads=[("h_d", t)], writes=[hk])
                    P.op("act", lambda e: e.activation(out=junkb[:], in_=hb[:], func=AF.Square, accum_out=st5[:, 0:1]),
                         reads=[hk], writes=["junkb5", "st5a"])
                    yield
                    yield
                    rstd_from_ss(st5[:, 0:1], st5[:, 1:2], D, "st5a", "st5b")
                    P.op("dve", lambda e: e.scalar_tensor_tensor(out=hg[:], in0=hb[:], scalar=st5[:, 1:2], in1=g2b[:],
                                                                 op0=ALU.mult, op1=ALU.mult),
                         reads=[hk, "st5b", "g2b"], writes=[hgk])
                    yield
                    xs5, xs5k = hngb[p2], "hngb%d" % p2
                    P.op("act", lambda e: e.activation(out=xs5[:], in_=hg[:], func=AF.Copy), reads=[hgk], writes=[xs5k])
                    yield
                    yield
                    for c in range(8):
                        P.op("pe", lambda e: e.transpose(out=pT[:, c * 128:(c + 1) * 128], in_=xs5[:, c * 128:(c + 1) * 128],
                                                         identity=ident[:]), reads=[xs5k, "ident"], writes=[("pT", c)])
                    P.op("dve", lambda e: e.tensor_copy(out=hT[:], in_=pT[:].rearrange("p (c t) -> p c t", c=8)),
                         reads=[("pT", c) for c in range(8)], writes=["hT"])
                    yield
                    yield
                    for hd in range(NH):
                        b = hd // 4
                        for c in range(8):
                            P.op("pe", lambda e: e.matmul(pb[b][:, (hd % 4) * 128:(hd % 4 + 1) * 128],
                                                          lhsT=Wq[:, c, hd * 128:(hd + 1) * 128], rhs=hT[:, c, :],
                                                          start=(c == 0), stop=(c == 7)),
                                 reads=["hT", ("Wq", c, (hd // 4) * 512)], writes=[("pb", b, hd % 4)])
                        yield
                    for b in range(2):
                        evac(qTt[:, 4 * b:4 * b + 4, :], pb[b][:, :].rearrange("p (h t) -> p h t", h=4),
                             [("pb", b, i) for i in range(4)], [("qTt", b)])
                    yield
                    for half8 in range(2):
                        for bl in range(8):
                            blk = half8 * 8 + bl
                            hd = blk // 2
                            b = 2 + bl // 4
                            P.op("pe", lambda e: e.matmul(pb[b][:, (bl % 4) * 128:(bl % 4 + 1) * 128],
                                                          lhsT=qTt[:, hd, :], rhs=kTb[:, blk, :], start=True, stop=True),
                                 reads=[("qTt", hd // 4), "kTb"], writes=[("pb", b, bl % 4)])
                        for b in range(2):
                            g4 = half8 * 2 + b
                            evac(sc[:, 4 * g4:4 * g4 + 4, :], pb[2 + b][:, :].rearrange("p (h t) -> p h t", h=4),
                                 [("pb", 2 + b, i) for i in range(4)], [("sc", g4)])
                        yield
                    for blk in range(16):
                        sk = ("sc", blk // 4)
                        P.op("dve", lambda e: e.max(out=tv[:, blk, 0:8], in_=sc[:, blk, :]), reads=[sk], writes=[("tv", blk, 0)])
                        yield
                        P.op("dve", lambda e: e.match_replace(out=wk[:, 0:128], in_to_replace=tv[:, blk, 0:8],
                                                              in_values=sc[:, blk, :], imm_value=-1e30),
                             reads=[sk, ("tv", blk, 0)], writes=["wk"])
                        yield
                        P.op("dve", lambda e: e.max(out=tv[:, blk, 8:16], in_=wk[:, 0:128]), reads=["wk"], writes=[("tv", blk, 1)])
                        yield
                        P.op("dve", lambda e: e.max_index(out=tix[:, blk, 0:8], in_max=tv[:, blk, 0:8], in_values=sc[:, blk, :]),
                             reads=[sk, ("tv", blk, 0)], writes=[("tix", blk, 0)])
                        yield
                        P.op("dve", lambda e: e.max_index(out=tix[:, blk, 8:16], in_max=tv[:, blk, 8:16], in_values=sc[:, blk, :]),
                             reads=[sk, ("tv", blk, 1)], writes=[("tix", blk, 1)])
                        yield
                        yield
                    TVK = [("tv", b, i) for b in range(16) for i in range(2)]
                    TIK = [("tix", b, i) for b in range(16) for i in range(2)]
                    P.op("dve", lambda e: e.tensor_copy(out=tif[:], in_=tix[:]), reads=TIK, writes=["tif"])
                    yield
                    tv4 = tv[:].rearrange("p (h j) k -> p h j k", j=2)
                    tif4 = tif[:].rearrange("p (h j) k -> p h j k", j=2)
                    for hd in range(NH):
                        P.op("dve", lambda e: e.tensor_tensor(out=cand[:, hd, :].rearrange("p (a b) -> p a b", a=16),
                                                              in0=tv4[:, hd, 0, :].unsqueeze(2).to_broadcast([128, 16, 16]),
                                                              in1=tv4[:, hd, 1:2, :].to_broadcast([128, 16, 16]), op=ALU.add),
                             reads=TVK, writes=[("cand", hd)])
                        yield
                        P.op("dve", lambda e: e.max(out=best[:, hd, 0:8], in_=cand[:, hd, :]), reads=[("cand", hd)], writes=[("best", hd, 0)])
                        yield
                        P.op("dve", lambda e: e.match_replace(out=wk[:, :], in_to_replace=best[:, hd, 0:8], in_values=cand[:, hd, :],
                                                              imm_value=-1e30), reads=[("cand", hd), ("best", hd, 0)], writes=["wk"])
                        yield
                        P.op("dve", lambda e: e.max(out=best[:, hd, 8:16], in_=wk[:, :]), reads=["wk"], writes=[("best", hd, 1)])
                        yield
                        P.op("dve", lambda e: e.max_index(out=pos[:, hd, 0:8], in_max=best[:, hd, 0:8], in_values=cand[:, hd, :]),
                             reads=[("cand", hd), ("best", hd, 0)], writes=[("pos", hd, 0)])
                        yield
                        P.op("dve", lambda e: e.max_index(out=pos[:, hd, 8:16], in_max=best[:, hd, 8:16], in_values=cand[:, hd, :]),
                             reads=[("cand", hd), ("best", hd, 1)], writes=[("pos", hd, 1)])
                        yield
                        yield
                    BK = [("best", h_, i) for h_ in range(NH) for i in range(2)]
                    PK = [("pos", h_, i) for h_ in range(NH) for i in range(2)]
                    posi = pos[:].bitcast(I32)
                    P.op("dve", lambda e: e.tensor_scalar(out=pa[:], in0=posi, scalar1=4, scalar2=None, op0=ALU.arith_shift_right),
                         reads=PK, writes=["pa"])
                    yield
                    P.op("dve", lambda e: e.tensor_scalar(out=pbb[:], in0=posi, scalar1=15, scalar2=None, op0=ALU.bitwise_and),
                         reads=PK, writes=["pbb"])
                    yield
                    P.op("dve", lambda e: e.tensor_copy(out=paf[:], in_=pa[:]), reads=["pa"], writes=["paf"])
                    yield
                    P.op("dve", lambda e: e.tensor_copy(out=pbf[:], in_=pbb[:]), reads=["pbb"], writes=["pbf"])
                    yield
                    yield
                    io4 = iota16[:].unsqueeze(1).unsqueeze(1).to_broadcast([128, 8, 16, 16])
                    for pf, pfk, half, sel, selk in ((paf, "paf", 0, sel1, "sel1"), (pbf, "pbf", 1, sel2, "sel2")):
                        P.op("dve", lambda e: e.tensor_tensor(out=oh[:], in0=pf[:].unsqueeze(3).to_broadcast([128, 8, 16, 16]),
                                                              in1=io4, op=ALU.is_equal), reads=[pfk, "iota16"], writes=["oh"])
                        yield
                        P.op("dve", lambda e: e.tensor_tensor(out=oh[:], in0=oh[:],
                                                              in1=tif4[:, :, half, :].unsqueeze(2).to_broadcast([128, 8, 16, 16]),
                                                              op=ALU.mult), reads=["oh", "tif"], writes=["oh"])
                        yield
                        P.op("dve", lambda e: e.tensor_reduce(out=sel[:], in_=oh[:], axis=AX.X, op=ALU.add), reads=["oh"], writes=[selk])
                        yield
                        yield
                    ix, ixk = idx[p2], "idx%d" % p2
                    P.op("dve", lambda e: e.scalar_tensor_tensor(out=idxf[:], in0=sel1[:].rearrange("p h k -> p (h k)"), scalar=128.0,
                                                                 in1=sel2[:].rearrange("p h k -> p (h k)"), op0=ALU.mult, op1=ALU.add),
                         reads=["sel1", "sel2"], writes=["idxf"])
                    yield
                    if l > 0:
                        P.op("dve", lambda e: e.tensor_scalar(out=idxf[:], in0=idxf[:], scalar1=float(l * NEXP), scalar2=None,
                                                              op0=ALU.add), reads=["idxf"], writes=["idxf"])
                        yield
                    P.op("dve", lambda e: e.tensor_copy(out=ix[:], in_=idxf[:]), reads=["idxf"], writes=[ixk])
                    yield
                    yield
                    gt, gtk = gate[p2], "gate%d" % p2
                    P.op("dve", lambda e: e.tensor_tensor(out=gt[:], in0=best[:], in1=best[:, :, 0:1].to_broadcast([128, 8, 16]),
                                                          op=ALU.subtract), reads=BK, writes=[gtk])
                    yield
                    P.op("act", lambda e: e.activation(out=gt[:], in_=gt[:], func=AF.Exp), reads=[gtk], writes=[gtk])
                    yield
                    P.op("dve", lambda e: e.tensor_reduce(out=gsum[:], in_=gt[:], axis=AX.X, op=ALU.add), reads=[gtk], writes=["gsum"])
                    yield
                    P.op("dve", lambda e: e.reciprocal(out=gsum[:], in_=gsum[:]), reads=["gsum"], writes=["gsum"])
                    yield
                    P.op("dve", lambda e: e.tensor_tensor(out=gt[:], in0=gt[:], in1=gsum[:].unsqueeze(2).to_broadcast([128, 8, 16]),
                                                          op=ALU.mult), reads=[gtk, "gsum"], writes=[gtk])
                    yield
                    if dbg:
                        P.dma("sp", lambda e: e.dma_start(out=idx_dbg[t * 128:(t + 1) * 128, :], in_=ix[:]), reads=[ixk], writes=[("idbg", t)])
                        P.dma("sp", lambda e: e.dma_start(out=gate_dbg[t * 128:(t + 1) * 128, :], in_=gt[:].rearrange("p h k -> p (h k)")),
                              reads=[gtk], writes=[("gdbg", t)])
                        P.dma("sp", lambda e: e.dma_start(out=sc_dbg[t * 128:(t + 1) * 128, :], in_=sc[:].rearrange("p h k -> p (h k)")),
                              reads=[("sc", b_) for b_ in range(4)], writes=[("sdbg", t)])
                    yield

                cnt5 = {"u": 0, "d": 0, "z": 0}

                def k_loop(t, fe_next):
                    p2 = t % 2
                    hb, hk = hb5[p2], "hb5_%d" % p2
                    hg, hgk = hng[0], "hng0"
                    hgb, hgbk = hngb[p2], "hngb%d" % p2
                    ix, ixk = idx[p2], "idx%d" % p2
                    gt, gtk = gate[p2], "gate%d" % p2
                    gtf = gt[:].rearrange("p h k -> p (h k)")
                    av, avk = act_[p2], "act%d" % p2
                    gl, glk = gl_[p2], "gl%d" % p2
                    cf, cfk = coef[p2], "coef%d" % p2
                    NBT = 128 // KB
                    bufs = {}

                    def stA(n):
                        bufs[n] = []
                        for k in range(n * KB, (n + 1) * KB):
                            u_ = cnt5["u"] % NB
                            cnt5["u"] += 1
                            bufs[n].append(u_)
                            P.dma("pool", lambda e: e.indirect_dma_start(out=uvb[u_][:], out_offset=None, in_=uv_d,
                                                                         in_offset=bass.IndirectOffsetOnAxis(ap=ix[:, k:k + 1], axis=0)),
                                  reads=[ixk], writes=["uvb%d" % u_])

                    def stB(n):
                        k0 = n * KB
                        for i, k in enumerate(range(k0, k0 + KB)):
                            u_ = bufs[n][i]
                            z_ = cnt5["z"] % NZ
                            cnt5["z"] += 1
                            P.op("dve", lambda e: e.tensor_tensor(out=zb[z_][:], in0=uvb[u_][:, 0:D], in1=hgb[:], op=ALU.mult),
                                 reads=["uvb%d" % u_, hgbk], writes=["zb%d" % z_])
                            P.op("act", lambda e: e.activation(out=zb[z_][:], in_=zb[z_][:], func=AF.Copy, accum_out=av[:, k:k + 1]),
                                 reads=["zb%d" % z_], writes=["zb%d" % z_, (avk, k0, i)])
                        ks = slice(k0, k0 + KB)
                        P.op("act", lambda e: e.activation(out=gl[:, ks], in_=av[:, ks], func=AF.Gelu_apprx_tanh),
                             reads=[(avk, k0, i) for i in range(KB)], writes=[(glk, k0)])

                    def stC(n):
                        k0 = n * KB
                        ks = slice(k0, k0 + KB)
                        P.op("dve", lambda e: e.tensor_tensor(out=cf[:, ks], in0=gl[:, ks], in1=gtf[:, ks], op=ALU.mult),
                             reads=[(glk, k0), gtk], writes=[(cfk, k0)])
                        for i, k in enumerate(range(k0, k0 + KB)):
                            u_ = bufs[n][i]
                            d_ = cnt5["d"] % NDG
                            cnt5["d"] += 1
                            P.op("dve", lambda e: e.tensor_scalar(out=dg[d_][:], in0=ident[:], scalar1=cf[:, k:k + 1], scalar2=None,
                                                                  op0=ALU.mult), reads=["ident", (cfk, k0)], writes=["dg%d" % d_])
                            for hf in range(2):
                                P.op("pe", lambda e: e.matmul(pb[4 + hf][:, :], lhsT=dg[d_][:], rhs=uvb[u_][:, D + hf * 512:D + (hf + 1) * 512],
                                                              start=(k == 0), stop=(k == 127)),
                                     reads=["dg%d" % d_, "uvb%d" % u_], writes=[("pb", 4 + hf)])

                    LA = 3
                    for n in range(min(LA, NBT)):
                        stA(n)
                    for n in range(NBT):
                        if n + LA < NBT:
                            stA(n + LA)
                        stB(n)
                        if n >= 1:
                            stC(n - 1)
                        for _ in range(7):
                            next(fe_next, None)
                    stC(NBT - 1)
                    for _ in fe_next:
                        pass
                    ob, obk = outb[0], "outb0"
                    for hf in range(2):
                        hs = slice(hf * 512, (hf + 1) * 512)
                        P.op("dve", lambda e: e.tensor_tensor(out=ob[:, hs], in0=pb[4 + hf][:, :], in1=hb[:, hs], op=ALU.add),
                             reads=[("pb", 4 + hf), hk], writes=[(obk, hf)])
                    OBK2 = [(obk, 0), (obk, 1)]
                    if final:
                        P.op("act", lambda e: e.activation(out=junkb[:], in_=ob[:], func=AF.Square, accum_out=st6[:, 0:1]),
                             reads=OBK2, writes=["junkb5", "st6a"])
                        rstd_from_ss(st6[:, 0:1], st6[:, 1:2], D, "st6a", "st6b")
                        P.op("dve", lambda e: e.scalar_tensor_tensor(out=ob[:], in0=ob[:], scalar=st6[:, 1:2], in1=nfb[:],
                                                                     op0=ALU.mult, op1=ALU.mult),
                             reads=OBK2 + ["st6b", "nfb"], writes=OBK2)
                        P.dma("sp", lambda e: e.dma_start(out=out_d[t * 128:(t + 1) * 128, :], in_=ob[:]), reads=OBK2,
                              writes=[("out_d", t)])
                    else:
                        P.dma("sp", lambda e: e.dma_start(out=h_d[t * 128:(t + 1) * 128, :], in_=ob[:]), reads=OBK2,
                              writes=[("h_d", t)])

                for _ in front_end(0):
                    pass
                for t in range(NT):
                    fe_next = front_end(t + 1) if t + 1 < NT else iter(())
                    if "x" in phases:
                        for _ in fe_next:
                            pass
                        continue
                    k_loop(t, fe_next)
                P.barrier()
            hsrc = h_d
    P.barrier()
    top.close()
    return nc, P


def _consts():
    ident = np.eye(128, dtype=np.float32)
    k = np.arange(128)
    negmask = np.where(k[:, None] > k[None, :], -30000.0, 0.0).astype(np.float32)
    gmask = (k[:, None] // 16 == np.arange(8)[None, :]).astype(np.float32)
    iota16 = np.broadcast_to(np.arange(16, dtype=np.float32), (128, 16)).copy()
    return {"ident": ident, "negmask": negmask, "gmask": gmask, "iota16": iota16}


def layout_weights(inp, L):
    f = lambda a: np.ascontiguousarray(np.asarray(a, dtype=np.float32))
    w = {}
    w["g1T"] = f(inp["norm1_g"].reshape(L, 8, 128).transpose(0, 2, 1))
    w["w_in"] = f(inp["w_in"])
    w["b_f"] = f(inp["fox_b_f"].reshape(L, 8, 1))
    lam_re, lam_im = np.asarray(inp["ssm_lambda_re"]), np.asarray(inp["ssm_lambda_im"])
    ldt = np.asarray(inp["ssm_log_dt"])
    rep = lambda a: f(np.repeat(a[:, :, None, :], 16, axis=2).reshape(L, 4, 128, 64))
    w["lamB_re"] = rep(lam_re)
    w["lamB_im"] = rep(lam_im)
    w["dtB"] = f(np.repeat(ldt[:, :, None], 16, axis=2).reshape(L, 4, 128, 1))
    w["bT_re"] = f(np.asarray(inp["ssm_b_re"]).transpose(0, 1, 3, 2).reshape(L, 4, 128, 64))
    w["bT_im"] = f(np.asarray(inp["ssm_b_im"]).transpose(0, 1, 3, 2).reshape(L, 4, 128, 64))
    tl = lambda a: f(a.reshape(L, 16, 2, 64).transpose(0, 2, 3, 1).reshape(L, 128, 16))
    w["lamT_re"] = tl(lam_re)
    w["lamT_im"] = tl(lam_im)
    w["dtT"] = tl(np.repeat(ldt[:, :, None], 64, axis=2))
    def cblk(c):
        c = np.asarray(c).reshape(L, 16, 2, 16, 64)
        o = np.zeros((L, 2, 64, 16, 2, 16), np.float32)
        for g2 in range(2):
            o[:, g2, :, :, g2, :] = c[:, :, g2].transpose(0, 3, 1, 2)
        return f(o.reshape(L, 128, 16, 32))
    w["cT_re"] = cblk(inp["ssm_c_re"])
    w["cT_im"] = cblk(inp["ssm_c_im"])
    d = np.asarray(inp["ssm_d"]).reshape(L, 4, 4, 32)
    dm = np.zeros((L, 4, 32, 4, 4, 32), np.float32)
    for i in range(4):
        for m in range(32):
            dm[:, i, m, :, i, m] = d[:, :, i, m]
    w["dmat"] = f(dm.reshape(L, 128, 16, 32))
    w["w_glu"] = f(inp["ssm_w_glu"])
    w["bgluT"] = f(np.asarray(inp["ssm_b_glu"]).reshape(L, 4, 128).transpose(0, 2, 1))
    gsa = np.concatenate([np.asarray(inp["g_ssm_out"]), np.asarray(inp["g_attn_out"])], axis=1)
    w["gsaT"] = f(gsa.reshape(L, 8, 128).transpose(0, 2, 1))
    w["w_o"] = f(inp["w_o"])
    w["g2"] = f(np.asarray(inp["norm2_g"]).reshape(L, 1, D))
    w["w_q"] = f(inp["peer_w_q"])
    kk = np.asarray(inp["peer_keys"]).transpose(0, 2, 4, 1, 3)
    kz = np.zeros((L, 2, 64, 8, 2, 128), np.float32)
    for j in range(2):
        kz[:, j, :, :, j, :] = kk[:, j]
    w["keysT"] = f(kz.reshape(L, 128, 16, 128))
    w["peer_u"] = f(np.asarray(inp["peer_u"]).reshape(L * NEXP, D))
    w["peer_v"] = f(np.asarray(inp["peer_v"]).reshape(L * NEXP, D))
    w["norm_f"] = f(np.asarray(inp["norm_f"]).reshape(1, D))
    w.update(_consts())
    return w


def kernel(**inputs):
    x = np.asarray(inputs["x"], dtype=np.float32)
    B, S, _ = x.shape
    L = int(np.asarray(inputs["w_in"]).shape[0])
    w = layout_weights(inputs, L)
    nc, _ = build(L, S)
    in_maps = []
    for b in range(B):
        m = dict(w)
        m["x"] = np.ascontiguousarray(x[b])
        in_maps.append(m)
    res = run_bass_kernel_spmd(nc, in_maps, core_ids=list(range(B)))
    return np.stack([np.asarray(r["out"], dtype=np.float32) for r in res.results], axis=0)
```
